# Optimizing a Trainium2 kernel written in Bass

```python
import numpy as np
import jax
import jax.numpy as jnp
from jax import lax

D_MODEL = 1024
BATCH = 16
SEQ = 2048
DEPTH = 2

GRID_W = 64
CTX_LEN = 256
HEAD_DIM = 64
ATTN_SCALE = HEAD_DIM ** -0.5
NA_HEADS = 4
NA_WIN_H = 8
NA_WIN_W = 16
NA_QCOLS = 16
NA_KCOLS = NA_WIN_W + NA_QCOLS
HG_HEADS = 4
HG_DK = 128
HG_DV = 128
HG_CHUNK = 64
SWA_Q_HEADS = 4
SWA_KV_HEADS = 2
SWA_WINDOW = 128
SWA_BLOCK = 128
ROPE_BASE = 10000.0
N_EXPERTS = 32
TOP_K = 4
D_FF_EXPERT = 1024
SWIGLU_LIMIT = 7.0
SWIGLU_ALPHA = 1.702
MOE_BLOCK = 256
NORM_EPS = 1e-6
NEG_INF = -1e30
NA_W = NA_HEADS * HEAD_DIM
HG_W = HG_HEADS * HG_DV
HG_KW = HG_HEADS * HG_DK
SWA_W = SWA_Q_HEADS * HEAD_DIM
SWA_KV_W = SWA_KV_HEADS * HEAD_DIM
MIX_W = NA_W + HG_W + SWA_W
IN_WIDTHS = (NA_W, NA_W, NA_W, HG_KW, HG_KW, HG_KW, HG_W, HG_W, SWA_W, SWA_KV_W, SWA_KV_W)
IN_COLS = sum(IN_WIDTHS)

kernel_name = 'hybrid_na_hgrn2_swa_moe_dit'


def _rmsnorm(x, g):
    xf = x.astype(jnp.float32)
    y = xf * lax.rsqrt(jnp.mean(xf * xf, axis=-1, keepdims=True) + NORM_EPS)
    return (y * g.astype(jnp.float32)).astype(x.dtype)


def _heads(t, n):
    return t.reshape(t.shape[0], t.shape[1], n, -1)


def _rope_2d(x, row, col):
    half = x.shape[-1] // 2
    quarter = half // 2
    inv = ROPE_BASE ** (-jnp.arange(quarter, dtype=jnp.float32) / quarter)

    def rot(t, pos):
        ang = pos.astype(jnp.float32)[:, None] * inv[None, :]
        cos = jnp.cos(ang)[:, None, :]
        sin = jnp.sin(ang)[:, None, :]
        t = t.astype(jnp.float32)
        t1, t2 = t[..., :quarter], t[..., quarter:]
        return jnp.concatenate([t1 * cos - t2 * sin, t2 * cos + t1 * sin], axis=-1)

    return jnp.concatenate([rot(x[..., :half], row), rot(x[..., half:], col)], axis=-1).astype(x.dtype)


def _context_attention(q, k, v, sink):
    B, Lc, HQ, Dh = q.shape
    HKV = k.shape[2]
    G = HQ // HKV
    qg = q.reshape(B, Lc, HKV, G, Dh)
    s = jnp.einsum('bqkgd,bckd->bkgqc', qg, k, preferred_element_type=jnp.float32) * ATTN_SCALE
    if sink is not None:
        sink_col = jnp.broadcast_to(sink.astype(jnp.float32).reshape(1, HKV, G, 1, 1), s.shape[:-1] + (1,))
        s = jnp.concatenate([s, sink_col], axis=-1)
    p = jax.nn.softmax(s, axis=-1)[..., :Lc].astype(v.dtype)
    return jnp.einsum('bkgqc,bckd->bqkgd', p, v).reshape(B, Lc, HQ, Dh)


def _neighborhood_attention(q, k, v, kc, vc, rpb):
    B, L, H, Dh = q.shape
    R = L // GRID_W
    wr = min(NA_WIN_H, R)
    nbw = GRID_W // NA_QCOLS
    rows = np.arange(R)
    row_idx = np.clip(rows - wr // 2, 0, R - wr)[:, None] + np.arange(wr)[None, :]
    q_cols = np.arange(GRID_W).reshape(nbw, NA_QCOLS)
    q_start = np.clip(q_cols - NA_WIN_W // 2, 0, GRID_W - NA_WIN_W)
    k_start = np.clip(np.arange(nbw) * NA_QCOLS - NA_WIN_W // 2, 0, GRID_W - NA_KCOLS)
    key_cols = k_start[:, None] + np.arange(NA_KCOLS)[None, :]
    col_ok = (key_cols[:, None, :] >= q_start[:, :, None]) & (key_cols[:, None, :] < q_start[:, :, None] + NA_WIN_W)
    dr = row_idx - rows[:, None] + (NA_WIN_H - 1)
    dc = np.clip(key_cols[:, None, :] - q_cols[:, :, None] + (NA_WIN_W - 1), 0, 2 * NA_WIN_W - 2)
    bias = rpb.astype(jnp.float32)[:, dr[:, None, None, :, None], dc[None, :, :, None, :]]
    qg = q.reshape(B, R, nbw, NA_QCOLS, H, Dh)
    kg = k.reshape(B, R, GRID_W, H, Dh)[:, :, key_cols][:, row_idx]
    vg = v.reshape(B, R, GRID_W, H, Dh)[:, :, key_cols][:, row_idx]
    s = jnp.einsum('brjqhd,brwjmhd->bhrjqwm', qg, kg, preferred_element_type=jnp.float32) * ATTN_SCALE + bias
    s = jnp.where(col_ok[:, :, None, :], s, NEG_INF)
    s_ctx = jnp.einsum('brjqhd,bchd->bhrjqc', qg, kc, preferred_element_type=jnp.float32) * ATTN_SCALE
    n_lat = wr * NA_KCOLS
    p = jax.nn.softmax(jnp.concatenate([s.reshape(s.shape[:5] + (n_lat,)), s_ctx], axis=-1), axis=-1).astype(v.dtype)
    p_lat = p[..., :n_lat].reshape(s.shape)
    p_ctx = p[..., n_lat:]
    o = jnp.einsum('bhrjqwm,brwjmhd->brjqhd', p_lat, vg) + jnp.einsum('bhrjqc,bchd->brjqhd', p_ctx, vc)
    return o.reshape(B, L, H, Dh)


def _window_attention(q, k, v, kc, vc, sink):
    B, L, HQ, Dh = q.shape
    HKV = k.shape[2]
    G = HQ // HKV
    nb = L // SWA_BLOCK
    qb = q.reshape(B, nb, SWA_BLOCK, HKV, G, Dh)

    def band(t):
        tp = jnp.pad(t, ((0, 0), (SWA_BLOCK, SWA_BLOCK), (0, 0), (0, 0))).reshape(B, nb + 2, SWA_BLOCK, HKV, Dh)
        return jnp.concatenate([tp[:, :-2], tp[:, 1:-1], tp[:, 2:]], axis=2)

    kb, vb = band(k), band(v)
    blk = np.arange(nb)[:, None, None]
    qpos = blk * SWA_BLOCK + np.arange(SWA_BLOCK)[None, :, None]
    kpos = (blk - 1) * SWA_BLOCK + np.arange(3 * SWA_BLOCK)[None, None, :]
    ok = (np.abs(qpos - kpos) <= SWA_WINDOW) & (kpos >= 0) & (kpos < L)
    s = jnp.einsum('bnqkgd,bnmkd->bkgnqm', qb, kb, preferred_element_type=jnp.float32) * ATTN_SCALE
    s = jnp.where(ok, s, NEG_INF)
    s_ctx = jnp.einsum('bnqkgd,bckd->bkgnqc', qb, kc, preferred_element_type=jnp.float32) * ATTN_SCALE
    sink_col = jnp.broadcast_to(sink.astype(jnp.float32).reshape(1, HKV, G, 1, 1, 1), s_ctx.shape[:-1] + (1,))
    n_lat = 3 * SWA_BLOCK
    n_ctx = kc.shape[1]
    p = jax.nn.softmax(jnp.concatenate([s, s_ctx, sink_col], axis=-1), axis=-1).astype(v.dtype)
    o = (jnp.einsum('bkgnqm,bnmkd->bnqkgd', p[..., :n_lat], vb)
         + jnp.einsum('bkgnqc,bckd->bnqkgd', p[..., n_lat:n_lat + n_ctx], vc))
    return o.reshape(B, L, HQ, Dh)


def _hgrn2_scan(q, k, v, logf, s0):
    B, L, H, DK = q.shape
    DV = v.shape[-1]
    n = L // HG_CHUNK

    def chunks(t):
        return t.reshape(B, n, HG_CHUNK, H, t.shape[-1]).transpose(1, 0, 3, 2, 4)

    tril = np.tril(np.ones((HG_CHUNK, HG_CHUNK), dtype=bool))

    def step(S, xs):
        qc, kc, vc, gc = xs
        b = jnp.cumsum(gc, axis=2)
        diff = b[:, :, :, None, :] - b[:, :, None, :, :]
        decay = jnp.exp(jnp.where(tril[:, :, None], diff, -jnp.inf))
        attn = jnp.einsum('bhtk,bhtsk,bhsk->bhts', qc, decay, kc)
        o = attn @ vc + jnp.einsum('bhtk,bhkv->bhtv', qc * jnp.exp(b), S)
        b_end = b[:, :, -1, :]
        S_new = (jnp.exp(b_end)[..., None] * S
                 + jnp.einsum('bhsk,bhsv->bhkv', kc * jnp.exp(b_end[:, :, None, :] - b), vc))
        return S_new, o

    S_final, o = lax.scan(step, s0, (chunks(q), chunks(k), chunks(v), chunks(logf)))
    return o.transpose(1, 0, 3, 2, 4).reshape(B, L, H, DV), S_final


def _hgrn2_final_state(k, logf, v):
    b = jnp.cumsum(logf, axis=1)
    return jnp.einsum('blhk,blhv->bhkv', k * jnp.exp(b[:, -1:] - b), v)


def _token_mixers(h, hc, w_in, lb, na_q_norm, na_k_norm, na_rpb, hg_norm_g, swa_q_norm, swa_k_norm, swa_sink, need_ctx):
    B, L, _ = h.shape
    splits = np.cumsum(IN_WIDTHS)[:-1].tolist()
    (na_q, na_k, na_v, hg_q, hg_ff, hg_fb, hg_i, hg_g, sw_q, sw_k, sw_v) = jnp.split(h @ w_in, splits, axis=-1)
    (cna_q, cna_k, cna_v, chg_q, chg_ff, chg_fb, chg_i, chg_g, csw_q, csw_k, csw_v) = jnp.split(hc @ w_in, splits, axis=-1)
    t = jnp.arange(L)
    row = t // GRID_W
    col = t % GRID_W

    ck_na = _rmsnorm(_heads(cna_k, NA_HEADS), na_k_norm)
    cv_na = _heads(cna_v, NA_HEADS)
    o_na = _neighborhood_attention(_rmsnorm(_heads(na_q, NA_HEADS), na_q_norm),
                                   _rmsnorm(_heads(na_k, NA_HEADS), na_k_norm),
                                   _heads(na_v, NA_HEADS), ck_na, cv_na, na_rpb)

    lbh = lb.reshape(HG_HEADS, HG_DK).astype(jnp.float32)

    def fgate(z):
        f = lbh + (1.0 - lbh) * jax.nn.sigmoid(_heads(z, HG_HEADS).astype(jnp.float32))
        return jnp.log(f), 1.0 - f

    def qin(z):
        return jax.nn.silu(_heads(z, HG_HEADS).astype(jnp.float32)) * HG_DK ** -0.5

    def readout(o, z):
        return (_rmsnorm(o, hg_norm_g) * jax.nn.silu(_heads(z, HG_HEADS).astype(jnp.float32))).astype(h.dtype)

    def flip(a):
        return a[:, ::-1]

    lf_f, k_f = fgate(hg_ff)
    lf_b, k_b = fgate(hg_fb)
    v_hg = _heads(hg_i, HG_HEADS).astype(jnp.float32)
    clf_f, ck_f = fgate(chg_ff)
    clf_b, ck_b = fgate(chg_fb)
    cv_hg = _heads(chg_i, HG_HEADS).astype(jnp.float32)
    if need_ctx:
        cq_hg = qin(chg_q)
        s0 = jnp.zeros((B, HG_HEADS, HG_DK, HG_DV), jnp.float32)
        oc_f, s_f = _hgrn2_scan(cq_hg, ck_f, cv_hg, clf_f, s0)
        oc_b, s_b = _hgrn2_scan(flip(cq_hg), flip(ck_b), flip(cv_hg), flip(clf_b), s0)
        oc_hg = readout(oc_f + flip(oc_b), chg_g)
    else:
        s_f = _hgrn2_final_state(ck_f, clf_f, cv_hg)
        s_b = _hgrn2_final_state(flip(ck_b), flip(clf_b), flip(cv_hg))
    q_hg = qin(hg_q)
    o_f, _ = _hgrn2_scan(q_hg, k_f, v_hg, lf_f, s_f)
    o_b, _ = _hgrn2_scan(flip(q_hg), flip(k_b), flip(v_hg), flip(lf_b), s_b)
    o_hg = readout(o_f + flip(o_b), hg_g)

    q_sw = _rope_2d(_rmsnorm(_heads(sw_q, SWA_Q_HEADS), swa_q_norm), row, col)
    k_sw = _rope_2d(_rmsnorm(_heads(sw_k, SWA_KV_HEADS), swa_k_norm), row, col)
    ck_sw = _rmsnorm(_heads(csw_k, SWA_KV_HEADS), swa_k_norm)
    cv_sw = _heads(csw_v, SWA_KV_HEADS)
    o_sw = _window_attention(q_sw, k_sw, _heads(sw_v, SWA_KV_HEADS), ck_sw, cv_sw, swa_sink)

    y = jnp.concatenate([o_na.reshape(B, L, NA_W), o_hg.reshape(B, L, HG_W), o_sw.reshape(B, L, SWA_W)], axis=-1)
    if not need_ctx:
        return y, None
    Lc = hc.shape[1]
    oc_na = _context_attention(_rmsnorm(_heads(cna_q, NA_HEADS), na_q_norm), ck_na, cv_na, None)
    oc_sw = _context_attention(_rmsnorm(_heads(csw_q, SWA_Q_HEADS), swa_q_norm), ck_sw, cv_sw, swa_sink)
    yc = jnp.concatenate([oc_na.reshape(B, Lc, NA_W), oc_hg.reshape(B, Lc, HG_W), oc_sw.reshape(B, Lc, SWA_W)], axis=-1)
    return y, yc


def _moe_ffn(h, router_w, router_b, w_gu, b_gu, w_down, b_down):
    T, D = h.shape
    logits = (h @ router_w).astype(jnp.float32) + router_b.astype(jnp.float32)
    top_v, top_e = lax.top_k(logits, TOP_K)
    gate = jax.nn.softmax(top_v, axis=-1)
    A = T * TOP_K
    flat_e = top_e.reshape(-1)
    flat_tok = jnp.repeat(jnp.arange(T, dtype=jnp.int32), TOP_K)
    flat_g = gate.reshape(-1)
    onehot = (flat_e[:, None] == jnp.arange(N_EXPERTS)[None, :]).astype(jnp.int32)
    counts = onehot.sum(axis=0)
    rank = jnp.take_along_axis(jnp.cumsum(onehot, axis=0), flat_e[:, None], axis=1)[:, 0] - 1
    padded = (counts + MOE_BLOCK - 1) // MOE_BLOCK * MOE_BLOCK
    pend = jnp.cumsum(padded)
    dest = (pend - padded)[flat_e] + rank
    n_blocks = (A + N_EXPERTS * (MOE_BLOCK - 1) + MOE_BLOCK - 1) // MOE_BLOCK
    P = n_blocks * MOE_BLOCK
    slot_tok = jnp.zeros((P,), jnp.int32).at[dest].set(flat_tok)
    slot_g = jnp.zeros((P,), jnp.float32).at[dest].set(flat_g)
    block_e = jnp.minimum(jnp.searchsorted(pend, jnp.arange(n_blocks) * MOE_BLOCK, side='right'), N_EXPERTS - 1)

    def step(out, xs):
        tok, g, e = xs
        gu = h[tok] @ w_gu[e] + b_gu[e]
        glu = jnp.minimum(gu[:, :D_FF_EXPERT], SWIGLU_LIMIT)
        lin = jnp.clip(gu[:, D_FF_EXPERT:], -SWIGLU_LIMIT, SWIGLU_LIMIT)
        act = glu * jax.nn.sigmoid(SWIGLU_ALPHA * glu) * (lin + 1.0)
        y = act @ w_down[e] + b_down[e]
        return out.at[tok].add((y * g[:, None]).astype(out.dtype)), None

    out, _ = lax.scan(step, jnp.zeros_like(h),
                      (slot_tok.reshape(n_blocks, MOE_BLOCK), slot_g.reshape(n_blocks, MOE_BLOCK), block_e))
    return out


def setup_inputs(seed: int = 0) -> dict:
    key = jax.random.key(seed)
    keys = iter(jax.random.split(key, 24))
    D = D_MODEL

    def nrm(shape, s):
        return jax.random.normal(next(keys), shape, jnp.float32) * s

    return {
        'x': nrm((BATCH, SEQ, D), 1.0),
        'c': nrm((BATCH, D), 1.0),
        'ctx': nrm((BATCH, CTX_LEN, D), 1.0),
        'c_ctx': nrm((D,), 1.0),
        'hg_lower_bounds': nrm((DEPTH, HG_KW), 1.0),
        'ada_w': nrm((DEPTH, D, 6 * D), 0.5 * D ** -0.5),
        'ada_b': nrm((DEPTH, 6 * D), 0.02),
        'norm1_g': 1.0 + nrm((DEPTH, D), 0.02),
        'norm2_g': 1.0 + nrm((DEPTH, D), 0.02),
        'w_in': nrm((DEPTH, D, IN_COLS), D ** -0.5),
        'na_q_norm': 1.0 + nrm((DEPTH, HEAD_DIM), 0.02),
        'na_k_norm': 1.0 + nrm((DEPTH, HEAD_DIM), 0.02),
        'na_rpb': nrm((DEPTH, NA_HEADS, 2 * NA_WIN_H - 1, 2 * NA_WIN_W - 1), 0.1),
        'hg_norm_g': 1.0 + nrm((DEPTH, HG_DV), 0.02),
        'swa_q_norm': 1.0 + nrm((DEPTH, HEAD_DIM), 0.02),
        'swa_k_norm': 1.0 + nrm((DEPTH, HEAD_DIM), 0.02),
        'swa_sink': nrm((DEPTH, SWA_Q_HEADS), 0.5),
        'w_out': nrm((DEPTH, MIX_W, D), MIX_W ** -0.5),
        'router_w': nrm((DEPTH, D, N_EXPERTS), D ** -0.5),
        'router_b': nrm((DEPTH, N_EXPERTS), 0.01),
        'w_gu': nrm((DEPTH, N_EXPERTS, D, 2 * D_FF_EXPERT), D ** -0.5),
        'b_gu': nrm((DEPTH, N_EXPERTS, 2 * D_FF_EXPERT), 0.01),
        'w_down': nrm((DEPTH, N_EXPERTS, D_FF_EXPERT, D), D_FF_EXPERT ** -0.5),
        'b_down': nrm((DEPTH, N_EXPERTS, D), 0.01),
    }


def reference(x, c, ctx, c_ctx, hg_lower_bounds, ada_w, ada_b, norm1_g, norm2_g, w_in, na_q_norm, na_k_norm,
              na_rpb, hg_norm_g, swa_q_norm, swa_k_norm, swa_sink, w_out, router_w, router_b, w_gu, b_gu,
              w_down, b_down):
    B, L, D = x.shape
    Lc = ctx.shape[1]
    p_lb = jax.nn.softmax(hg_lower_bounds.astype(jnp.float32), axis=0)
    lbs = jnp.cumsum(p_lb, axis=0) - p_lb[0]
    for l in range(DEPTH):
        last = l == DEPTH - 1
        n_mod_c = 2 if last else 6
        mod = jax.nn.silu(c) @ ada_w[l] + ada_b[l]
        sh1, sc1, g1, sh2, sc2, g2 = [m[:, None, :] for m in jnp.split(mod, 6, axis=-1)]
        cmods = jnp.split(jax.nn.silu(c_ctx) @ ada_w[l][:, :n_mod_c * D] + ada_b[l][:n_mod_c * D], n_mod_c)
        h = _rmsnorm(x, norm1_g[l]) * (1.0 + sc1) + sh1
        hc = _rmsnorm(ctx, norm1_g[l]) * (1.0 + cmods[1]) + cmods[0]
        y, yc = _token_mixers(h, hc, w_in[l], lbs[l], na_q_norm[l], na_k_norm[l], na_rpb[l], hg_norm_g[l],
                              swa_q_norm[l], swa_k_norm[l], swa_sink[l], not last)
        x = x + g1 * (y @ w_out[l])
        h2 = _rmsnorm(x, norm2_g[l]) * (1.0 + sc2) + sh2
        if last:
            f = _moe_ffn(h2.reshape(B * L, D), router_w[l], router_b[l], w_gu[l], b_gu[l], w_down[l], b_down[l])
            x = x + g2 * f.reshape(B, L, D)
        else:
            ctx = ctx + cmods[2] * (yc @ w_out[l])
            hc2 = _rmsnorm(ctx, norm2_g[l]) * (1.0 + cmods[4]) + cmods[3]
            f = _moe_ffn(jnp.concatenate([hc2.reshape(B * Lc, D), h2.reshape(B * L, D)], axis=0),
                         router_w[l], router_b[l], w_gu[l], b_gu[l], w_down[l], b_down[l])
            ctx = ctx + cmods[5] * f[:B * Lc].reshape(B, Lc, D)
            x = x + g2 * f[B * Lc:].reshape(B, L, D)
    return x
```

```python
import numpy as np
from contextlib import ExitStack
import concourse.bass as bass
import concourse.mybir as mybir
from concourse.bass_utils import run_bass_kernel_spmd

F32 = mybir.dt.float32
BF16 = mybir.dt.bfloat16
AF = mybir.ActivationFunctionType
ALU = mybir.AluOpType
AX = mybir.AxisListType

D = 1024
GRID_W = 64
HD = 64
NA_H = 4
HG_H = 4
HG_DK = 128
SW_QH = 4
SW_KVH = 2
IN_COLS = 3840
EPS = 1e-6
NEG = -1e30
TOPK = 4
C_NAQ, C_NAK, C_NAV = 0, 256, 512
C_HGQ, C_HGFF, C_HGFB, C_HGI, C_HGG = 768, 1280, 1792, 2304, 2816
C_SWQ, C_SWK, C_SWV = 3328, 3584, 3712


class Cfg:
    def __init__(self, nb=2, L=2048, LC=256, NE=32, DFF=1024, depth=2, TG=1536):
        self.nb, self.L, self.LC, self.NE, self.DFF, self.depth, self.TG = nb, L, LC, NE, DFF, depth, TG
        self.N = L + LC
        self.T = nb * self.N
        self.R = L // GRID_W


class Res:
    __slots__ = ("name", "w", "rs", "sem", "cnt", "swq", "used")

    def __init__(self, name):
        self.name, self.w, self.rs, self.sem, self.cnt, self.used = name, {}, {}, None, 0, False


class _Rec:
    def __getattr__(self, name):
        def f(*a, **k):
            self.call = (name, a, k)
            return self
        return f


class _Eng:
    def __init__(self, key, sem):
        self.key, self.sem, self.count, self.ops, self.last, self.pending, self.waited = key, sem, 0, [], None, False, {}


class Prog:
    def __init__(self, nc, es):
        self.nc, self.es = nc, es
        self.eng = {}
        for k in ("pe", "act", "dve", "pool", "sp"):
            self.eng[k] = _Eng(k, es.enter_context(nc.semaphore("e_" + k)))
        self.semown = {id(e.sem): e for e in self.eng.values()}
        self.nsem = 5
        self.dma_res = []
        self.sem_pool = {}
        self.all_res = []
        self.nres = 0

    def res(self, name):
        self.nres += 1
        r = Res(name)
        self.all_res.append(r)
        return r

    def _wait(self, E, ev):
        if ev is None:
            return
        sem, val = ev
        own = self.semown.get(id(sem))
        if own is not None and own is E and E.key == "pe":
            return
        if own is not None and val > own.count:
            assert val == own.count + 1 and own.pending, (own.key, val, own.count)
            own.last["inc"] = True
            own.count += 1
            own.pending = False
        if E.waited.get(id(sem), 0) >= val:
            return
        E.waited[id(sem)] = val
        E.ops.append({"wait": (sem, val)})

    def _deps(self, E, R, W, dma_write=False):
        for r in R:
            for ev in list(r.w.values()):
                self._wait(E, ev)
        for w in W:
            for ev in list(w.w.values()):
                if dma_write and id(ev[0]) not in self.semown:
                    continue
                self._wait(E, ev)
            for ev in list(w.rs.values()):
                self._wait(E, ev)

    def _commit(self, ev, R, W, dma_write=False):
        for r in R:
            old = r.rs.get(id(ev[0]))
            if old is None or old[1] < ev[1]:
                r.rs[id(ev[0])] = ev
        for w in W:
            if dma_write:
                w.w = {k: v for k, v in w.w.items() if k not in self.semown}
                w.w[id(ev[0])] = ev
            else:
                w.w = {id(ev[0]): ev}
            w.rs = {}

    def op(self, ek, fn, R=(), W=()):
        E = self.eng[ek]
        self._deps(E, R, W)
        rec = _Rec()
        fn(rec)
        ent = {"fn": rec.call, "inc": False}
        E.ops.append(ent)
        E.last = ent
        E.pending = True
        self._commit((E.sem, E.count + 1), R, W)

    def dma(self, qk, out, in_, R=(), W=(), **kw):
        E = self.eng[qk]
        is_store = ("DRam" in type(out.tensor).__name__) and ("DRam" not in type(in_.tensor).__name__)
        own = R[0] if is_store else W[0]
        if own.sem is None:
            pool = self.sem_pool.setdefault(qk == "pool", [])
            own.swq = (qk == "pool")
            if pool:
                own.sem, own.cnt = pool.pop()
            else:
                own.sem, own.cnt = self.es.enter_context(self.nc.semaphore("d_%d" % self.nsem)), 0
                self.nsem += 1
            self.dma_res.append(own)
        self._deps(E, R, W, dma_write=True)
        if own.sem is not None and getattr(own, "used", False):
            if is_store:
                cont = id(own.sem) in own.rs
            else:
                cont = (not own.rs) and all(k not in self.semown for k in own.w)
            if not cont:
                self._wait(E, (own.sem, own.cnt))
        own.used = True
        own.cnt += 16
        E.ops.append({"dma": (out, in_, kw, own.sem)})
        self._commit((own.sem, own.cnt), R, W, dma_write=True)

    def barrier(self, tok_d, r_bar):
        sp = self.eng["sp"]
        for k in ("pe", "act", "dve", "pool"):
            X = self.eng[k]
            if X.pending:
                self._wait(sp, (X.sem, X.count + 1))
            elif X.count > 0:
                self._wait(sp, (X.sem, X.count))
        for r in self.dma_res:
            if r is not r_bar:
                self._wait(sp, (r.sem, r.cnt))
        if r_bar.sem is not None:
            self._wait(sp, (r_bar.sem, r_bar.cnt))
        self.dma("sp", tok_d[0], tok_d[1], W=[r_bar])
        for r in self.dma_res:
            if r.swq:
                self._wait(self.eng["pool"], (r.sem, r.cnt))
        for k in ("pe", "act", "dve", "pool"):
            for ev in list(r_bar.w.values()):
                self._wait(self.eng[k], ev)
        for r in self.all_res:
            r.w = {}
            r.rs = {}
            if r.sem is not None and r is not r_bar:
                self.sem_pool[r.swq].append((r.sem, r.cnt))
                r.sem = None
                r.used = False
        self.dma_res = []

    def finish(self, outs):
        E = self.eng["sp"]
        for r in outs:
            for ev in list(r.w.values()):
                self._wait(E, ev)
        nc = self.nc
        with nc.Block() as block:
            def run(E, e):
                for o in E.ops:
                    if "wait" in o:
                        e.wait_ge(o["wait"][0], o["wait"][1])
                    elif "dma" in o:
                        out, in_, kw, sem = o["dma"]
                        e.dma_start(out=out, in_=in_, **kw).then_inc(sem, 16)
                    else:
                        nm_, a_, k_ = o["fn"]
                        ins = getattr(e, nm_)(*a_, **k_)
                        if o["inc"]:
                            ins.then_inc(E.sem, 1)

            @block.sync
            def _(e):
                run(self.eng["sp"], e)

            @block.tensor
            def _(e):
                run(self.eng["pe"], e)

            @block.scalar
            def _(e):
                run(self.eng["act"], e)

            @block.vector
            def _(e):
                run(self.eng["dve"], e)

            @block.gpsimd
            def _(e):
                run(self.eng["pool"], e)


def _fm(v, p=128):
    sh = v.shape
    return np.ascontiguousarray(np.swapaxes(v.reshape(sh[:-1] + (sh[-1] // p, p)), -1, -2))


def _na_bias_table(rpb):
    H = rpb.shape[0]
    kc = np.arange(64)[:, None]
    qc = np.arange(64)[None, :]
    qs = np.clip(qc - 8, 0, 48)
    ok = (kc >= qs) & (kc < qs + 16)
    dc = np.clip(kc - qc + 15, 0, 30)
    g = rpb[:, :, dc]
    g = np.where(ok[None, None], g, np.float32(NEG)).astype(np.float32)
    tab = np.empty((2, 64, H, 14, 64), np.float32)
    for dl in range(2):
        tab[dl] = np.transpose(g[:, dl:dl + 14], (2, 0, 1, 3))
    return np.ascontiguousarray(tab.reshape(128, H * 14 * 64))


def _consts(cfg):
    L = cfg.L
    c = {}
    c["ident"] = np.eye(128, dtype=np.float32)
    s = np.arange(64)[:, None]
    t = np.arange(64)[None, :]
    c["maskUL"] = np.concatenate([(s <= t), (s >= t)], axis=1).astype(np.float32)
    kp = np.arange(128)[:, None]
    qp = np.arange(128)[None, :]
    ml = np.where(kp >= qp, 0.0, NEG)
    mr = np.where(kp <= qp, 0.0, NEG)
    c["swam"] = np.concatenate([ml, mr], axis=1).astype(np.float32)
    tok = np.arange(L)
    row = (tok // GRID_W).astype(np.float64)
    col = (tok % GRID_W).astype(np.float64)
    inv = 10000.0 ** (-np.arange(16, dtype=np.float64) / 16)
    C = np.zeros((64, L)); S = np.zeros((64, L))
    for d in range(64):
        pos = row if d < 32 else col
        ang = pos * inv[d % 16]
        C[d] = np.cos(ang)
        S[d] = -np.sin(ang) if (d % 32) < 16 else np.sin(ang)
    c["ropeC"] = np.concatenate([C, C], 0).astype(np.float32)
    c["ropeS"] = np.concatenate([S, S], 0).astype(np.float32)
    P = np.zeros((128, 128), np.float32)
    for do in range(128):
        dd = do % 32
        di = do + 16 if dd < 16 else do - 16
        P[di, do] = 1.0
    c["ropeP"] = P
    o2 = np.zeros((128, 128), np.float32)
    o2[:64, :64] = 1.0
    o2[64:, 64:] = 1.0
    c["ones2"] = o2
    return c


def prep_inputs(cfg, core, x, c, ctx, c_ctx, hg_lower_bounds, ada_w, ada_b, norm1_g, norm2_g, w_in, na_q_norm,
                na_k_norm, na_rpb, hg_norm_g, swa_q_norm, swa_k_norm, swa_sink, w_out, router_w, router_b,
                w_gu, b_gu, w_down, b_down, shared=None):
    nb, dp = cfg.nb, cfg.depth
    f = np.float32
    b0 = core * nb
    m = {}
    m["x"] = np.ascontiguousarray(x[b0:b0 + nb].reshape(nb * cfg.L, D), f)
    m["ctx"] = np.ascontiguousarray(ctx[b0:b0 + nb].reshape(nb * cfg.LC, D), f)
    cc = np.concatenate([c[b0:b0 + nb], c_ctx[None]], 0).astype(f)
    m["ccT"] = np.ascontiguousarray(np.transpose(cc.reshape(nb + 1, 8, 128), (2, 1, 0)).reshape(128, 8 * (nb + 1)))
    if shared is None:
        shared = {}
        shared["ada_w"] = np.ascontiguousarray(ada_w, f)
        shared["ada_bT"] = _fm(ada_b.astype(f))
        shared["ada_b"] = np.ascontiguousarray(ada_b.astype(f).reshape(dp, 1, 6 * D))
        shared["n1T"] = _fm(norm1_g.astype(f))
        shared["n2T"] = _fm(norm2_g.astype(f))
        shared["w_in"] = np.ascontiguousarray(w_in, f)
        shared["w_out"] = np.ascontiguousarray(w_out, f)
        g4 = np.stack([np.tile(na_q_norm, (1, 2)), np.tile(na_k_norm, (1, 2)), np.tile(swa_q_norm, (1, 2)),
                       np.tile(swa_k_norm, (1, 2))], axis=-1)
        shared["gains4"] = np.ascontiguousarray(g4, f)
        shared["nab"] = np.stack([_na_bias_table(na_rpb[l].astype(f)) for l in range(dp)])
        shared["hlbT"] = np.ascontiguousarray(np.transpose(hg_lower_bounds.astype(f).reshape(dp, HG_H, 128), (2, 0, 1)).reshape(128, dp * HG_H))
        shared["hgn"] = np.ascontiguousarray(hg_norm_g.astype(f).reshape(dp, 128, 1))
        shared["sink"] = np.ascontiguousarray(np.broadcast_to(swa_sink.astype(f)[:, None, :], (dp, 128, SW_QH)))
        shared["router_w"] = np.ascontiguousarray(router_w, f)
        shared["router_b"] = np.ascontiguousarray(router_b.astype(f).reshape(dp, 1, cfg.NE))
        shared["w_gu"] = np.ascontiguousarray(w_gu, f)
        shared["b_guT"] = np.ascontiguousarray(_fm(b_gu.astype(f)).transpose(0, 2, 1, 3).reshape(dp, 128, -1))
        shared["w_down"] = np.ascontiguousarray(w_down, f)
        shared["b_down"] = np.ascontiguousarray(b_down, f)
        shared.update(_consts(cfg))
    m.update(shared)
    return m, shared


def build_nc(cfg, dbg=()):
    nc = bass.Bass("TRN2", target_bir_lowering=False)
    nb, L, LC, N, T, NE, DFF, dp, R = cfg.nb, cfg.L, cfg.LC, cfg.N, cfg.T, cfg.NE, cfg.DFF, cfg.depth, cfg.R
    NB1 = nb + 1
    NT = T // 128
    FC = DFF // 128
    ins = {}

    def din(name, shape):
        ins[name] = nc.dram_tensor(name, list(shape), F32, kind="ExternalInput")
        return ins[name]

    x_d = din("x", [nb * L, D]); ctx_d = din("ctx", [nb * LC, D]); ccT_d = din("ccT", [128, 8 * NB1])
    ada_w = din("ada_w", [dp, D, 6 * D]); ada_bT = din("ada_bT", [dp, 128, 48]); ada_b = din("ada_b", [dp, 1, 6 * D])
    n1T_d = din("n1T", [dp, 128, 8]); n2T_d = din("n2T", [dp, 128, 8])
    w_in = din("w_in", [dp, D, IN_COLS]); w_out = din("w_out", [dp, D, D])
    gains4 = din("gains4", [dp, 128, 4]); nab_d = din("nab", [dp, 128, NA_H * 14 * 64])
    hlbT_d = din("hlbT", [128, dp * HG_H]); hgn_d = din("hgn", [dp, 128, 1]); sink_d = din("sink", [dp, 128, SW_QH])
    rw_d = din("router_w", [dp, D, NE]); rb_d = din("router_b", [dp, 1, NE])
    wgu_d = din("w_gu", [dp, NE, D, 2 * DFF]); bguT_d = din("b_guT", [dp, 128, NE * 2 * FC])
    wdn_d = din("w_down", [dp, NE, DFF, D]); bdn_d = din("b_down", [dp, NE, D])
    ident_d = din("ident", [128, 128]); maskUL_d = din("maskUL", [64, 128]); swam_d = din("swam", [128, 256])
    ropeC_d = din("ropeC", [128, L]); ropeS_d = din("ropeS", [128, L]); ropeP_d = din("ropeP", [128, 128])
    ones2_d = din("ones2", [128, 128])
    y_d = nc.dram_tensor("y", [nb * L, D], F32, kind="ExternalOutput")
    dbg_d = {k: nc.dram_tensor("dbg_" + k, list(s), F32, kind="ExternalOutput") for k, s in dbg}

    xres = nc.dram_tensor("xres", [T, D], F32)
    qk_d = nc.dram_tensor("qk_d", [7, 128, T], BF16)
    v_d = nc.dram_tensor("v_d", [T, 384], BF16)
    yT_d = nc.dram_tensor("yT_d", [D, T], BF16)
    h2T_d = nc.dram_tensor("h2T_d", [128, 8, T], BF16)
    Gt_d = nc.dram_tensor("Gt_d", [NB1 * 2, 128, D], F32)

    with ExitStack() as es:
        pg = Prog(nc, es)

        uid = [0]

        def sb(name, shape, dt=F32, stack=es):
            uid[0] += 1
            return stack.enter_context(nc.sbuf_tensor("s%d_%s" % (uid[0], name), list(shape), dt))

        def ps(name, shape, dt=F32, stack=es):
            uid[0] += 1
            return stack.enter_context(nc.psum_tensor("p%d_%s" % (uid[0], name), list(shape), dt))

        RS = pg.res
        r_hT = RS("hT")
        tok_d = nc.dram_tensor("bar_tok", [2, 64], F32); r_bar = RS("bar")
        identf = sb("identf", [128, 128]); identb = sb("identb", [128, 128], BF16)
        ones2 = sb("ones2b", [128, 128], BF16); onesb = sb("onesb", [128, 128], BF16); onesf = sb("onesf", [128, 128])
        zerosf = sb("zerosf", [128, 512])
        csf = sb("csf", [128, 8 * NB1]); csb = sb("csb", [128, 8 * NB1], BF16)
        csrep = sb("csrep", [128, 8 * NB1 * 128], BF16)
        MV = sb("MV", [128, 4 * 8 * NB1])
        stage = sb("stage", [128, 128])
        r_const = RS("const"); r_cs = RS("cs"); r_MV = RS("MV"); r_stage = RS("stage")
        psT = ps("psT", [128, 1024]); r_psT = RS("psT")
        pb = [ps("pb%d" % i, [128, 512]) for i in range(5)]; r_pb = [RS("pb%d" % i) for i in range(5)]
        pbf = ps("pbf", [128, 1024], BF16); r_pbf = RS("pbf")
        r_xres = [RS("xres%d" % i) for i in range(NT)]
        r_y = RS("y"); r_qk = RS("qk_d"); r_v = RS("v_d"); r_yT = RS("yT_d"); r_h2 = RS("h2T_d"); r_Gt = RS("Gt_d")
        r_dbg = {k: RS("dbg" + k) for k in dbg_d}
        rot = {"pb": 0}

        def nextpb():
            i = rot["pb"]; rot["pb"] = (i + 1) % 5
            return pb[i], r_pb[i]

        def tile_info(i):
            g0 = i * 128; b = g0 // N; pos0 = g0 - b * N
            return b, pos0, pos0 < LC

        pg.dma("sp", identf[:, :], ident_d[:, :], W=[r_const])
        pg.dma("sp", stage[:, :], ones2_d[:, :], W=[r_stage])
        pg.op("dve", lambda e: e.tensor_copy(identb[:, :], identf[:, :]), R=[r_const], W=[r_const])
        pg.op("dve", lambda e: e.tensor_copy(ones2[:, :], stage[:, :]), R=[r_stage], W=[r_const])
        pg.op("dve", lambda e: e.memset(onesb[:, :], 1.0), W=[r_const])
        pg.op("dve", lambda e: e.memset(onesf[:, :], 1.0), W=[r_const])
        pg.op("dve", lambda e: e.memset(zerosf[:, :], 0.0), W=[r_const])
        pg.dma("sp", csf[:, :], ccT_d[:, :], W=[r_cs])
        pg.op("act", lambda e: e.activation(csf[:, :], csf[:, :], AF.Silu), R=[r_cs], W=[r_cs])
        pg.op("dve", lambda e: e.tensor_copy(csb[:, :], csf[:, :]), R=[r_cs], W=[r_cs])
        for kc in range(8):
            for b in range(NB1):
                c0 = (kc * NB1 + b)
                pg.op("dve", lambda e, c0=c0: e.tensor_scalar(csrep[:, c0 * 128:(c0 + 1) * 128], zerosf[:, 0:128],
                                                              csf[:, c0:c0 + 1], None, ALU.add), R=[r_cs, r_const], W=[r_cs])
        r_init = RS("init")
        for b in range(nb):
            for (src, s0, n, p0) in ((ctx_d, b * LC, LC, 0), (x_d, b * L, L, LC)):
                for j in range(n // 128):
                    gi = (b * N + p0) // 128 + j
                    pg.dma("sp", xres[gi * 128:(gi + 1) * 128, :], src[s0 + j * 128:s0 + (j + 1) * 128, :], W=[r_init])

        pg.barrier((tok_d[0:1, :], ident_d[0:1, 0:64]), r_bar)

        def dbg_out(key, ap, res_list, rows=128):
            if key in dbg_d:
                pg.dma("sp", dbg_d[key][0:rows, :], ap, R=res_list, W=[r_dbg[key]])

        def phaseA(l):
            with ExitStack() as st:
                wts = [sb("adaw%d" % i, [128, 8 * 1024], BF16, st) for i in range(2)]
                r_w = [RS("adaw%d" % i) for i in range(2)]
                abT = sb("abT", [128, 48], F32, st); abrow = sb("abrow", [1, 6 * D], F32, st); abrowb = sb("abrowb", [1, 6 * D], BF16, st)
                nT = sb("nT", [128, 16], F32, st); modT = sb("modT", [128, 4 * 8 * NB1], F32, st)
                gt = [sb("gt%d" % i, [128, 512], F32, st) for i in range(2)]; r_gt = [RS("gt%d" % i) for i in range(2)]
                r_ab = RS("ab"); r_mod = RS("modT")
                pg.dma("sp", abT[:, :], ada_bT[l, :, :], W=[r_ab])
                pg.dma("sp", abrow[:, :], ada_b[l, :, :], W=[r_ab])
                pg.dma("sp", nT[:, 0:8], n1T_d[l, :, :], W=[r_ab])
                pg.dma("sp", nT[:, 8:16], n2T_d[l, :, :], W=[r_ab])
                pg.op("act", lambda e: e.activation(abrowb[:, :], abrow[:, :], AF.Copy), R=[r_ab], W=[r_ab])
                gi = 0
                for blk in range(6):
                    wt, rw = wts[blk % 2], r_w[blk % 2]
                    for kc in range(8):
                        pg.dma("pool", wt[:, kc * 1024:(kc + 1) * 1024], ada_w[l, kc * 128:(kc + 1) * 128, blk * 1024:(blk + 1) * 1024], W=[rw])
                    if blk in (0, 1, 3, 4):
                        fm = {0: 0, 1: 1, 3: 2, 4: 3}[blk]
                        p, rp = nextpb()
                        for j in range(8):
                            for kc in range(8):
                                pg.op("pe", lambda e, p=p, j=j, kc=kc, wt=wt: e.matmul(
                                    p[:, j * NB1:(j + 1) * NB1], wt[:, kc * 1024 + j * 128: kc * 1024 + (j + 1) * 128],
                                    csb[:, kc * NB1:(kc + 1) * NB1], start=(kc == 0), stop=(kc == 7)), R=[rw, r_cs], W=[rp])
                        for j in range(8):
                            pg.op("dve", lambda e, p=p, j=j, fm=fm, blk=blk: e.tensor_scalar(
                                modT[:, (fm * 8 + j) * NB1:(fm * 8 + j + 1) * NB1], p[:, j * NB1:(j + 1) * NB1],
                                abT[:, blk * 8 + j: blk * 8 + j + 1], None, ALU.add), R=[rp, r_ab], W=[r_mod])
                    else:
                        which = 0 if blk == 2 else 1
                        for b in range(NB1):
                            for half in range(2):
                                p, rp = nextpb()
                                for kc in range(8):
                                    c0 = kc * NB1 + b
                                    pg.op("pe", lambda e, p=p, kc=kc, c0=c0, half=half, wt=wt: e.matmul(
                                        p[:, :], csrep[:, c0 * 128:(c0 + 1) * 128], wt[:, kc * 1024 + half * 512: kc * 1024 + (half + 1) * 512],
                                        start=(kc == 0), stop=False), R=[rw, r_cs], W=[rp])
                                cb = blk * 1024 + half * 512
                                pg.op("pe", lambda e, p=p, cb=cb: e.matmul(p[:, :], onesb[0:1, :], abrowb[0:1, cb:cb + 512], start=False, stop=True),
                                      R=[r_ab, r_const], W=[rp])
                                g, rg = gt[gi % 2], r_gt[gi % 2]; gi += 1
                                pg.op("act", lambda e, g=g, p=p: e.activation(g[:, :], p[:, :], AF.Copy), R=[rp], W=[rg])
                                pg.dma("sp", Gt_d[b * 2 + which, :, half * 512:(half + 1) * 512], g[:, :], R=[rg], W=[r_Gt])
                for j in range(8):
                    for (w, scf, shf, nofs) in ((0, 1, 0, 0), (2, 3, 2, 8)):
                        pg.op("dve", lambda e, j=j, w=w, scf=scf, nofs=nofs: e.tensor_scalar(
                            MV[:, (w * 8 + j) * NB1:(w * 8 + j + 1) * NB1], modT[:, (scf * 8 + j) * NB1:(scf * 8 + j + 1) * NB1],
                            1.0, nT[:, nofs + j:nofs + j + 1], ALU.add, ALU.mult), R=[r_mod, r_ab], W=[r_MV])
                        pg.op("dve", lambda e, j=j, w=w, shf=shf: e.tensor_copy(
                            MV[:, ((w + 1) * 8 + j) * NB1:((w + 1) * 8 + j + 1) * NB1], modT[:, (shf * 8 + j) * NB1:(shf * 8 + j + 1) * NB1]),
                            R=[r_mod], W=[r_MV])

        nm = {}

        def norm_setup(st):
            nm["junk"] = sb("nm_junk", [128, 1024], BF16, st); nm["ss"] = [sb("nm_ss%d" % i, [128, 2], F32, st) for i in range(2)]
            nm["xn"] = [sb("nm_xn%d" % i, [128, 1024], F32, st) for i in range(2)]
            nm["r_junk"] = RS("nm_junk"); nm["r_ss"] = [RS("nm_ss%d" % i) for i in range(2)]; nm["r_xn"] = [RS("nm_xn%d" % i) for i in range(2)]
            nm["k"] = 0

        def norm_mod_T(i, xt, r_xt, w, dstT=None, r_dst=None, keep_xnT=None):
            b, pos0, isc = tile_info(i)
            bm = nb if isc else b
            k = nm["k"]; nm["k"] = 1 - k
            ss, r_ss, xn, r_xn = nm["ss"][k], nm["r_ss"][k], nm["xn"][k], nm["r_xn"][k]
            pg.op("act", lambda e: e.activation(nm["junk"][:, :], xt[:, :], AF.Square, accum_out=ss[:, 0:1]), R=[r_xt], W=[nm["r_junk"], r_ss])
            pg.op("act", lambda e: e.activation(ss[:, 1:2], ss[:, 0:1], AF.Ln, scale=1.0 / D, bias=EPS), R=[r_ss], W=[r_ss])
            pg.op("act", lambda e: e.activation(ss[:, 1:2], ss[:, 1:2], AF.Exp, scale=-0.5), R=[r_ss], W=[r_ss])
            pg.op("dve", lambda e: e.tensor_scalar(xn[:, :], xt[:, :], ss[:, 1:2], None, ALU.mult), R=[r_xt, r_ss], W=[r_xn])
            for j in range(8):
                pg.op("pe", lambda e, j=j: e.transpose(psT[:, j * 128:(j + 1) * 128], xn[:, j * 128:(j + 1) * 128], identf[:, :]),
                      R=[r_xn, r_const], W=[r_psT])
            for j in range(8):
                ca = (w * 8 + j) * NB1 + bm; cbb = ((w + 1) * 8 + j) * NB1 + bm
                eng = "act" if j % 2 == 0 else "dve"
                dst = hT[:, j * T + i * 128: j * T + (i + 1) * 128]
                if eng == "act":
                    pg.op("act", lambda e, j=j, ca=ca, cbb=cbb, dst=dst: e.activation(dst, psT[:, j * 128:(j + 1) * 128], AF.Identity,
                                                                                    bias=MV[:, cbb:cbb + 1], scale=MV[:, ca:ca + 1]),
                          R=[r_psT, r_MV], W=[r_hT])
                else:
                    pg.op("dve", lambda e, j=j, ca=ca, cbb=cbb, dst=dst: e.tensor_scalar(dst, psT[:, j * 128:(j + 1) * 128], MV[:, ca:ca + 1],
                                                                                       MV[:, cbb:cbb + 1], ALU.mult, ALU.add),
                          R=[r_psT, r_MV], W=[r_hT])
            if keep_xnT is not None:
                kx, r_kx = keep_xnT
                pg.op("act", lambda e: e.activation(kx[:, :], psT[:, :], AF.Copy), R=[r_psT], W=[r_kx])

        def phaseB(l):
            with ExitStack() as st:
                norm_setup(st)
                xts = [sb("xt%d" % i, [128, 1024], F32, st) for i in range(2)]; r_xts = [RS("xt%d" % i) for i in range(2)]
                for i in range(NT):
                    xt, r_xt = xts[i % 2], r_xts[i % 2]
                    pg.dma("sp", xt[:, :], xres[i * 128:(i + 1) * 128, :], R=[r_xres[i]], W=[r_xt])
                    norm_mod_T(i, xt, r_xt, 0)

        def tok_chunks(maxn=512):
            out = []
            for b in range(nb):
                for (p0, n, isc) in ((0, LC, True), (LC, L, False)):
                    o = 0
                    while o < n:
                        m = min(maxn, n - o)
                        out.append((b, b * N + p0 + o, m, isc, o))
                        o += m
            return out

        SLOTS = [((C_NAQ, C_NAQ + 64), 0, False), ((C_NAQ + 128, C_NAQ + 192), 0, False),
                 ((C_NAK, C_NAK + 64), 1, False), ((C_NAK + 128, C_NAK + 192), 1, False),
                 ((C_SWQ, C_SWQ + 128), 2, True), ((C_SWQ + 64, C_SWQ + 192), 2, True),
                 ((C_SWK, C_SWK + 64), 3, True)]

        def phaseC(l):
            with ExitStack() as st:
                wqk = sb("wqk", [128, 8, 7 * 128], BF16, st); wv = sb("wv", [128, 8, 384], BF16, st)
                gt4 = sb("gt4", [128, 4], F32, st); r_w = RS("wqk"); r_g = RS("gt4")
                rC = sb("ropeC", [128, L], F32, st); rS = sb("ropeS", [128, L], F32, st); rP = sb("ropeP", [128, 128], BF16, st); r_rope = RS("rope")
                sq = [sb("c_sq%d" % i, [128, 512], BF16, st) for i in range(2)]; r_sq = [RS("c_sq%d" % i) for i in range(2)]
                rstd = [sb("c_rstd%d" % i, [128, 512], F32, st) for i in range(2)]; r_rstd = [RS("c_rstd%d" % i) for i in range(2)]
                qn = [sb("c_qn%d" % i, [128, 512], BF16, st) for i in range(2)]; r_qn = [RS("c_qn%d" % i) for i in range(2)]
                t1 = [sb("c_t1%d" % i, [128, 512], F32, st) for i in range(2)]; r_t1 = [RS("c_t1%d" % i) for i in range(2)]
                t2 = [sb("c_t2%d" % i, [128, 512], F32, st) for i in range(2)]; r_t2 = [RS("c_t2%d" % i) for i in range(2)]
                qo = [sb("c_qo%d" % i, [128, 512], BF16, st) for i in range(2)]; r_qo = [RS("c_qo%d" % i) for i in range(2)]
                vs = [sb("c_vs%d" % i, [128, 384], BF16, st) for i in range(2)]; r_vs = [RS("c_vs%d" % i) for i in range(2)]
                for s, (cols, gcol, rope) in enumerate(SLOTS):
                    for g, c0 in enumerate(cols):
                        pg.dma("pool", wqk[:, :, s * 128 + g * 64: s * 128 + (g + 1) * 64],
                               w_in[l, :, c0:c0 + 64].rearrange("(kc p) n -> p kc n", p=128), W=[r_w])
                for (c0, n, o) in ((C_NAV, 256, 0), (C_SWV, 128, 256)):
                    pg.dma("pool", wv[:, :, o:o + n], w_in[l, :, c0:c0 + n].rearrange("(kc p) n -> p kc n", p=128), W=[r_w])
                pg.dma("sp", gt4[:, :], gains4[l, :, :], W=[r_g])
                for c in (0, 2):
                    pg.op("dve", lambda e, c=c: e.tensor_scalar(gt4[:, c:c + 1], gt4[:, c:c + 1], HD ** -0.5, None, ALU.mult), R=[r_g], W=[r_g])
                pg.dma("sp", rC[:, :], ropeC_d[:, :], W=[r_rope]); pg.dma("sp", rS[:, :], ropeS_d[:, :], W=[r_rope])
                pg.dma("sp", stage[:, :], ropeP_d[:, :], W=[r_stage])
                pg.op("dve", lambda e: e.tensor_copy(rP[:, :], stage[:, :]), R=[r_stage], W=[r_rope])
                it = 0
                for (b, g0, n, isc, o) in tok_chunks():
                    for s, (cols, gcol, rope) in enumerate(SLOTS):
                        k = it % 2; it += 1
                        p, rp = nextpb()
                        for kc in range(8):
                            pg.op("pe", lambda e, p=p, kc=kc, s=s, g0=g0, n=n: e.matmul(
                                p[:, 0:n], wqk[:, kc, s * 128:(s + 1) * 128], hT[:, kc * T + g0: kc * T + g0 + n], start=(kc == 0), stop=(kc == 7)),
                                R=[r_w, r_hT], W=[rp])
                        pg.op("act", lambda e, p=p, k=k, n=n: e.activation(sq[k][:, 0:n], p[:, 0:n], AF.Square), R=[rp], W=[r_sq[k]])
                        p2, rp2 = nextpb()
                        pg.op("pe", lambda e, p2=p2, k=k, n=n: e.matmul(p2[:, 0:n], ones2[:, :], sq[k][:, 0:n], start=True, stop=True),
                              R=[r_sq[k], r_const], W=[rp2])
                        pg.op("act", lambda e, p2=p2, k=k, n=n: e.activation(rstd[k][:, 0:n], p2[:, 0:n], AF.Ln, scale=1.0 / HD, bias=EPS), R=[rp2], W=[r_rstd[k]])
                        pg.op("act", lambda e, k=k, n=n: e.activation(rstd[k][:, 0:n], rstd[k][:, 0:n], AF.Exp, scale=-0.5), R=[r_rstd[k]], W=[r_rstd[k]])
                        dorope = rope and not isc
                        dst, r_dst = (qn[k], r_qn[k]) if dorope else (qo[k], r_qo[k])
                        pg.op("dve", lambda e, p=p, k=k, n=n, dst=dst, gcol=gcol: e.scalar_tensor_tensor(
                            dst[:, 0:n], p[:, 0:n], gt4[:, gcol:gcol + 1], rstd[k][:, 0:n], ALU.mult, ALU.mult), R=[rp, r_g, r_rstd[k]], W=[r_dst])
                        if dorope:
                            p3, rp3 = nextpb()
                            pg.op("pe", lambda e, p3=p3, k=k, n=n: e.matmul(p3[:, 0:n], rP[:, :], qn[k][:, 0:n], start=True, stop=True),
                                  R=[r_qn[k], r_rope], W=[rp3])
                            pg.op("dve", lambda e, k=k, n=n, o=o: e.tensor_tensor(t1[k][:, 0:n], qn[k][:, 0:n], rC[:, o:o + n], ALU.mult),
                                  R=[r_qn[k], r_rope], W=[r_t1[k]])
                            pg.op("dve", lambda e, p3=p3, k=k, n=n, o=o: e.tensor_tensor(t2[k][:, 0:n], p3[:, 0:n], rS[:, o:o + n], ALU.mult),
                                  R=[rp3, r_rope], W=[r_t2[k]])
                            pg.op("pool", lambda e, k=k, n=n: e.tensor_tensor(qo[k][:, 0:n], t1[k][:, 0:n], t2[k][:, 0:n], ALU.add),
                                  R=[r_t1[k], r_t2[k]], W=[r_qo[k]])
                        pg.dma("sp", qk_d[s, :, g0:g0 + n], qo[k][:, 0:n], R=[r_qo[k]], W=[r_qk])
                for i in range(NT):
                    k = i % 2
                    p, rp = nextpb()
                    for kc in range(8):
                        pg.op("pe", lambda e, p=p, kc=kc, i=i: e.matmul(p[:, 0:384], hT[:, kc * T + i * 128: kc * T + (i + 1) * 128], wv[:, kc, :],
                                                                    start=(kc == 0), stop=(kc == 7)), R=[r_w, r_hT], W=[rp])
                    pg.op("act", lambda e, p=p, k=k: e.activation(vs[k][:, :], p[:, 0:384], AF.Copy), R=[rp], W=[r_vs[k]])
                    pg.dma("sp", v_d[i * 128:(i + 1) * 128, :], vs[k][:, :], R=[r_vs[k]], W=[r_v])

        at = {}

        def attn_setup(st):
            at["E"] = [sb("at_E%d" % i, [128, 512], BF16, st) for i in range(3)]; at["r_E"] = [RS("at_E%d" % i) for i in range(3)]
            at["rd"] = [sb("at_rd%d" % i, [64, 128], F32, st) for i in range(2)]; at["r_rd"] = [RS("at_rd%d" % i) for i in range(2)]
            at["ke"] = 0; at["kr"] = 0

        def attn_unit(hp, q_ap, keys, nq, dst, r_dst, Rq, sink_ap=None):
            per = 512 // nq
            groups = [keys[i:i + per] for i in range(0, len(keys), per)]
            Es = []
            for grp in groups:
                p, rp = nextpb()
                for t, (k_ap, b_ap, v_ap) in enumerate(grp):
                    pg.op("pe", lambda e, p=p, t=t, k_ap=k_ap, b_ap=b_ap: e.matmul(p[:, t * nq:(t + 1) * nq], k_ap, q_ap, start=True, stop=(b_ap is None)),
                          R=Rq, W=[rp])
                    if b_ap is not None:
                        pg.op("pe", lambda e, p=p, t=t, b_ap=b_ap: e.matmul(p[:, t * nq:(t + 1) * nq], identb[:, :], b_ap, start=False, stop=True),
                              R=Rq + [r_const], W=[rp])
                ke = at["ke"]; at["ke"] = (ke + 1) % 3
                E, rE = at["E"][ke], at["r_E"][ke]
                w = len(grp) * nq
                pg.op("act", lambda e, p=p, E=E, w=w: e.activation(E[:, 0:w], p[:, 0:w], AF.Exp), R=[rp], W=[rE])
                Es.append((E, rE, grp))
            po, rpo = nextpb()
            flat = [(E, rE, t, v_ap) for (E, rE, grp) in Es for t, (_, _, v_ap) in enumerate(grp)]
            for i, (E, rE, t, v_ap) in enumerate(flat):
                pg.op("pe", lambda e, E=E, t=t, i=i: e.matmul(po[0:64, nq:2 * nq], onesb[:, 0:64], E[:, t * nq:(t + 1) * nq], start=(i == 0), stop=(i == len(flat) - 1)),
                      R=[rE, r_const], W=[rpo])
            for i, (E, rE, t, v_ap) in enumerate(flat):
                pg.op("pe", lambda e, E=E, t=t, i=i, v_ap=v_ap: e.matmul(po[0:64, 0:nq], v_ap, E[:, t * nq:(t + 1) * nq], start=(i == 0), stop=(i == len(flat) - 1)),
                      R=[rE] + Rq, W=[rpo])
            kr = at["kr"]; at["kr"] = 1 - kr
            rd, r_rd = at["rd"][kr], at["r_rd"][kr]
            if sink_ap is not None:
                pg.op("dve", lambda e: e.tensor_scalar(rd[:, 0:nq], po[0:64, nq:2 * nq], sink_ap, None, ALU.add), R=[rpo] + Rq, W=[r_rd])
                pg.op("dve", lambda e: e.reciprocal(rd[:, 0:nq], rd[:, 0:nq]), R=[r_rd], W=[r_rd])
            else:
                pg.op("dve", lambda e: e.reciprocal(rd[:, 0:nq], po[0:64, nq:2 * nq]), R=[rpo], W=[r_rd])
            pg.op("dve", lambda e: e.tensor_tensor(dst, po[0:64, 0:nq], rd[:, 0:nq], ALU.mult), R=[rpo, r_rd], W=[r_dst])

        def phaseEF(l, last):
            with ExitStack() as st:
                attn_setup(st)
                QK = sb("QK", [128, 7, N], BF16, st); VE = sb("VE", [128, N // 128, 384], BF16, st); VO = sb("VO", [128, L // 128, 384], BF16, st)
                nabf = sb("nabf", [128, 14 * 64], F32, st); nabt = sb("nabt", [128, NA_H * 14 * 64], BF16, st)
                swmf = sb("swmf", [128, 256], F32, st); swm = sb("swm", [128, 256], BF16, st); esink = sb("esink", [128, SW_QH], F32, st)
                stg = [sb("stg%d" % i, [64, N], BF16, st) for i in range(2)]; r_stg = [RS("stg%d" % i) for i in range(2)]
                r_in = RS("attn_in"); r_tab = RS("attn_tab"); r_nabf = RS("nabf")
                for h in range(NA_H):
                    pg.dma("sp", nabf[:, :], nab_d[l, :, h * 896:(h + 1) * 896], W=[r_nabf])
                    pg.op("act", lambda e, h=h: e.activation(nabt[:, h * 896:(h + 1) * 896], nabf[:, :], AF.Copy), R=[r_nabf], W=[r_tab])
                pg.dma("sp", swmf[:, :], swam_d[:, :], W=[r_nabf])
                pg.op("act", lambda e: e.activation(swm[:, :], swmf[:, :], AF.Copy), R=[r_nabf], W=[r_tab])
                pg.dma("sp", esink[:, :], sink_d[l, :, :], W=[r_tab])
                pg.op("act", lambda e: e.activation(esink[:, :], esink[:, :], AF.Exp), R=[r_tab], W=[r_tab])
                Rq = [r_in, r_tab]
                ks = 0
                LT = LC // 128
                for b in range(nb):
                    g0 = b * N
                    for s in range(7):
                        pg.dma("sp", QK[:, s, :], qk_d[s, :, g0:g0 + N], R=[r_qk], W=[r_in])
                    pg.dma("sp", VE[:, :, :], v_d[g0:g0 + N, :].rearrange("(t p) c -> p t c", p=128), R=[r_v], W=[r_in])
                    pg.dma("sp", VO[:, 0:L // 128 - 1, :], v_d[g0 + LC + 64:g0 + LC + 64 + L - 128, :].rearrange("(t p) c -> p t c", p=128), R=[r_v], W=[r_in])
                    for h in range(NA_H):
                        sq_, sk_, hp = h // 2, 2 + h // 2, h % 2
                        ps0 = hp * 64
                        sg, r_sg = stg[ks % 2], r_stg[ks % 2]; ks += 1
                        ckeys = [(QK[ps0:ps0 + 64, sk_, t * 128:(t + 1) * 128], None, VE[:, t, h * 64:(h + 1) * 64]) for t in range(LT)]
                        if not last:
                            for qt in range(LT):
                                attn_unit(hp, QK[ps0:ps0 + 64, sq_, qt * 128:(qt + 1) * 128], ckeys, 128, sg[:, qt * 128:(qt + 1) * 128], r_sg, Rq)
                        for r in range(R):
                            s0 = min(max(r - 4, 0), R - 8)
                            keys = []
                            for t in range(4):
                                kr_ = s0 + 2 * t
                                pos = LC + 64 * kr_
                                d0 = (s0 - r + 7) + 2 * t
                                v_ap = VE[:, LT + kr_ // 2, h * 64:(h + 1) * 64] if kr_ % 2 == 0 else VO[:, (kr_ - 1) // 2, h * 64:(h + 1) * 64]
                                keys.append((QK[ps0:ps0 + 64, sk_, pos:pos + 128], nabt[:, (h * 14 + d0) * 64:(h * 14 + d0 + 1) * 64], v_ap))
                            attn_unit(hp, QK[ps0:ps0 + 64, sq_, LC + 64 * r: LC + 64 * (r + 1)], keys + ckeys, 64,
                                      sg[:, LC + 64 * r: LC + 64 * (r + 1)], r_sg, Rq)
                        c0 = 0 if not last else LC
                        pg.dma("sp", yT_d[h * 64:(h + 1) * 64, g0 + c0:g0 + N], sg[:, c0:N], R=[r_sg], W=[r_yT])
                    for hq in range(SW_QH):
                        sq_, half, kv = 4 + hq % 2, hq // 2, hq // 2
                        ps0 = half * 64
                        vc0 = 256 + kv * 64
                        sg, r_sg = stg[ks % 2], r_stg[ks % 2]; ks += 1
                        ckeys = [(QK[ps0:ps0 + 64, 6, t * 128:(t + 1) * 128], None, VE[:, t, vc0:vc0 + 64]) for t in range(LT)]
                        sk_ap = esink[0:64, hq:hq + 1]
                        if not last:
                            for qt in range(LT):
                                attn_unit(half, QK[ps0:ps0 + 64, sq_, qt * 128:(qt + 1) * 128], ckeys, 128, sg[:, qt * 128:(qt + 1) * 128], r_sg, Rq, sk_ap)
                        nblk = L // 128
                        for n in range(nblk):
                            keys = []
                            for (bk, m0) in ((n - 1, 0), (n, None), (n + 1, 128)):
                                if 0 <= bk < nblk:
                                    pos = LC + 128 * bk
                                    keys.append((QK[ps0:ps0 + 64, 6, pos:pos + 128], None if m0 is None else swm[:, m0:m0 + 128], VE[:, LT + bk, vc0:vc0 + 64]))
                            attn_unit(half, QK[ps0:ps0 + 64, sq_, LC + 128 * n: LC + 128 * (n + 1)], keys + ckeys, 128,
                                      sg[:, LC + 128 * n: LC + 128 * (n + 1)], r_sg, Rq, sk_ap)
                        c0 = 0 if not last else LC
                        pg.dma("sp", yT_d[768 + hq * 64:768 + (hq + 1) * 64, g0 + c0:g0 + N], sg[:, c0:N], R=[r_sg], W=[r_yT])

        def phaseG(l, last):
            CH = 16
            NCH = N // CH
            LCH = LC // CH
            with ExitStack() as st:
                whg = [sb("whg%d" % i, [128, 8, 640], BF16, st) for i in range(2)]; r_whg = [RS("whg%d" % i) for i in range(2)]
                qs = sb("g_qs", [128, N], F32, st); kd = [sb("g_k%d" % i, [128, N], F32, st) for i in range(2)]
                Pd = [sb("g_P%d" % i, [128, N + 1], F32, st) for i in range(2)]; nPd = [sb("g_nP%d" % i, [128, NCH + 1], F32, st) for i in range(2)]
                lf = sb("g_lf", [128, N], F32, st); sgate = sb("g_sg", [128, N], F32, st); oacc = sb("g_oacc", [128, N], F32, st)
                vch = [sb("g_vch%d" % i, [CH, 128], BF16, st) for i in range(2)]; r_vch = [RS("g_vch%d" % i) for i in range(2)]
                mUL = sb("g_mUL", [64, 128], F32, st); hlb = sb("g_hlb", [128, dp * HG_H], F32, st); lbt = sb("g_lbt", [128, 2 * HG_H], F32, st)
                gn = sb("g_gn", [128, 1], F32, st)
                tmpa = [sb("g_ta%d" % i, [128, 512], F32, st) for i in range(2)]; r_tmpa = [RS("g_ta%d" % i) for i in range(2)]
                tmpb = [sb("g_tb%d" % i, [128, 512], F32, st) for i in range(2)]; r_tmpb = [RS("g_tb%d" % i) for i in range(2)]
                sqb = [sb("g_sqb%d" % i, [128, 512], BF16, st) for i in range(2)]; r_sqb = [RS("g_sqb%d" % i) for i in range(2)]
                yo = [sb("g_yo%d" % i, [128, 512], BF16, st) for i in range(2)]; r_yo = [RS("g_yo%d" % i) for i in range(2)]
                ex = [[sb("g_e%d%d" % (j, i), [128, CH], F32, st) for i in range(2)] for j in range(3)]
                r_ex = [[RS("g_e%d%d" % (j, i)) for i in range(2)] for j in range(3)]
                qt = [sb("g_qt%d" % i, [128, CH], BF16, st) for i in range(2)]; r_qt = [RS("g_qt%d" % i) for i in range(2)]
                kt = [sb("g_kt%d" % i, [128, CH], BF16, st) for i in range(2)]; r_kt = [RS("g_kt%d" % i) for i in range(2)]
                kh = [sb("g_kh%d" % i, [128, CH], BF16, st) for i in range(2)]; r_kh = [RS("g_kh%d" % i) for i in range(2)]
                Am = [sb("g_Am%d" % i, [CH, CH], BF16, st) for i in range(2)]; r_Am = [RS("g_Am%d" % i) for i in range(2)]
                khT = [sb("g_khT%d" % i, [CH, 128], BF16, st) for i in range(2)]; r_khT = [RS("g_khT%d" % i) for i in range(2)]
                Sf = [sb("g_S%d" % i, [128, 128], F32, st) for i in range(2)]; r_Sf = [RS("g_S%d" % i) for i in range(2)]
                Sb_ = [sb("g_Sb%d" % i, [128, 128], BF16, st) for i in range(2)]; r_Sb = [RS("g_Sb%d" % i) for i in range(2)]
                r_qs = RS("g_qs"); r_k = [RS("g_k0"), RS("g_k1")]; r_P = [RS("g_P0"), RS("g_P1")]; r_lf = RS("g_lf"); r_sgt = RS("g_sg")
                r_oacc = RS("g_oacc"); r_c = RS("g_const")
                pg.dma("sp", mUL[:, :], maskUL_d[:, :], W=[r_c])
                pg.dma("sp", hlb[:, :], hlbT_d[:, :], W=[r_c])
                pg.dma("sp", gn[:, :], hgn_d[l, :, :], W=[r_c])
                if l == 0:
                    pg.op("dve", lambda e: e.memset(lbt[:, 0:HG_H], 0.0), R=[r_c], W=[r_c])
                else:
                    pg.op("dve", lambda e: e.tensor_tensor(lbt[:, 0:HG_H], hlb[:, l * HG_H:(l + 1) * HG_H], hlb[:, 0:HG_H], ALU.subtract), R=[r_c], W=[r_c])
                    pg.op("act", lambda e: e.activation(lbt[:, 0:HG_H], lbt[:, 0:HG_H], AF.Sigmoid), R=[r_c], W=[r_c])
                pg.op("dve", lambda e: e.tensor_scalar(lbt[:, HG_H:2 * HG_H], lbt[:, 0:HG_H], -1.0, 1.0, ALU.mult, ALU.add), R=[r_c], W=[r_c])
                it = {"a": 0, "c": 0}
                for h in range(HG_H):
                    wt, rw = whg[h % 2], r_whg[h % 2]
                    for j, c0 in enumerate((C_HGQ, C_HGFF, C_HGFB, C_HGI, C_HGG)):
                        pg.dma("pool", wt[:, :, j * 128:(j + 1) * 128], w_in[l, :, c0 + h * 128:c0 + (h + 1) * 128].rearrange("(kc p) n -> p kc n", p=128), W=[rw])
                    for b in range(nb):
                        g0 = b * N
                        chunks = [(o, min(512, N - o)) for o in range(0, N, 512)]

                        def proj(j, o, n):
                            p, rp = nextpb()
                            for kc in range(8):
                                pg.op("pe", lambda e, p=p, kc=kc: e.matmul(p[:, 0:n], wt[:, kc, j * 128:(j + 1) * 128], hT[:, kc * T + g0 + o: kc * T + g0 + o + n],
                                                                         start=(kc == 0), stop=(kc == 7)), R=[rw, r_hT], W=[rp])
                            return p, rp
                        for (o, n) in chunks:
                            p, rp = proj(0, o, n)
                            pg.op("act", lambda e, p=p, o=o, n=n: e.activation(qs[:, o:o + n], p[:, 0:n], AF.Silu), R=[rp], W=[r_qs])
                            p, rp = proj(4, o, n)
                            pg.op("act", lambda e, p=p, o=o, n=n: e.activation(sgate[:, o:o + n], p[:, 0:n], AF.Silu), R=[rp], W=[r_sgt])
                        for dirn in range(2):
                            kk, rk, P, rP, nP = kd[dirn], r_k[dirn], Pd[dirn], r_P[dirn], nPd[dirn]
                            for (o, n) in chunks:
                                p, rp = proj(1 + dirn, o, n)
                                a = it["a"] % 2; it["a"] += 1
                                pg.op("act", lambda e, p=p, a=a, n=n: e.activation(tmpa[a][:, 0:n], p[:, 0:n], AF.Sigmoid), R=[rp], W=[r_tmpa[a]])
                                pg.op("dve", lambda e, a=a, n=n: e.tensor_scalar(tmpb[a][:, 0:n], tmpa[a][:, 0:n], lbt[:, HG_H + h:HG_H + h + 1], lbt[:, h:h + 1], ALU.mult, ALU.add),
                                      R=[r_tmpa[a], r_c], W=[r_tmpb[a]])
                                pg.op("act", lambda e, a=a, o=o, n=n: e.activation(lf[:, o:o + n], tmpb[a][:, 0:n], AF.Ln), R=[r_tmpb[a]], W=[r_lf])
                                pg.op("dve", lambda e, a=a, o=o, n=n, kk=kk: e.tensor_scalar(kk[:, o:o + n], tmpb[a][:, 0:n], -1.0, 1.0, ALU.mult, ALU.add),
                                      R=[r_tmpb[a]], W=[rk])
                            pg.op("dve", lambda e, P=P: e.memset(P[:, 0:1], 0.0), W=[rP])
                            pg.op("dve", lambda e, P=P: e.tensor_tensor_scan(P[:, 1:N + 1], lf[:, :], lf[:, :], 0.0, ALU.add, ALU.bypass), R=[r_lf, r_c], W=[rP])
                            pg.op("dve", lambda e, P=P, nP=nP: e.tensor_scalar(nP[:, :], P[:, 0:N + 1:CH], -1.0, None, ALU.mult), R=[rP], W=[rP])
                        for dirn in range(2):
                            kk, rk, P, rP, nP = kd[dirn], r_k[dirn], Pd[dirn], r_P[dirn], nPd[dirn]
                            order = list(range(NCH)) if dirn == 0 else (list(range(LCH - 1, -1, -1)) + list(range(NCH - 1, LCH - 1, -1)))
                            sc = 0
                            pg.op("dve", lambda e: e.memset(Sf[0][:, :], 0.0), W=[r_Sf[0]])
                            pg.op("dve", lambda e: e.memset(Sb_[0][:, :], 0.0), W=[r_Sb[0]])
                            for c in order:
                                a0 = c * CH
                                z = it["c"] % 2; it["c"] += 1
                                need_out = not (last and c < LCH)
                                pV, rpV = nextpb()
                                for kc in range(8):
                                    pg.op("pe", lambda e: e.matmul(pV[0:CH, 0:128], hT[:, kc * T + g0 + a0: kc * T + g0 + a0 + CH], wt[:, kc, 384:512], start=(kc == 0), stop=(kc == 7)),
                                          R=[rw, r_hT], W=[rpV])
                                pg.op("act", lambda e: e.activation(vch[z][:, :], pV[0:CH, 0:128], AF.Copy), R=[rpV], W=[r_vch[z]])
                                if dirn == 0:
                                    src = P[:, a0 + 1:a0 + CH + 1]
                                    specs = ((1.0, nP[:, c:c + 1]), (-1.0, P[:, a0:a0 + 1]), (-1.0, P[:, a0 + CH:a0 + CH + 1]))
                                    ebe = ex[0][z][:, CH - 1:CH]; mk = mUL[0:CH, 0:CH]
                                else:
                                    src = P[:, a0:a0 + CH]
                                    specs = ((-1.0, P[:, a0 + CH:a0 + CH + 1]), (1.0, nP[:, c + 1:c + 2]), (1.0, nP[:, c:c + 1]))
                                    ebe = ex[0][z][:, 0:1]; mk = mUL[0:CH, 64:64 + CH]
                                for j, (scl, bias) in enumerate(specs):
                                    pg.op("act", lambda e, j=j, z=z, src=src, scl=scl, bias=bias: e.activation(ex[j][z][:, :], src, AF.Exp, bias=bias, scale=scl),
                                          R=[rP], W=[r_ex[j][z]])
                                pg.op("dve", lambda e, z=z, a0=a0: e.scalar_tensor_tensor(qt[z][:, :], qs[:, a0:a0 + CH], HG_DK ** -0.5, ex[0][z][:, :], ALU.mult, ALU.mult),
                                      R=[r_qs, r_ex[0][z]], W=[r_qt[z]])
                                pg.op("dve", lambda e, z=z, a0=a0, kk=kk: e.tensor_tensor(kh[z][:, :], kk[:, a0:a0 + CH], ex[2][z][:, :], ALU.mult),
                                      R=[rk, r_ex[2][z]], W=[r_kh[z]])
                                if need_out:
                                    pg.op("dve", lambda e, z=z, a0=a0, kk=kk: e.tensor_tensor(kt[z][:, :], kk[:, a0:a0 + CH], ex[1][z][:, :], ALU.mult),
                                          R=[rk, r_ex[1][z]], W=[r_kt[z]])
                                    pA, rpA = nextpb()
                                    pg.op("pe", lambda e, pA=pA, z=z: e.matmul(pA[0:CH, 0:CH], kt[z][:, :], qt[z][:, :], start=True, stop=True), R=[r_kt[z], r_qt[z]], W=[rpA])
                                    pg.op("dve", lambda e, pA=pA, z=z, mk=mk: e.tensor_tensor(Am[z][:, :], pA[0:CH, 0:CH], mk, ALU.mult), R=[rpA, r_c], W=[r_Am[z]])
                                pg.op("pe", lambda e, z=z: e.transpose(pbf[0:CH, 0:128], kh[z][:, :], identb[:, :]), R=[r_kh[z], r_const], W=[r_pbf])
                                pg.op("act", lambda e, z=z: e.activation(khT[z][:, :], pbf[0:CH, 0:128], AF.Copy), R=[r_pbf], W=[r_khT[z]])
                                if need_out:
                                    pO, rpO = nextpb()
                                    pg.op("pe", lambda e, pO=pO, z=z, c=c: e.matmul(pO[:, 0:CH], vch[z][:, :], Am[z][:, :], start=True, stop=False), R=[r_vch[z], r_Am[z]], W=[rpO])
                                    pg.op("pe", lambda e, pO=pO, z=z, sc=sc: e.matmul(pO[:, 0:CH], Sb_[sc][:, :], qt[z][:, :], start=False, stop=True), R=[r_Sb[sc], r_qt[z]], W=[rpO])
                                    if dirn == 0:
                                        pg.op("act", lambda e, pO=pO, a0=a0: e.activation(oacc[:, a0:a0 + CH], pO[:, 0:CH], AF.Copy), R=[rpO], W=[r_oacc])
                                    else:
                                        pg.op("dve", lambda e, pO=pO, a0=a0: e.tensor_tensor(oacc[:, a0:a0 + CH], oacc[:, a0:a0 + CH], pO[:, 0:CH], ALU.add), R=[rpO, r_oacc], W=[r_oacc])
                                pS, rpS = nextpb()
                                pg.op("pe", lambda e, pS=pS, z=z, c=c: e.matmul(pS[:, 0:128], khT[z][:, :], vch[z][:, :], start=True, stop=True), R=[r_khT[z], r_vch[z]], W=[rpS])
                                pg.op("dve", lambda e, pS=pS, sc=sc, ebe=ebe: e.scalar_tensor_tensor(Sf[1 - sc][:, :], Sf[sc][:, :], ebe, pS[:, 0:128], ALU.mult, ALU.add),
                                      R=[r_Sf[sc], rpS, r_ex[0][z]], W=[r_Sf[1 - sc]])
                                pg.op("act", lambda e, sc=sc: e.activation(Sb_[1 - sc][:, :], Sf[1 - sc][:, :], AF.Copy), R=[r_Sf[1 - sc]], W=[r_Sb[1 - sc]])
                                sc = 1 - sc
                        r0 = LC if last else 0
                        for ci, (o, n) in enumerate([(o, min(512, N - o)) for o in range(r0, N, 512)]):
                            a = ci % 2
                            pg.op("act", lambda e, a=a, o=o, n=n: e.activation(sqb[a][:, 0:n], oacc[:, o:o + n], AF.Square), R=[r_oacc], W=[r_sqb[a]])
                            p, rp = nextpb()
                            pg.op("pe", lambda e, p=p, a=a, n=n: e.matmul(p[:, 0:n], onesb[:, :], sqb[a][:, 0:n], start=True, stop=True), R=[r_sqb[a], r_const], W=[rp])
                            pg.op("act", lambda e, p=p, a=a, n=n: e.activation(tmpa[a][:, 0:n], p[:, 0:n], AF.Ln, scale=1.0 / 128, bias=EPS), R=[rp], W=[r_tmpa[a]])
                            pg.op("act", lambda e, a=a, n=n: e.activation(tmpa[a][:, 0:n], tmpa[a][:, 0:n], AF.Exp, scale=-0.5), R=[r_tmpa[a]], W=[r_tmpa[a]])
                            pg.op("dve", lambda e, a=a, o=o, n=n: e.scalar_tensor_tensor(tmpb[a][:, 0:n], oacc[:, o:o + n], gn[:, 0:1], tmpa[a][:, 0:n], ALU.mult, ALU.mult),
                                  R=[r_oacc, r_tmpa[a], r_c], W=[r_tmpb[a]])
                            pg.op("dve", lambda e, a=a, o=o, n=n: e.tensor_tensor(yo[a][:, 0:n], tmpb[a][:, 0:n], sgate[:, o:o + n], ALU.mult), R=[r_tmpb[a], r_sgt], W=[r_yo[a]])
                            pg.dma("sp", yT_d[256 + h * 128:256 + (h + 1) * 128, g0 + o:g0 + o + n], yo[a][:, 0:n], R=[r_yo[a]], W=[r_yT])

        Gall = sb("Gall", [128, NT * NE]); GT = sb("GT", [NE, T], BF16); r_Gall = RS("Gall"); r_GT = RS("GT")

        def phaseH(l, last):
            with ExitStack() as st:
                norm_setup(st)
                wo = sb("wo", [128, 8, 1024], BF16, st); r_wo = RS("wo")
                Wr = sb("Wr", [128, 8, NE], F32, st); Wrp = sb("Wrp", [128, NB1 * 8 * NE], F32, st); rbt = sb("rbt", [1, NE], F32, st)
                rbias = sb("rbias", [1, NB1 * NE], F32, st); r_r = RS("router")
                G1t = sb("G1t", [128, NB1, 1024], F32, st); r_G1 = RS("G1t")
                yts = [sb("h_yt%d" % i, [128, 8, 128], BF16, st) for i in range(2)]; r_yts = [RS("h_yt%d" % i) for i in range(2)]
                xts = [sb("h_xt%d" % i, [128, 1024], F32, st) for i in range(2)]; r_xts = [RS("h_xt%d" % i) for i in range(2)]
                tmp = [sb("h_tmp%d" % i, [128, 512], F32, st) for i in range(2)]; r_tmp = [RS("h_tmp%d" % i) for i in range(2)]
                xnT = sb("h_xnT", [128, 1024], F32, st); r_xnT = RS("h_xnT")
                lg = [sb("h_lg%d" % i, [128, NE], F32, st) for i in range(2)]; r_lg = [RS("h_lg%d" % i) for i in range(2)]
                m8 = [sb("h_m8%d" % i, [128, 16], F32, st) for i in range(2)]
                ee = [sb("h_ee%d" % i, [128, 2 * NE], F32, st) for i in range(2)]
                for kc in range(8):
                    pg.dma("pool", wo[:, kc, :], w_out[l, kc * 128:(kc + 1) * 128, :], W=[r_wo])
                pg.dma("sp", Wr[:, :, :], rw_d[l, :, :].rearrange("(kc p) n -> p kc n", p=128), W=[r_r])
                pg.dma("sp", rbt[:, :], rb_d[l, :, :], W=[r_r])
                for b in range(NB1):
                    pg.dma("sp", G1t[:, b, :], Gt_d[b * 2 + 0, :, :], R=[r_Gt], W=[r_G1])
                    for kc in range(8):
                        ca = (2 * 8 + kc) * NB1 + b
                        pg.op("dve", lambda e: e.tensor_scalar(Wrp[:, (b * 8 + kc) * NE:(b * 8 + kc + 1) * NE], Wr[:, kc, :], MV[:, ca:ca + 1], None, ALU.mult),
                              R=[r_r, r_MV], W=[r_r])
                    p, rp = nextpb()
                    for kc in range(8):
                        cb_ = (3 * 8 + kc) * NB1 + b
                        pg.op("pe", lambda e: e.matmul(p[0:1, 0:NE], MV[:, cb_:cb_ + 1], Wr[:, kc, :], start=(kc == 0), stop=False), R=[r_r, r_MV], W=[rp])
                    pg.op("pe", lambda e: e.matmul(p[0:1, 0:NE], onesf[0:1, 0:1], rbt[0:1, :], start=False, stop=True), R=[r_r, r_const], W=[rp])
                    pg.op("act", lambda e: e.activation(rbias[0:1, b * NE:(b + 1) * NE], p[0:1, 0:NE], AF.Copy), R=[rp], W=[r_r])
                tiles = [i for i in range(NT) if not (last and tile_info(i)[2])]
                for n_, i in enumerate(tiles):
                    b, pos0, isc = tile_info(i)
                    bm = nb if isc else b
                    k = n_ % 2
                    yt, r_yt, xt, r_xt = yts[k], r_yts[k], xts[k], r_xts[k]
                    pg.dma("sp", yt[:, :, :], yT_d[:, i * 128:(i + 1) * 128].rearrange("(kc p) t -> p kc t", p=128), R=[r_yT], W=[r_yt])
                    pg.dma("sp", xt[:, :], xres[i * 128:(i + 1) * 128, :], R=[r_xres[i]], W=[r_xt])
                    for half in range(2):
                        p, rp = nextpb()
                        for kc in range(8):
                            pg.op("pe", lambda e: e.matmul(p[:, :], yt[:, kc, :], wo[:, kc, half * 512:(half + 1) * 512], start=(kc == 0), stop=(kc == 7)),
                                  R=[r_yt, r_wo], W=[rp])
                        pg.op("dve", lambda e: e.tensor_tensor(tmp[half][:, :], p[:, :], G1t[:, bm, half * 512:(half + 1) * 512], ALU.mult), R=[rp, r_G1], W=[r_tmp[half]])
                        pg.op("pool", lambda e: e.tensor_tensor(xt[:, half * 512:(half + 1) * 512], xt[:, half * 512:(half + 1) * 512], tmp[half][:, :], ALU.add),
                              R=[r_tmp[half], r_xt], W=[r_xt])
                    pg.dma("sp", xres[i * 128:(i + 1) * 128, :], xt[:, :], R=[r_xt], W=[r_xres[i]])
                    norm_mod_T(i, xt, r_xt, 2, keep_xnT=(xnT, r_xnT))
                    p, rp = nextpb()
                    for kc in range(8):
                        pg.op("pe", lambda e: e.matmul(p[:, 0:NE], xnT[:, kc * 128:(kc + 1) * 128], Wrp[:, (bm * 8 + kc) * NE:(bm * 8 + kc + 1) * NE], start=(kc == 0), stop=False),
                              R=[r_xnT, r_r], W=[rp])
                    pg.op("pe", lambda e: e.matmul(p[:, 0:NE], onesf[0:1, :], rbias[0:1, bm * NE:(bm + 1) * NE], start=False, stop=True), R=[r_r, r_const], W=[rp])
                    L_, rL = lg[k], r_lg[k]
                    M_, E_ = m8[k], ee[k]
                    pg.op("act", lambda e: e.activation(L_[:, :], p[:, 0:NE], AF.Copy), R=[rp], W=[rL])
                    pg.op("dve", lambda e: e.max(M_[:, 0:8], L_[:, :]), R=[rL], W=[rL])
                    pg.op("dve", lambda e: e.tensor_scalar(M_[:, 8:9], M_[:, 0:1], -1.0, None, ALU.mult), R=[rL], W=[rL])
                    pg.op("act", lambda e: e.activation(E_[:, 0:NE], L_[:, :], AF.Exp, bias=M_[:, 8:9], scale=1.0), R=[rL], W=[rL])
                    pg.op("dve", lambda e: e.tensor_scalar(E_[:, NE:2 * NE], L_[:, :], M_[:, TOPK - 1:TOPK], None, ALU.is_ge), R=[rL], W=[rL])
                    pg.op("dve", lambda e: e.tensor_tensor(E_[:, 0:NE], E_[:, 0:NE], E_[:, NE:2 * NE], ALU.mult), R=[rL], W=[rL])
                    pg.op("dve", lambda e: e.reduce_sum(M_[:, 9:10], E_[:, 0:NE], AX.X), R=[rL], W=[rL])
                    pg.op("dve", lambda e: e.reciprocal(M_[:, 9:10], M_[:, 9:10]), R=[rL], W=[rL])
                    pg.op("dve", lambda e: e.tensor_scalar(Gall[:, i * NE:(i + 1) * NE], E_[:, 0:NE], M_[:, 9:10], None, ALU.mult), R=[rL], W=[r_Gall])
                    p2, rp2 = nextpb()
                    pg.op("pe", lambda e: e.transpose(p2[0:NE, 0:128], Gall[:, i * NE:(i + 1) * NE], identf[:, :]), R=[r_Gall, r_const], W=[rp2])
                    pg.op("act", lambda e: e.activation(GT[:, i * 128:(i + 1) * 128], p2[0:NE, 0:128], AF.Copy), R=[rp2], W=[r_GT])
                for kc in range(8):
                    pg.dma("sp", h2T_d[:, kc, :], hT[:, kc * T:(kc + 1) * T], R=[r_hT], W=[r_h2])

        def phaseI(l, last):
            TGT = cfg.TG // 128
            with ExitStack() as st:
                acc = sb("i_acc", [128, TGT, 1024], F32, st); r_acc = [RS("i_acc%d" % j) for j in range(TGT)]
                h2g = sb("i_h2g", [128, 8, cfg.TG], BF16, st); r_h2g = RS("i_h2g")
                actT = sb("i_actT", [128, FC, cfg.TG], BF16, st); r_actT = RS("i_actT")
                wd = [sb("i_wd%d" % i, [128, FC, 1024], BF16, st) for i in range(2)]; r_wd = [RS("i_wd%d" % i) for i in range(2)]
                wgp = [sb("i_wg%d" % i, [128, 8, 256], BF16, st) for i in range(4)]; r_wgp = [RS("i_wg%d" % i) for i in range(4)]
                bgu = sb("i_bgu", [128, NE * 2 * FC], F32, st); bdf = sb("i_bdf", [NE, 1024], F32, st); bdb = sb("i_bdb", [NE, 1024], BF16, st); r_b = RS("i_bias")
                G2t = sb("i_G2t", [128, NB1, 1024], F32, st); r_G2 = RS("i_G2t")
                tg = [[sb("i_t%d%d" % (j, i), [128, 512], F32, st) for i in range(2)] for j in range(5)]
                r_tg = [[RS("i_t%d%d" % (j, i)) for i in range(2)] for j in range(5)]
                xts = [sb("i_xt%d" % i, [128, 1024], F32, st) for i in range(2)]; r_xts = [RS("i_xt%d" % i) for i in range(2)]
                pg.dma("sp", bgu[:, :], bguT_d[l, :, :], W=[r_b])
                pg.dma("sp", bdf[:, :], bdn_d[l, :, :], W=[r_b])
                pg.op("act", lambda e: e.activation(bdb[:, :], bdf[:, :], AF.Copy), R=[r_b], W=[r_b])
                for b in range(NB1):
                    pg.dma("sp", G2t[:, b, :], Gt_d[b * 2 + 1, :, :], R=[r_Gt], W=[r_G2])
                tiles = [i for i in range(NT) if not (last and tile_info(i)[2])]
                groups = [tiles[i:i + TGT] for i in range(0, len(tiles), TGT)]
                cnt = {"w": 0, "d": 0, "t": 0, "x": 0}
                for grp in groups:
                    ng = len(grp); ntok = ng * 128
                    for j, i in enumerate(grp):
                        pg.dma("sp", h2g[:, :, j * 128:(j + 1) * 128], h2T_d[:, :, i * 128:(i + 1) * 128], R=[r_h2], W=[r_h2g])
                        for half in range(2):
                            p, rp = nextpb()
                            pg.op("pe", lambda e: e.matmul(p[:, :], GT[:, i * 128:(i + 1) * 128], bdb[:, half * 512:(half + 1) * 512], start=True, stop=True),
                                  R=[r_GT, r_b], W=[rp])
                            pg.op("act", lambda e: e.activation(acc[:, j, half * 512:(half + 1) * 512], p[:, :], AF.Copy), R=[rp], W=[r_acc[j]])
                    tts = [(o, min(512, ntok - o)) for o in range(0, ntok, 512)]
                    for ex_ in range(NE):
                        wdt, rwd = wd[cnt["d"] % 2], r_wd[cnt["d"] % 2]; cnt["d"] += 1
                        for fc in range(FC):
                            pg.dma("pool", wdt[:, fc, :], wdn_d[l, ex_, fc * 128:(fc + 1) * 128, :], W=[rwd])
                        for fc in range(FC):
                            wg, rwg = wgp[cnt["w"] % 4], r_wgp[cnt["w"] % 4]; cnt["w"] += 1
                            for (c0, o_) in ((fc * 128, 0), (DFF + fc * 128, 128)):
                                pg.dma("pool", wg[:, :, o_:o_ + 128], wgu_d[l, ex_, :, c0:c0 + 128].rearrange("(kc p) n -> p kc n", p=128), W=[rwg])
                            cg = ex_ * 2 * FC + fc; cl = ex_ * 2 * FC + FC + fc
                            for (o, n) in tts:
                                z = cnt["t"] % 2; cnt["t"] += 1
                                pgl, rpgl = nextpb()
                                for kc in range(8):
                                    pg.op("pe", lambda e: e.matmul(pgl[:, 0:n], wg[:, kc, 0:128], h2g[:, kc, o:o + n], start=(kc == 0), stop=(kc == 7)), R=[rwg, r_h2g], W=[rpgl])
                                pli, rpli = nextpb()
                                for kc in range(8):
                                    pg.op("pe", lambda e: e.matmul(pli[:, 0:n], wg[:, kc, 128:256], h2g[:, kc, o:o + n], start=(kc == 0), stop=(kc == 7)), R=[rwg, r_h2g], W=[rpli])
                                glu, sg_, linb, linc, tt_ = (tg[j][z] for j in range(5))
                                rglu, rsg, rlinb, rlinc, rtt = (r_tg[j][z] for j in range(5))
                                pg.op("dve", lambda e: e.tensor_scalar(glu[:, 0:n], pgl[:, 0:n], bgu[:, cg:cg + 1], 7.0, ALU.add, ALU.min), R=[rpgl, r_b], W=[rglu])
                                pg.op("act", lambda e: e.activation(sg_[:, 0:n], glu[:, 0:n], AF.Sigmoid, scale=1.702), R=[rglu], W=[rsg])
                                pg.op("act", lambda e: e.activation(linb[:, 0:n], pli[:, 0:n], AF.Identity, bias=bgu[:, cl:cl + 1], scale=1.0), R=[rpli, r_b], W=[rlinb])
                                pg.op("pool", lambda e: e.tensor_scalar(linc[:, 0:n], linb[:, 0:n], 7.0, -7.0, ALU.min, ALU.max), R=[rlinb], W=[rlinc])
                                pg.op("dve", lambda e: e.tensor_tensor(tt_[:, 0:n], glu[:, 0:n], sg_[:, 0:n], ALU.mult), R=[rglu, rsg], W=[rtt])
                                pg.op("dve", lambda e: e.scalar_tensor_tensor(actT[:, fc, o:o + n], linc[:, 0:n], 1.0, tt_[:, 0:n], ALU.add, ALU.mult), R=[rlinc, rtt], W=[r_actT])
                        for j, i in enumerate(grp):
                            for half in range(2):
                                p, rp = nextpb()
                                for fc in range(FC):
                                    pg.op("pe", lambda e: e.matmul(p[:, :], actT[:, fc, j * 128:(j + 1) * 128], wdt[:, fc, half * 512:(half + 1) * 512], start=(fc == 0), stop=(fc == FC - 1)),
                                          R=[r_actT, rwd], W=[rp])
                                pg.op("dve", lambda e: e.scalar_tensor_tensor(acc[:, j, half * 512:(half + 1) * 512], p[:, :], Gall[:, i * NE + ex_: i * NE + ex_ + 1],
                                                                            acc[:, j, half * 512:(half + 1) * 512], ALU.mult, ALU.add), R=[rp, r_Gall, r_acc[j]], W=[r_acc[j]])
                    for j, i in enumerate(grp):
                        b, pos0, isc = tile_info(i)
                        bm = nb if isc else b
                        k = cnt["x"] % 2; cnt["x"] += 1
                        xt, r_xt = xts[k], r_xts[k]
                        pg.dma("sp", xt[:, :], xres[i * 128:(i + 1) * 128, :], R=[r_xres[i]], W=[r_xt])
                        pg.op("dve", lambda e: e.tensor_tensor(acc[:, j, :], acc[:, j, :], G2t[:, bm, :], ALU.mult), R=[r_acc[j], r_G2], W=[r_acc[j]])
                        pg.op("pool", lambda e: e.tensor_tensor(xt[:, :], xt[:, :], acc[:, j, :], ALU.add), R=[r_acc[j], r_xt], W=[r_xt])
                        if last:
                            row = b * L + (pos0 - LC)
                            pg.dma("sp", y_d[row:row + 128, :], xt[:, :], R=[r_xt], W=[r_y])
                        else:
                            pg.dma("sp", xres[i * 128:(i + 1) * 128, :], xt[:, :], R=[r_xt], W=[r_xres[i]])

        stop_after = getattr(cfg, "stop_after", None)
        def bar():
            pg.barrier((tok_d[0:1, :], ident_d[0:1, 0:64]), r_bar)

        for l in range(dp):
            last = (l == dp - 1)
            with ExitStack() as stL:
                hT = sb("hT%d" % l, [128, 8 * T], BF16, stL)
                phaseA(l); bar()
                phaseB(l); bar()
                phaseC(l); bar()
                phaseEF(l, last); bar()
                phaseG(l, last); bar()
                phaseH(l, last); bar()
            phaseI(l, last); bar()
        pg.finish([r_y] + list(r_dbg.values()))
        nc._n_sems = pg.nsem
        nc._pg = pg
    return nc


_CACHE = {}


def kernel(**inputs):
    n_cores = 8
    B = inputs["x"].shape[0]
    cfg = Cfg(nb=B // n_cores, L=inputs["x"].shape[1], LC=inputs["ctx"].shape[1], NE=inputs["w_gu"].shape[1],
              DFF=inputs["w_down"].shape[2], depth=inputs["w_in"].shape[0], TG=1024)
    nc = build_nc(cfg)
    in_maps = []
    shared = None
    for core in range(n_cores):
        m, shared = prep_inputs(cfg, core, shared=shared, **inputs)
        in_maps.append(m)
    res = run_bass_kernel_spmd(nc, in_maps, core_ids=list(range(n_cores)))
    outs = [np.asarray(r["y"], np.float32).reshape(cfg.nb, cfg.L, D) for r in res.results]
    return np.concatenate(outs, axis=0)
```

```python
import numpy as np
from contextlib import ExitStack
import concourse.bass as bass
import concourse.mybir as mybir
from concourse.bass_utils import run_bass_kernel_spmd

F32 = mybir.dt.float32
BF16 = mybir.dt.bfloat16
AF = mybir.ActivationFunctionType
ALU = mybir.AluOpType
AX = mybir.AxisListType

D = 1024
GRID_W = 64
HD = 64
NA_H = 4
HG_H = 4
HG_DK = 128
SW_QH = 4
SW_KVH = 2
IN_COLS = 3840
EPS = 1e-6
NEG = -1e30
TOPK = 4
C_NAQ, C_NAK, C_NAV = 0, 256, 512
C_HGQ, C_HGFF, C_HGFB, C_HGI, C_HGG = 768, 1280, 1792, 2304, 2816
C_SWQ, C_SWK, C_SWV = 3328, 3584, 3712


class Cfg:
    def __init__(self, nb=2, L=2048, LC=256, NE=32, DFF=1024, depth=2, TG=1536):
        self.nb, self.L, self.LC, self.NE, self.DFF, self.depth, self.TG = nb, L, LC, NE, DFF, depth, TG
        self.N = L + LC
        self.T = nb * self.N
        self.R = L // GRID_W


class Res:
    __slots__ = ("name", "w", "rs", "sem", "cnt", "swq", "used")

    def __init__(self, name):
        self.name, self.w, self.rs, self.sem, self.cnt, self.used = name, {}, {}, None, 0, False


class _Rec:
    def __getattr__(self, name):
        def f(*a, **k):
            self.call = (name, a, k)
            return self
        return f


class _Eng:
    def __init__(self, key, sem):
        self.key, self.sem, self.count, self.ops, self.last, self.pending, self.waited = key, sem, 0, [], None, False, {}


class Prog:
    def __init__(self, nc, es):
        self.nc, self.es = nc, es
        self.eng = {}
        for k in ("pe", "act", "dve", "pool", "sp"):
            self.eng[k] = _Eng(k, es.enter_context(nc.semaphore("e_" + k)))
        self.semown = {id(e.sem): e for e in self.eng.values()}
        self.nsem = 5
        self.dma_res = []
        self.sem_pool = {}
        self.all_res = []
        self.nres = 0

    def res(self, name):
        self.nres += 1
        r = Res(name)
        self.all_res.append(r)
        return r

    def _wait(self, E, ev):
        if ev is None:
            return
        sem, val = ev
        own = self.semown.get(id(sem))
        if own is not None and own is E and E.key == "pe":
            return
        if own is not None and val > own.count:
            assert val == own.count + 1 and own.pending, (own.key, val, own.count)
            own.last["inc"] = True
            own.count += 1
            own.pending = False
        if E.waited.get(id(sem), 0) >= val:
            return
        E.waited[id(sem)] = val
        E.ops.append({"wait": (sem, val)})

    def _deps(self, E, R, W, dma_write=False):
        for r in R:
            for ev in list(r.w.values()):
                self._wait(E, ev)
        for w in W:
            for ev in list(w.w.values()):
                if dma_write and id(ev[0]) not in self.semown:
                    continue
                self._wait(E, ev)
            for ev in list(w.rs.values()):
                self._wait(E, ev)

    def _commit(self, ev, R, W, dma_write=False):
        for r in R:
            old = r.rs.get(id(ev[0]))
            if old is None or old[1] < ev[1]:
                r.rs[id(ev[0])] = ev
        for w in W:
            if dma_write:
                w.w = {k: v for k, v in w.w.items() if k not in self.semown}
                w.w[id(ev[0])] = ev
            else:
                w.w = {id(ev[0]): ev}
            w.rs = {}

    def op(self, ek, fn, R=(), W=()):
        E = self.eng[ek]
        self._deps(E, R, W)
        rec = _Rec()
        fn(rec)
        ent = {"fn": rec.call, "inc": False}
        E.ops.append(ent)
        E.last = ent
        E.pending = True
        self._commit((E.sem, E.count + 1), R, W)

    def dma(self, qk, out, in_, R=(), W=(), **kw):
        E = self.eng[qk]
        is_store = ("DRam" in type(out.tensor).__name__) and ("DRam" not in type(in_.tensor).__name__)
        own = R[0] if is_store else W[0]
        if own.sem is None:
            pool = self.sem_pool.setdefault(qk == "pool", [])
            own.swq = (qk == "pool")
            if pool:
                own.sem, own.cnt = pool.pop()
            else:
                own.sem, own.cnt = self.es.enter_context(self.nc.semaphore("d_%d" % self.nsem)), 0
                self.nsem += 1
            self.dma_res.append(own)
        self._deps(E, R, W, dma_write=True)
        if own.sem is not None and getattr(own, "used", False):
            if is_store:
                cont = id(own.sem) in own.rs
            else:
                cont = (not own.rs) and all(k not in self.semown for k in own.w)
            if not cont:
                self._wait(E, (own.sem, own.cnt))
        own.used = True
        own.cnt += 16
        E.ops.append({"dma": (out, in_, kw, own.sem)})
        self._commit((own.sem, own.cnt), R, W, dma_write=True)

    def barrier(self, tok_d, r_bar):
        sp = self.eng["sp"]
        for k in ("pe", "act", "dve", "pool"):
            X = self.eng[k]
            if X.pending:
                self._wait(sp, (X.sem, X.count + 1))
            elif X.count > 0:
                self._wait(sp, (X.sem, X.count))
        for r in self.dma_res:
            if r is not r_bar:
                self._wait(sp, (r.sem, r.cnt))
        if r_bar.sem is not None:
            self._wait(sp, (r_bar.sem, r_bar.cnt))
        self.dma("sp", tok_d[0], tok_d[1], W=[r_bar])
        for r in self.dma_res:
            if r.swq:
                self._wait(self.eng["pool"], (r.sem, r.cnt))
        for k in ("pe", "act", "dve", "pool"):
            for ev in list(r_bar.w.values()):
                self._wait(self.eng[k], ev)
        for r in self.all_res:
            r.w = {}
            r.rs = {}
            if r.sem is not None and r is not r_bar:
                self.sem_pool[r.swq].append((r.sem, r.cnt))
                r.sem = None
                r.used = False
        self.dma_res = []

    def finish(self, outs):
        E = self.eng["sp"]
        for r in outs:
            for ev in list(r.w.values()):
                self._wait(E, ev)
        nc = self.nc
        with nc.Block() as block:
            def run(E, e):
                for o in E.ops:
                    if "wait" in o:
                        e.wait_ge(o["wait"][0], o["wait"][1])
                    elif "dma" in o:
                        out, in_, kw, sem = o["dma"]
                        e.dma_start(out=out, in_=in_, **kw).then_inc(sem, 16)
                    else:
                        nm_, a_, k_ = o["fn"]
                        ins = getattr(e, nm_)(*a_, **k_)
                        if o["inc"]:
                            ins.then_inc(E.sem, 1)

            @block.sync
            def _(e):
                run(self.eng["sp"], e)

            @block.tensor
            def _(e):
                run(self.eng["pe"], e)

            @block.scalar
            def _(e):
                run(self.eng["act"], e)

            @block.vector
            def _(e):
                run(self.eng["dve"], e)

            @block.gpsimd
            def _(e):
                run(self.eng["pool"], e)


def _fm(v, p=128):
    sh = v.shape
    return np.ascontiguousarray(np.swapaxes(v.reshape(sh[:-1] + (sh[-1] // p, p)), -1, -2))


def _na_bias_table(rpb):
    H = rpb.shape[0]
    kc = np.arange(64)[:, None]
    qc = np.arange(64)[None, :]
    qs = np.clip(qc - 8, 0, 48)
    ok = (kc >= qs) & (kc < qs + 16)
    dc = np.clip(kc - qc + 15, 0, 30)
    g = rpb[:, :, dc]
    g = np.where(ok[None, None], g, np.float32(NEG)).astype(np.float32)
    tab = np.empty((2, 64, H, 14, 64), np.float32)
    for dl in range(2):
        tab[dl] = np.transpose(g[:, dl:dl + 14], (2, 0, 1, 3))
    return np.ascontiguousarray(tab.reshape(128, H * 14 * 64))


def _consts(cfg):
    L = cfg.L
    c = {}
    c["ident"] = np.eye(128, dtype=np.float32)
    s = np.arange(64)[:, None]
    t = np.arange(64)[None, :]
    c["maskUL"] = np.concatenate([(s <= t), (s >= t)], axis=1).astype(np.float32)
    kp = np.arange(128)[:, None]
    qp = np.arange(128)[None, :]
    ml = np.where(kp >= qp, 0.0, NEG)
    mr = np.where(kp <= qp, 0.0, NEG)
    c["swam"] = np.concatenate([ml, mr], axis=1).astype(np.float32)
    tok = np.arange(L)
    row = (tok // GRID_W).astype(np.float64)
    col = (tok % GRID_W).astype(np.float64)
    inv = 10000.0 ** (-np.arange(16, dtype=np.float64) / 16)
    C = np.zeros((64, L)); S = np.zeros((64, L))
    for d in range(64):
        pos = row if d < 32 else col
        ang = pos * inv[d % 16]
        C[d] = np.cos(ang)
        S[d] = -np.sin(ang) if (d % 32) < 16 else np.sin(ang)
    c["ropeC"] = np.concatenate([C, C], 0).astype(np.float32)
    c["ropeS"] = np.concatenate([S, S], 0).astype(np.float32)
    P = np.zeros((128, 128), np.float32)
    for do in range(128):
        dd = do % 32
        di = do + 16 if dd < 16 else do - 16
        P[di, do] = 1.0
    c["ropeP"] = P
    o2 = np.zeros((128, 128), np.float32)
    o2[:64, :64] = 1.0
    o2[64:, 64:] = 1.0
    c["ones2"] = o2
    return c


def prep_inputs(cfg, core, x, c, ctx, c_ctx, hg_lower_bounds, ada_w, ada_b, norm1_g, norm2_g, w_in, na_q_norm,
                na_k_norm, na_rpb, hg_norm_g, swa_q_norm, swa_k_norm, swa_sink, w_out, router_w, router_b,
                w_gu, b_gu, w_down, b_down, shared=None):
    nb, dp = cfg.nb, cfg.depth
    f = np.float32
    b0 = core * nb
    m = {}
    m["x"] = np.ascontiguousarray(x[b0:b0 + nb].reshape(nb * cfg.L, D), f)
    m["ctx"] = np.ascontiguousarray(ctx[b0:b0 + nb].reshape(nb * cfg.LC, D), f)
    cc = np.concatenate([c[b0:b0 + nb], c_ctx[None]], 0).astype(f)
    m["ccT"] = np.ascontiguousarray(np.transpose(cc.reshape(nb + 1, 8, 128), (2, 1, 0)).reshape(128, 8 * (nb + 1)))
    if shared is None:
        shared = {}
        shared["ada_w"] = np.ascontiguousarray(ada_w, f)
        shared["ada_bT"] = _fm(ada_b.astype(f))
        shared["ada_b"] = np.ascontiguousarray(ada_b.astype(f).reshape(dp, 1, 6 * D))
        shared["n1T"] = _fm(norm1_g.astype(f))
        shared["n2T"] = _fm(norm2_g.astype(f))
        shared["w_in"] = np.ascontiguousarray(w_in, f)
        shared["w_out"] = np.ascontiguousarray(w_out, f)
        g4 = np.stack([np.tile(na_q_norm, (1, 2)), np.tile(na_k_norm, (1, 2)), np.tile(swa_q_norm, (1, 2)),
                       np.tile(swa_k_norm, (1, 2))], axis=-1)
        shared["gains4"] = np.ascontiguousarray(g4, f)
        shared["nab"] = np.stack([_na_bias_table(na_rpb[l].astype(f)) for l in range(dp)])
        shared["hlbT"] = np.ascontiguousarray(np.transpose(hg_lower_bounds.astype(f).reshape(dp, HG_H, 128), (2, 0, 1)).reshape(128, dp * HG_H))
        shared["hgn"] = np.ascontiguousarray(hg_norm_g.astype(f).reshape(dp, 128, 1))
        shared["sink"] = np.ascontiguousarray(np.broadcast_to(swa_sink.astype(f)[:, None, :], (dp, 128, SW_QH)))
        shared["router_w"] = np.ascontiguousarray(router_w, f)
        shared["router_b"] = np.ascontiguousarray(router_b.astype(f).reshape(dp, 1, cfg.NE))
        shared["w_gu"] = np.ascontiguousarray(w_gu, f)
        shared["b_guT"] = np.ascontiguousarray(_fm(b_gu.astype(f)).transpose(0, 2, 1, 3).reshape(dp, 128, -1))
        shared["w_down"] = np.ascontiguousarray(w_down, f)
        shared["b_down"] = np.ascontiguousarray(b_down, f)
        shared.update(_consts(cfg))
    m.update(shared)
    return m, shared


def build_nc(cfg, dbg=()):
    nc = bass.Bass("TRN2", target_bir_lowering=False)
    nb, L, LC, N, T, NE, DFF, dp, R = cfg.nb, cfg.L, cfg.LC, cfg.N, cfg.T, cfg.NE, cfg.DFF, cfg.depth, cfg.R
    NB1 = nb + 1
    NT = T // 128
    FC = DFF // 128
    ins = {}

    def din(name, shape):
        ins[name] = nc.dram_tensor(name, list(shape), F32, kind="ExternalInput")
        return ins[name]

    x_d = din("x", [nb * L, D]); ctx_d = din("ctx", [nb * LC, D]); ccT_d = din("ccT", [128, 8 * NB1])
    ada_w = din("ada_w", [dp, D, 6 * D]); ada_bT = din("ada_bT", [dp, 128, 48]); ada_b = din("ada_b", [dp, 1, 6 * D])
    n1T_d = din("n1T", [dp, 128, 8]); n2T_d = din("n2T", [dp, 128, 8])
    w_in = din("w_in", [dp, D, IN_COLS]); w_out = din("w_out", [dp, D, D])
    gains4 = din("gains4", [dp, 128, 4]); nab_d = din("nab", [dp, 128, NA_H * 14 * 64])
    hlbT_d = din("hlbT", [128, dp * HG_H]); hgn_d = din("hgn", [dp, 128, 1]); sink_d = din("sink", [dp, 128, SW_QH])
    rw_d = din("router_w", [dp, D, NE]); rb_d = din("router_b", [dp, 1, NE])
    wgu_d = din("w_gu", [dp, NE, D, 2 * DFF]); bguT_d = din("b_guT", [dp, 128, NE * 2 * FC])
    wdn_d = din("w_down", [dp, NE, DFF, D]); bdn_d = din("b_down", [dp, NE, D])
    ident_d = din("ident", [128, 128]); maskUL_d = din("maskUL", [64, 128]); swam_d = din("swam", [128, 256])
    ropeC_d = din("ropeC", [128, L]); ropeS_d = din("ropeS", [128, L]); ropeP_d = din("ropeP", [128, 128])
    ones2_d = din("ones2", [128, 128])
    y_d = nc.dram_tensor("y", [nb * L, D], F32, kind="ExternalOutput")
    dbg_d = {k: nc.dram_tensor("dbg_" + k, list(s), F32, kind="ExternalOutput") for k, s in dbg}

    xres = nc.dram_tensor("xres", [T, D], F32)
    qk_d = nc.dram_tensor("qk_d", [7, 128, T], BF16)
    v_d = nc.dram_tensor("v_d", [T, 384], BF16)
    yT_d = nc.dram_tensor("yT_d", [D, T], BF16)
    h2T_d = nc.dram_tensor("h2T_d", [128, 8, T], BF16)
    Gt_d = nc.dram_tensor("Gt_d", [NB1 * 2, 128, D], F32)

    with ExitStack() as es:
        pg = Prog(nc, es)

        uid = [0]
        minrem = [1 << 30]

        def sb(name, shape, dt=F32, stack=es):
            uid[0] += 1
            t_ = stack.enter_context(nc.sbuf_tensor("s%d_%s" % (uid[0], name), list(shape), dt))
            minrem[0] = min(minrem[0], nc.sbuf_bytes_remaining)
            return t_

        def ps(name, shape, dt=F32, stack=es):
            uid[0] += 1
            return stack.enter_context(nc.psum_tensor("p%d_%s" % (uid[0], name), list(shape), dt))

        RS = pg.res
        r_hT = RS("hT")
        tok_d = nc.dram_tensor("bar_tok", [2, 64], F32); r_bar = RS("bar")
        identf = sb("identf", [128, 128]); identb = sb("identb", [128, 128], BF16)
        ones2 = sb("ones2b", [128, 128], BF16); onesb = sb("onesb", [128, 128], BF16); onesf = sb("onesf", [128, 128])
        csf = sb("csf", [128, 8 * NB1]); csb = sb("csb", [128, 8 * NB1], BF16)
        MV = sb("MV", [128, 4 * 8 * NB1])
        stage = sb("stage", [128, 128])
        r_const = RS("const"); r_cs = RS("cs"); r_MV = RS("MV"); r_stage = RS("stage")
        psT = ps("psT", [128, 1024]); r_psT = RS("psT")
        pb = [ps("pb%d" % i, [128, 512]) for i in range(5)]; r_pb = [RS("pb%d" % i) for i in range(5)]
        pbf = ps("pbf", [128, 1024], BF16); r_pbf = RS("pbf")
        r_xres = [RS("xres%d" % i) for i in range(NT)]
        r_y = RS("y"); r_qk = RS("qk_d"); r_v = RS("v_d"); r_yT = RS("yT_d"); r_h2 = RS("h2T_d"); r_Gt = RS("Gt_d")
        r_dbg = {k: RS("dbg" + k) for k in dbg_d}
        rot = {"pb": 0}

        def nextpb():
            i = rot["pb"]; rot["pb"] = (i + 1) % 5
            return pb[i], r_pb[i]

        def tile_info(i):
            g0 = i * 128; b = g0 // N; pos0 = g0 - b * N
            return b, pos0, pos0 < LC

        pg.dma("sp", identf[:, :], ident_d[:, :], W=[r_const])
        pg.dma("sp", stage[:, :], ones2_d[:, :], W=[r_stage])
        pg.op("dve", lambda e: e.tensor_copy(identb[:, :], identf[:, :]), R=[r_const], W=[r_const])
        pg.op("dve", lambda e: e.tensor_copy(ones2[:, :], stage[:, :]), R=[r_stage], W=[r_const])
        pg.op("dve", lambda e: e.memset(onesb[:, :], 1.0), W=[r_const])
        pg.op("dve", lambda e: e.memset(onesf[:, :], 1.0), W=[r_const])
        pg.dma("sp", csf[:, :], ccT_d[:, :], W=[r_cs])
        pg.op("act", lambda e: e.activation(csf[:, :], csf[:, :], AF.Silu), R=[r_cs], W=[r_cs])
        pg.op("dve", lambda e: e.tensor_copy(csb[:, :], csf[:, :]), R=[r_cs], W=[r_cs])
        r_init = RS("init")
        for b in range(nb):
            for (src, s0, n, p0) in ((ctx_d, b * LC, LC, 0), (x_d, b * L, L, LC)):
                for j in range(n // 128):
                    gi = (b * N + p0) // 128 + j
                    pg.dma("sp", xres[gi * 128:(gi + 1) * 128, :], src[s0 + j * 128:s0 + (j + 1) * 128, :], W=[r_init])

        pg.barrier((tok_d[0:1, :], ident_d[0:1, 0:64]), r_bar)

        def dbg_out(key, ap, res_list, rows=128):
            if key in dbg_d:
                pg.dma("sp", dbg_d[key][0:rows, :], ap, R=res_list, W=[r_dbg[key]])

        def phaseA(l):
            with ExitStack() as st:
                wts = [sb("adaw%d" % i, [128, 8 * 1024], BF16, st) for i in range(2)]
                r_w = [RS("adaw%d" % i) for i in range(2)]
                abT = sb("abT", [128, 48], F32, st); abrow = sb("abrow", [1, 6 * D], F32, st); abrowb = sb("abrowb", [1, 6 * D], BF16, st)
                nT = sb("nT", [128, 16], F32, st); modT = sb("modT", [128, 4 * 8 * NB1], F32, st)
                gt = [sb("gt%d" % i, [128, 512], F32, st) for i in range(2)]; r_gt = [RS("gt%d" % i) for i in range(2)]
                r_ab = RS("ab"); r_mod = RS("modT")
                csrep = sb("csrep", [128, 8 * NB1 * 128], BF16, st); zerosf = sb("zerosf", [128, 128], F32, st)
                pg.op("dve", lambda e: e.memset(zerosf[:, :], 0.0), W=[r_cs])
                for kc in range(8):
                    for b in range(NB1):
                        c0 = (kc * NB1 + b)
                        pg.op("dve", lambda e: e.tensor_scalar(csrep[:, c0 * 128:(c0 + 1) * 128], zerosf[:, 0:128], csf[:, c0:c0 + 1], None, ALU.add), R=[r_cs], W=[r_cs])
                pg.dma("sp", abT[:, :], ada_bT[l, :, :], W=[r_ab])
                pg.dma("sp", abrow[:, :], ada_b[l, :, :], W=[r_ab])
                pg.dma("sp", nT[:, 0:8], n1T_d[l, :, :], W=[r_ab])
                pg.dma("sp", nT[:, 8:16], n2T_d[l, :, :], W=[r_ab])
                pg.op("act", lambda e: e.activation(abrowb[:, :], abrow[:, :], AF.Copy), R=[r_ab], W=[r_ab])
                gi = 0
                for blk in range(6):
                    wt, rw = wts[blk % 2], r_w[blk % 2]
                    for kc in range(8):
                        pg.dma("pool", wt[:, kc * 1024:(kc + 1) * 1024], ada_w[l, kc * 128:(kc + 1) * 128, blk * 1024:(blk + 1) * 1024], W=[rw])
                    if blk in (0, 1, 3, 4):
                        fm = {0: 0, 1: 1, 3: 2, 4: 3}[blk]
                        p, rp = nextpb()
                        for j in range(8):
                            for kc in range(8):
                                pg.op("pe", lambda e, p=p, j=j, kc=kc, wt=wt: e.matmul(
                                    p[:, j * NB1:(j + 1) * NB1], wt[:, kc * 1024 + j * 128: kc * 1024 + (j + 1) * 128],
                                    csb[:, kc * NB1:(kc + 1) * NB1], start=(kc == 0), stop=(kc == 7)), R=[rw, r_cs], W=[rp])
                        for j in range(8):
                            pg.op("dve", lambda e, p=p, j=j, fm=fm, blk=blk: e.tensor_scalar(
                                modT[:, (fm * 8 + j) * NB1:(fm * 8 + j + 1) * NB1], p[:, j * NB1:(j + 1) * NB1],
                                abT[:, blk * 8 + j: blk * 8 + j + 1], None, ALU.add), R=[rp, r_ab], W=[r_mod])
                    else:
                        which = 0 if blk == 2 else 1
                        for b in range(NB1):
                            for half in range(2):
                                p, rp = nextpb()
                                for kc in range(8):
                                    c0 = kc * NB1 + b
                                    pg.op("pe", lambda e, p=p, kc=kc, c0=c0, half=half, wt=wt: e.matmul(
                                        p[:, :], csrep[:, c0 * 128:(c0 + 1) * 128], wt[:, kc * 1024 + half * 512: kc * 1024 + (half + 1) * 512],
                                        start=(kc == 0), stop=False), R=[rw, r_cs], W=[rp])
                                cb = blk * 1024 + half * 512
                                pg.op("pe", lambda e, p=p, cb=cb: e.matmul(p[:, :], onesb[0:1, :], abrowb[0:1, cb:cb + 512], start=False, stop=True),
                                      R=[r_ab, r_const], W=[rp])
                                g, rg = gt[gi % 2], r_gt[gi % 2]; gi += 1
                                pg.op("act", lambda e, g=g, p=p: e.activation(g[:, :], p[:, :], AF.Copy), R=[rp], W=[rg])
                                pg.dma("sp", Gt_d[b * 2 + which, :, half * 512:(half + 1) * 512], g[:, :], R=[rg], W=[r_Gt])
                for j in range(8):
                    for (w, scf, shf, nofs) in ((0, 1, 0, 0), (2, 3, 2, 8)):
                        pg.op("dve", lambda e, j=j, w=w, scf=scf, nofs=nofs: e.tensor_scalar(
                            MV[:, (w * 8 + j) * NB1:(w * 8 + j + 1) * NB1], modT[:, (scf * 8 + j) * NB1:(scf * 8 + j + 1) * NB1],
                            1.0, nT[:, nofs + j:nofs + j + 1], ALU.add, ALU.mult), R=[r_mod, r_ab], W=[r_MV])
                        pg.op("dve", lambda e, j=j, w=w, shf=shf: e.tensor_copy(
                            MV[:, ((w + 1) * 8 + j) * NB1:((w + 1) * 8 + j + 1) * NB1], modT[:, (shf * 8 + j) * NB1:(shf * 8 + j + 1) * NB1]),
                            R=[r_mod], W=[r_MV])

        nm = {}

        def norm_setup(st):
            nm["junk"] = sb("nm_junk", [128, 1024], BF16, st); nm["ss"] = [sb("nm_ss%d" % i, [128, 2], F32, st) for i in range(2)]
            nm["xn"] = [sb("nm_xn%d" % i, [128, 1024], F32, st) for i in range(2)]
            nm["r_junk"] = RS("nm_junk"); nm["r_ss"] = [RS("nm_ss%d" % i) for i in range(2)]; nm["r_xn"] = [RS("nm_xn%d" % i) for i in range(2)]
            nm["k"] = 0

        def norm_mod_T(i, xt, r_xt, w, dstT=None, r_dst=None, keep_xnT=None):
            b, pos0, isc = tile_info(i)
            bm = nb if isc else b
            k = nm["k"]; nm["k"] = 1 - k
            ss, r_ss, xn, r_xn = nm["ss"][k], nm["r_ss"][k], nm["xn"][k], nm["r_xn"][k]
            pg.op("act", lambda e: e.activation(nm["junk"][:, :], xt[:, :], AF.Square, accum_out=ss[:, 0:1]), R=[r_xt], W=[nm["r_junk"], r_ss])
            pg.op("act", lambda e: e.activation(ss[:, 1:2], ss[:, 0:1], AF.Ln, scale=1.0 / D, bias=EPS), R=[r_ss], W=[r_ss])
            pg.op("act", lambda e: e.activation(ss[:, 1:2], ss[:, 1:2], AF.Exp, scale=-0.5), R=[r_ss], W=[r_ss])
            pg.op("dve", lambda e: e.tensor_scalar(xn[:, :], xt[:, :], ss[:, 1:2], None, ALU.mult), R=[r_xt, r_ss], W=[r_xn])
            for j in range(8):
                pg.op("pe", lambda e, j=j: e.transpose(psT[:, j * 128:(j + 1) * 128], xn[:, j * 128:(j + 1) * 128], identf[:, :]),
                      R=[r_xn, r_const], W=[r_psT])
            for j in range(8):
                ca = (w * 8 + j) * NB1 + bm; cbb = ((w + 1) * 8 + j) * NB1 + bm
                eng = "act" if j % 2 == 0 else "dve"
                dst = hT[:, j * T + i * 128: j * T + (i + 1) * 128]
                if eng == "act":
                    pg.op("act", lambda e, j=j, ca=ca, cbb=cbb, dst=dst: e.activation(dst, psT[:, j * 128:(j + 1) * 128], AF.Identity,
                                                                                    bias=MV[:, cbb:cbb + 1], scale=MV[:, ca:ca + 1]),
                          R=[r_psT, r_MV], W=[r_hT])
                else:
                    pg.op("dve", lambda e, j=j, ca=ca, cbb=cbb, dst=dst: e.tensor_scalar(dst, psT[:, j * 128:(j + 1) * 128], MV[:, ca:ca + 1],
                                                                                       MV[:, cbb:cbb + 1], ALU.mult, ALU.add),
                          R=[r_psT, r_MV], W=[r_hT])
            if keep_xnT is not None:
                kx, r_kx = keep_xnT
                pg.op("act", lambda e: e.activation(kx[:, :], psT[:, :], AF.Copy), R=[r_psT], W=[r_kx])

        def phaseB(l):
            with ExitStack() as st:
                norm_setup(st)
                xts = [sb("xt%d" % i, [128, 1024], F32, st) for i in range(2)]; r_xts = [RS("xt%d" % i) for i in range(2)]
                for i in range(NT):
                    xt, r_xt = xts[i % 2], r_xts[i % 2]
                    pg.dma("sp", xt[:, :], xres[i * 128:(i + 1) * 128, :], R=[r_xres[i]], W=[r_xt])
                    norm_mod_T(i, xt, r_xt, 0)

        def tok_chunks(maxn=512):
            out = []
            for b in range(nb):
                for (p0, n, isc) in ((0, LC, True), (LC, L, False)):
                    o = 0
                    while o < n:
                        m = min(maxn, n - o)
                        out.append((b, b * N + p0 + o, m, isc, o))
                        o += m
            return out

        SLOTS = [((C_NAQ, C_NAQ + 64), 0, False), ((C_NAQ + 128, C_NAQ + 192), 0, False),
                 ((C_NAK, C_NAK + 64), 1, False), ((C_NAK + 128, C_NAK + 192), 1, False),
                 ((C_SWQ, C_SWQ + 128), 2, True), ((C_SWQ + 64, C_SWQ + 192), 2, True),
                 ((C_SWK, C_SWK + 64), 3, True)]

        def phaseC(l):
            with ExitStack() as st:
                wqk = sb("wqk", [128, 8, 7 * 128], BF16, st); wv = sb("wv", [128, 8, 384], BF16, st)
                gt4 = sb("gt4", [128, 4], F32, st); r_w = RS("wqk"); r_g = RS("gt4")
                rC = sb("ropeC", [128, L], F32, st); rS = sb("ropeS", [128, L], F32, st); rP = sb("ropeP", [128, 128], BF16, st); r_rope = RS("rope")
                sq = [sb("c_sq%d" % i, [128, 512], BF16, st) for i in range(2)]; r_sq = [RS("c_sq%d" % i) for i in range(2)]
                rstd = [sb("c_rstd%d" % i, [128, 512], F32, st) for i in range(2)]; r_rstd = [RS("c_rstd%d" % i) for i in range(2)]
                qn = [sb("c_qn%d" % i, [128, 512], BF16, st) for i in range(2)]; r_qn = [RS("c_qn%d" % i) for i in range(2)]
                t1 = [sb("c_t1%d" % i, [128, 512], F32, st) for i in range(2)]; r_t1 = [RS("c_t1%d" % i) for i in range(2)]
                t2 = [sb("c_t2%d" % i, [128, 512], F32, st) for i in range(2)]; r_t2 = [RS("c_t2%d" % i) for i in range(2)]
                qo = [sb("c_qo%d" % i, [128, 512], BF16, st) for i in range(2)]; r_qo = [RS("c_qo%d" % i) for i in range(2)]
                vs = [sb("c_vs%d" % i, [128, 384], BF16, st) for i in range(2)]; r_vs = [RS("c_vs%d" % i) for i in range(2)]
                for s, (cols, gcol, rope) in enumerate(SLOTS):
                    for g, c0 in enumerate(cols):
                        pg.dma("pool", wqk[:, :, s * 128 + g * 64: s * 128 + (g + 1) * 64],
                               w_in[l, :, c0:c0 + 64].rearrange("(kc p) n -> p kc n", p=128), W=[r_w])
                for (c0, n, o) in ((C_NAV, 256, 0), (C_SWV, 128, 256)):
                    pg.dma("pool", wv[:, :, o:o + n], w_in[l, :, c0:c0 + n].rearrange("(kc p) n -> p kc n", p=128), W=[r_w])
                pg.dma("sp", gt4[:, :], gains4[l, :, :], W=[r_g])
                for c in (0, 2):
                    pg.op("dve", lambda e, c=c: e.tensor_scalar(gt4[:, c:c + 1], gt4[:, c:c + 1], HD ** -0.5, None, ALU.mult), R=[r_g], W=[r_g])
                pg.dma("sp", rC[:, :], ropeC_d[:, :], W=[r_rope]); pg.dma("sp", rS[:, :], ropeS_d[:, :], W=[r_rope])
                pg.dma("sp", stage[:, :], ropeP_d[:, :], W=[r_stage])
                pg.op("dve", lambda e: e.tensor_copy(rP[:, :], stage[:, :]), R=[r_stage], W=[r_rope])
                it = 0
                for (b, g0, n, isc, o) in tok_chunks():
                    for s, (cols, gcol, rope) in enumerate(SLOTS):
                        k = it % 2; it += 1
                        p, rp = nextpb()
                        for kc in range(8):
                            pg.op("pe", lambda e, p=p, kc=kc, s=s, g0=g0, n=n: e.matmul(
                                p[:, 0:n], wqk[:, kc, s * 128:(s + 1) * 128], hT[:, kc * T + g0: kc * T + g0 + n], start=(kc == 0), stop=(kc == 7)),
                                R=[r_w, r_hT], W=[rp])
                        pg.op("act", lambda e, p=p, k=k, n=n: e.activation(sq[k][:, 0:n], p[:, 0:n], AF.Square), R=[rp], W=[r_sq[k]])
                        p2, rp2 = nextpb()
                        pg.op("pe", lambda e, p2=p2, k=k, n=n: e.matmul(p2[:, 0:n], ones2[:, :], sq[k][:, 0:n], start=True, stop=True),
                              R=[r_sq[k], r_const], W=[rp2])
                        pg.op("act", lambda e, p2=p2, k=k, n=n: e.activation(rstd[k][:, 0:n], p2[:, 0:n], AF.Ln, scale=1.0 / HD, bias=EPS), R=[rp2], W=[r_rstd[k]])
                        pg.op("act", lambda e, k=k, n=n: e.activation(rstd[k][:, 0:n], rstd[k][:, 0:n], AF.Exp, scale=-0.5), R=[r_rstd[k]], W=[r_rstd[k]])
                        dorope = rope and not isc
                        dst, r_dst = (qn[k], r_qn[k]) if dorope else (qo[k], r_qo[k])
                        pg.op("dve", lambda e, p=p, k=k, n=n, dst=dst, gcol=gcol: e.scalar_tensor_tensor(
                            dst[:, 0:n], p[:, 0:n], gt4[:, gcol:gcol + 1], rstd[k][:, 0:n], ALU.mult, ALU.mult), R=[rp, r_g, r_rstd[k]], W=[r_dst])
                        if dorope:
                            p3, rp3 = nextpb()
                            pg.op("pe", lambda e, p3=p3, k=k, n=n: e.matmul(p3[:, 0:n], rP[:, :], qn[k][:, 0:n], start=True, stop=True),
                                  R=[r_qn[k], r_rope], W=[rp3])
                            pg.op("dve", lambda e, k=k, n=n, o=o: e.tensor_tensor(t1[k][:, 0:n], qn[k][:, 0:n], rC[:, o:o + n], ALU.mult),
                                  R=[r_qn[k], r_rope], W=[r_t1[k]])
                            pg.op("dve", lambda e, p3=p3, k=k, n=n, o=o: e.tensor_tensor(t2[k][:, 0:n], p3[:, 0:n], rS[:, o:o + n], ALU.mult),
                                  R=[rp3, r_rope], W=[r_t2[k]])
                            pg.op("pool", lambda e, k=k, n=n: e.tensor_tensor(qo[k][:, 0:n], t1[k][:, 0:n], t2[k][:, 0:n], ALU.add),
                                  R=[r_t1[k], r_t2[k]], W=[r_qo[k]])
                        pg.dma("sp", qk_d[s, :, g0:g0 + n], qo[k][:, 0:n], R=[r_qo[k]], W=[r_qk])
                for i in range(NT):
                    k = i % 2
                    p, rp = nextpb()
                    for kc in range(8):
                        pg.op("pe", lambda e, p=p, kc=kc, i=i: e.matmul(p[:, 0:384], hT[:, kc * T + i * 128: kc * T + (i + 1) * 128], wv[:, kc, :],
                                                                    start=(kc == 0), stop=(kc == 7)), R=[r_w, r_hT], W=[rp])
                    pg.op("act", lambda e, p=p, k=k: e.activation(vs[k][:, :], p[:, 0:384], AF.Copy), R=[rp], W=[r_vs[k]])
                    pg.dma("sp", v_d[i * 128:(i + 1) * 128, :], vs[k][:, :], R=[r_vs[k]], W=[r_v])

        at = {}

        def attn_setup(st):
            at["E"] = [sb("at_E%d" % i, [128, 512], BF16, st) for i in range(3)]; at["r_E"] = [RS("at_E%d" % i) for i in range(3)]
            at["rd"] = [sb("at_rd%d" % i, [64, 128], F32, st) for i in range(2)]; at["r_rd"] = [RS("at_rd%d" % i) for i in range(2)]
            at["ke"] = 0; at["kr"] = 0

        def attn_unit(hp, q_ap, keys, nq, dst, r_dst, Rq, sink_ap=None):
            per = 512 // nq
            groups = [keys[i:i + per] for i in range(0, len(keys), per)]
            Es = []
            for grp in groups:
                p, rp = nextpb()
                for t, (k_ap, b_ap, v_ap) in enumerate(grp):
                    pg.op("pe", lambda e, p=p, t=t, k_ap=k_ap, b_ap=b_ap: e.matmul(p[:, t * nq:(t + 1) * nq], k_ap, q_ap, start=True, stop=(b_ap is None)),
                          R=Rq, W=[rp])
                    if b_ap is not None:
                        pg.op("pe", lambda e, p=p, t=t, b_ap=b_ap: e.matmul(p[:, t * nq:(t + 1) * nq], identb[:, :], b_ap, start=False, stop=True),
                              R=Rq + [r_const], W=[rp])
                ke = at["ke"]; at["ke"] = (ke + 1) % 3
                E, rE = at["E"][ke], at["r_E"][ke]
                w = len(grp) * nq
                pg.op("act", lambda e, p=p, E=E, w=w: e.activation(E[:, 0:w], p[:, 0:w], AF.Exp), R=[rp], W=[rE])
                Es.append((E, rE, grp))
            po, rpo = nextpb()
            flat = [(E, rE, t, v_ap) for (E, rE, grp) in Es for t, (_, _, v_ap) in enumerate(grp)]
            for i, (E, rE, t, v_ap) in enumerate(flat):
                pg.op("pe", lambda e, E=E, t=t, i=i: e.matmul(po[0:64, nq:2 * nq], onesb[:, 0:64], E[:, t * nq:(t + 1) * nq], start=(i == 0), stop=(i == len(flat) - 1)),
                      R=[rE, r_const], W=[rpo])
            for i, (E, rE, t, v_ap) in enumerate(flat):
                pg.op("pe", lambda e, E=E, t=t, i=i, v_ap=v_ap: e.matmul(po[0:64, 0:nq], v_ap, E[:, t * nq:(t + 1) * nq], start=(i == 0), stop=(i == len(flat) - 1)),
                      R=[rE] + Rq, W=[rpo])
            kr = at["kr"]; at["kr"] = 1 - kr
            rd, r_rd = at["rd"][kr], at["r_rd"][kr]
            if sink_ap is not None:
                pg.op("dve", lambda e: e.tensor_scalar(rd[:, 0:nq], po[0:64, nq:2 * nq], sink_ap, None, ALU.add), R=[rpo] + Rq, W=[r_rd])
                pg.op("dve", lambda e: e.reciprocal(rd[:, 0:nq], rd[:, 0:nq]), R=[r_rd], W=[r_rd])
            else:
                pg.op("dve", lambda e: e.reciprocal(rd[:, 0:nq], po[0:64, nq:2 * nq]), R=[rpo], W=[r_rd])
            pg.op("dve", lambda e: e.tensor_tensor(dst, po[0:64, 0:nq], rd[:, 0:nq], ALU.mult), R=[rpo, r_rd], W=[r_dst])

        def phaseEF(l, last):
            with ExitStack() as st:
                attn_setup(st)
                QK = sb("QK", [128, 7, N], BF16, st); VE = sb("VE", [128, N // 128, 384], BF16, st); VO = sb("VO", [128, L // 128, 384], BF16, st)
                nabf = sb("nabf", [128, 14 * 64], F32, st); nabt = sb("nabt", [128, NA_H * 14 * 64], BF16, st)
                swmf = sb("swmf", [128, 256], F32, st); swm = sb("swm", [128, 256], BF16, st); esink = sb("esink", [128, SW_QH], F32, st)
                stg = [sb("stg%d" % i, [64, N], BF16, st) for i in range(2)]; r_stg = [RS("stg%d" % i) for i in range(2)]
                r_in = RS("attn_in"); r_tab = RS("attn_tab"); r_nabf = RS("nabf")
                for h in range(NA_H):
                    pg.dma("sp", nabf[:, :], nab_d[l, :, h * 896:(h + 1) * 896], W=[r_nabf])
                    pg.op("act", lambda e, h=h: e.activation(nabt[:, h * 896:(h + 1) * 896], nabf[:, :], AF.Copy), R=[r_nabf], W=[r_tab])
                pg.dma("sp", swmf[:, :], swam_d[:, :], W=[r_nabf])
                pg.op("act", lambda e: e.activation(swm[:, :], swmf[:, :], AF.Copy), R=[r_nabf], W=[r_tab])
                pg.dma("sp", esink[:, :], sink_d[l, :, :], W=[r_tab])
                pg.op("act", lambda e: e.activation(esink[:, :], esink[:, :], AF.Exp), R=[r_tab], W=[r_tab])
                Rq = [r_in, r_tab]
                ks = 0
                LT = LC // 128
                for b in range(nb):
                    g0 = b * N
                    for s in range(7):
                        pg.dma("sp", QK[:, s, :], qk_d[s, :, g0:g0 + N], R=[r_qk], W=[r_in])
                    pg.dma("sp", VE[:, :, :], v_d[g0:g0 + N, :].rearrange("(t p) c -> p t c", p=128), R=[r_v], W=[r_in])
                    pg.dma("sp", VO[:, 0:L // 128 - 1, :], v_d[g0 + LC + 64:g0 + LC + 64 + L - 128, :].rearrange("(t p) c -> p t c", p=128), R=[r_v], W=[r_in])
                    for h in range(NA_H):
                        sq_, sk_, hp = h // 2, 2 + h // 2, h % 2
                        ps0 = hp * 64
                        sg, r_sg = stg[ks % 2], r_stg[ks % 2]; ks += 1
                        ckeys = [(QK[ps0:ps0 + 64, sk_, t * 128:(t + 1) * 128], None, VE[:, t, h * 64:(h + 1) * 64]) for t in range(LT)]
                        if not last:
                            for qt in range(LT):
                                attn_unit(hp, QK[ps0:ps0 + 64, sq_, qt * 128:(qt + 1) * 128], ckeys, 128, sg[:, qt * 128:(qt + 1) * 128], r_sg, Rq)
                        for r in range(R):
                            s0 = min(max(r - 4, 0), R - 8)
                            keys = []
                            for t in range(4):
                                kr_ = s0 + 2 * t
                                pos = LC + 64 * kr_
                                d0 = (s0 - r + 7) + 2 * t
                                v_ap = VE[:, LT + kr_ // 2, h * 64:(h + 1) * 64] if kr_ % 2 == 0 else VO[:, (kr_ - 1) // 2, h * 64:(h + 1) * 64]
                                keys.append((QK[ps0:ps0 + 64, sk_, pos:pos + 128], nabt[:, (h * 14 + d0) * 64:(h * 14 + d0 + 1) * 64], v_ap))
                            attn_unit(hp, QK[ps0:ps0 + 64, sq_, LC + 64 * r: LC + 64 * (r + 1)], keys + ckeys, 64,
                                      sg[:, LC + 64 * r: LC + 64 * (r + 1)], r_sg, Rq)
                        c0 = 0 if not last else LC
                        pg.dma("sp", yT_d[h * 64:(h + 1) * 64, g0 + c0:g0 + N], sg[:, c0:N], R=[r_sg], W=[r_yT])
                    for hq in range(SW_QH):
                        sq_, half, kv = 4 + hq % 2, hq // 2, hq // 2
                        ps0 = half * 64
                        vc0 = 256 + kv * 64
                        sg, r_sg = stg[ks % 2], r_stg[ks % 2]; ks += 1
                        ckeys = [(QK[ps0:ps0 + 64, 6, t * 128:(t + 1) * 128], None, VE[:, t, vc0:vc0 + 64]) for t in range(LT)]
                        sk_ap = esink[0:64, hq:hq + 1]
                        if not last:
                            for qt in range(LT):
                                attn_unit(half, QK[ps0:ps0 + 64, sq_, qt * 128:(qt + 1) * 128], ckeys, 128, sg[:, qt * 128:(qt + 1) * 128], r_sg, Rq, sk_ap)
                        nblk = L // 128
                        for n in range(nblk):
                            keys = []
                            for (bk, m0) in ((n - 1, 0), (n, None), (n + 1, 128)):
                                if 0 <= bk < nblk:
                                    pos = LC + 128 * bk
                                    keys.append((QK[ps0:ps0 + 64, 6, pos:pos + 128], None if m0 is None else swm[:, m0:m0 + 128], VE[:, LT + bk, vc0:vc0 + 64]))
                            attn_unit(half, QK[ps0:ps0 + 64, sq_, LC + 128 * n: LC + 128 * (n + 1)], keys + ckeys, 128,
                                      sg[:, LC + 128 * n: LC + 128 * (n + 1)], r_sg, Rq, sk_ap)
                        c0 = 0 if not last else LC
                        pg.dma("sp", yT_d[768 + hq * 64:768 + (hq + 1) * 64, g0 + c0:g0 + N], sg[:, c0:N], R=[r_sg], W=[r_yT])

        def phaseG(l, last):
            CH = 16
            NCH = N // CH
            LCH = LC // CH
            with ExitStack() as st:
                whg = [sb("whg%d" % i, [128, 8, 640], BF16, st) for i in range(2)]; r_whg = [RS("whg%d" % i) for i in range(2)]
                qs = sb("g_qs", [128, N], BF16, st); kd = [sb("g_k%d" % i, [128, N], BF16, st) for i in range(2)]
                Pd = [sb("g_P%d" % i, [128, N + 1], F32, st) for i in range(2)]; nPd = [sb("g_nP%d" % i, [128, NCH + 1], F32, st) for i in range(2)]
                sgate = sb("g_sg", [128, N], BF16, st)
                vch = [sb("g_vch%d" % i, [CH, 128], BF16, st) for i in range(4)]; r_vch = [RS("g_vch%d" % i) for i in range(4)]
                mUL = sb("g_mUL", [64, 128], F32, st); hlb = sb("g_hlb", [128, dp * HG_H], F32, st); lbt = sb("g_lbt", [128, 2 * HG_H], F32, st)
                gn = sb("g_gn", [128, 1], F32, st)
                tmpa = [sb("g_ta%d" % i, [128, 512], F32, st) for i in range(2)]; r_tmpa = [RS("g_ta%d" % i) for i in range(2)]
                tmpb = [sb("g_tb%d" % i, [128, 512], F32, st) for i in range(2)]; r_tmpb = [RS("g_tb%d" % i) for i in range(2)]
                sqb = [sb("g_sqb%d" % i, [128, 512], BF16, st) for i in range(2)]; r_sqb = [RS("g_sqb%d" % i) for i in range(2)]
                yo = [sb("g_yo%d" % i, [128, 512], BF16, st) for i in range(2)]; r_yo = [RS("g_yo%d" % i) for i in range(2)]
                ex = [[sb("g_e%d%d" % (j, i), [128, CH], F32, st) for i in range(4)] for j in range(3)]
                r_ex = [[RS("g_e%d%d" % (j, i)) for i in range(4)] for j in range(3)]
                oacc2 = [sb("g_oacc2%d" % i, [128, N], F32, st) for i in range(2)]; r_oacc2 = [RS("g_oacc2%d" % i) for i in range(2)]
                r_pbf2 = [RS("pbf2%d" % i) for i in range(2)]
                oacc = oacc2[0]
                lf = oacc2[1]
                qt = [sb("g_qt%d" % i, [128, CH], BF16, st) for i in range(4)]; r_qt = [RS("g_qt%d" % i) for i in range(4)]
                kt = [sb("g_kt%d" % i, [128, CH], BF16, st) for i in range(4)]; r_kt = [RS("g_kt%d" % i) for i in range(4)]
                kh = [sb("g_kh%d" % i, [128, CH], BF16, st) for i in range(4)]; r_kh = [RS("g_kh%d" % i) for i in range(4)]
                Am = [sb("g_Am%d" % i, [CH, CH], BF16, st) for i in range(4)]; r_Am = [RS("g_Am%d" % i) for i in range(4)]
                khT = [sb("g_khT%d" % i, [CH, 128], BF16, st) for i in range(4)]; r_khT = [RS("g_khT%d" % i) for i in range(4)]
                Sf = [sb("g_S%d" % i, [128, 128], F32, st) for i in range(4)]; r_Sf = [RS("g_S%d" % i) for i in range(4)]
                Sb_ = [sb("g_Sb%d" % i, [128, 128], BF16, st) for i in range(4)]; r_Sb = [RS("g_Sb%d" % i) for i in range(4)]
                r_qs = RS("g_qs"); r_k = [RS("g_k0"), RS("g_k1")]; r_P = [RS("g_P0"), RS("g_P1")]; r_sgt = RS("g_sg")
                r_c = RS("g_const")
                pg.dma("sp", mUL[:, :], maskUL_d[:, :], W=[r_c])
                pg.dma("sp", hlb[:, :], hlbT_d[:, :], W=[r_c])
                pg.dma("sp", gn[:, :], hgn_d[l, :, :], W=[r_c])
                if l == 0:
                    pg.op("dve", lambda e: e.memset(lbt[:, 0:HG_H], 0.0), R=[r_c], W=[r_c])
                else:
                    pg.op("dve", lambda e: e.tensor_tensor(lbt[:, 0:HG_H], hlb[:, l * HG_H:(l + 1) * HG_H], hlb[:, 0:HG_H], ALU.subtract), R=[r_c], W=[r_c])
                    pg.op("act", lambda e: e.activation(lbt[:, 0:HG_H], lbt[:, 0:HG_H], AF.Sigmoid), R=[r_c], W=[r_c])
                pg.op("dve", lambda e: e.tensor_scalar(lbt[:, HG_H:2 * HG_H], lbt[:, 0:HG_H], -1.0, 1.0, ALU.mult, ALU.add), R=[r_c], W=[r_c])
                it = {"a": 0, "c": 0}
                for h in range(HG_H):
                    wt, rw = whg[h % 2], r_whg[h % 2]
                    for j, c0 in enumerate((C_HGQ, C_HGFF, C_HGFB, C_HGI, C_HGG)):
                        pg.dma("pool", wt[:, :, j * 128:(j + 1) * 128], w_in[l, :, c0 + h * 128:c0 + (h + 1) * 128].rearrange("(kc p) n -> p kc n", p=128), W=[rw])
                    for b in range(nb):
                        g0 = b * N
                        chunks = [(o, min(512, N - o)) for o in range(0, N, 512)]

                        def proj(j, o, n):
                            p, rp = nextpb()
                            for kc in range(8):
                                pg.op("pe", lambda e, p=p, kc=kc: e.matmul(p[:, 0:n], wt[:, kc, j * 128:(j + 1) * 128], hT[:, kc * T + g0 + o: kc * T + g0 + o + n],
                                                                         start=(kc == 0), stop=(kc == 7)), R=[rw, r_hT], W=[rp])
                            return p, rp
                        for (o, n) in chunks:
                            p, rp = proj(0, o, n)
                            pg.op("act", lambda e, p=p, o=o, n=n: e.activation(qs[:, o:o + n], p[:, 0:n], AF.Silu), R=[rp], W=[r_qs])
                            p, rp = proj(4, o, n)
                            pg.op("act", lambda e, p=p, o=o, n=n: e.activation(sgate[:, o:o + n], p[:, 0:n], AF.Silu), R=[rp], W=[r_sgt])
                        for dirn in range(2):
                            kk, rk, P, rP, nP = kd[dirn], r_k[dirn], Pd[dirn], r_P[dirn], nPd[dirn]
                            for (o, n) in chunks:
                                p, rp = proj(1 + dirn, o, n)
                                a = it["a"] % 2; it["a"] += 1
                                pg.op("act", lambda e, p=p, a=a, n=n: e.activation(tmpa[a][:, 0:n], p[:, 0:n], AF.Sigmoid), R=[rp], W=[r_tmpa[a]])
                                pg.op("dve", lambda e, a=a, n=n: e.tensor_scalar(tmpb[a][:, 0:n], tmpa[a][:, 0:n], lbt[:, HG_H + h:HG_H + h + 1], lbt[:, h:h + 1], ALU.mult, ALU.add),
                                      R=[r_tmpa[a], r_c], W=[r_tmpb[a]])
                                pg.op("act", lambda e, a=a, o=o, n=n: e.activation(lf[:, o:o + n], tmpb[a][:, 0:n], AF.Ln), R=[r_tmpb[a]], W=[r_oacc2[1]])
                                pg.op("dve", lambda e, a=a, o=o, n=n, kk=kk: e.tensor_scalar(kk[:, o:o + n], tmpb[a][:, 0:n], -1.0, 1.0, ALU.mult, ALU.add),
                                      R=[r_tmpb[a]], W=[rk])
                            pg.op("dve", lambda e, P=P: e.memset(P[:, 0:1], 0.0), W=[rP])
                            pg.op("dve", lambda e, P=P: e.tensor_tensor_scan(P[:, 1:N + 1], lf[:, :], lf[:, :], 0.0, ALU.add, ALU.bypass), R=[r_oacc2[1], r_c], W=[rP])
                            pg.op("dve", lambda e, P=P, nP=nP: e.tensor_scalar(nP[:, :], P[:, 0:N + 1:CH], -1.0, None, ALU.mult), R=[rP], W=[rP])
                        orders = [list(range(NCH)), list(range(LCH - 1, -1, -1)) + list(range(NCH - 1, LCH - 1, -1))]
                        stt_ = [{"sc": 0, "n": 0}, {"sc": 0, "n": 0}]
                        for dirn in range(2):
                            pg.op("dve", lambda e: e.memset(Sf[2 * dirn][:, :], 0.0), W=[r_Sf[2 * dirn]])
                            pg.op("dve", lambda e: e.memset(Sb_[2 * dirn][:, :], 0.0), W=[r_Sb[2 * dirn]])

                        def step(dirn, c):
                            kk, rk, P, rP, nP = kd[dirn], r_k[dirn], Pd[dirn], r_P[dirn], nPd[dirn]
                            sd = stt_[dirn]
                            sc = 2 * dirn + sd["sc"]; sn = 2 * dirn + 1 - sd["sc"]
                            z = 2 * dirn + sd["n"] % 2; sd["n"] += 1
                            a0 = c * CH
                            need_out = not (last and c < LCH)
                            pV, rpV = nextpb()
                            for kc in range(8):
                                pg.op("pe", lambda e: e.matmul(pV[0:CH, 0:128], hT[:, kc * T + g0 + a0: kc * T + g0 + a0 + CH], wt[:, kc, 384:512], start=(kc == 0), stop=(kc == 7)),
                                      R=[rw, r_hT], W=[rpV])
                            pg.op("act", lambda e: e.activation(vch[z][:, :], pV[0:CH, 0:128], AF.Copy), R=[rpV], W=[r_vch[z]])
                            if dirn == 0:
                                src = P[:, a0 + 1:a0 + CH + 1]
                                specs = ((1.0, nP[:, c:c + 1]), (-1.0, P[:, a0:a0 + 1]), (-1.0, P[:, a0 + CH:a0 + CH + 1]))
                                ebe = ex[0][z][:, CH - 1:CH]; mk = mUL[0:CH, 0:CH]
                            else:
                                src = P[:, a0:a0 + CH]
                                specs = ((-1.0, P[:, a0 + CH:a0 + CH + 1]), (1.0, nP[:, c + 1:c + 2]), (1.0, nP[:, c:c + 1]))
                                ebe = ex[0][z][:, 0:1]; mk = mUL[0:CH, 64:64 + CH]
                            for j, (scl, bias) in enumerate(specs):
                                if j == 1 and not need_out:
                                    continue
                                pg.op("act", lambda e: e.activation(ex[j][z][:, :], src, AF.Exp, bias=bias, scale=scl), R=[rP], W=[r_ex[j][z]])
                            pg.op("dve", lambda e: e.scalar_tensor_tensor(qt[z][:, :], qs[:, a0:a0 + CH], HG_DK ** -0.5, ex[0][z][:, :], ALU.mult, ALU.mult),
                                  R=[r_qs, r_ex[0][z]], W=[r_qt[z]])
                            pg.op("dve", lambda e: e.tensor_tensor(kh[z][:, :], kk[:, a0:a0 + CH], ex[2][z][:, :], ALU.mult), R=[rk, r_ex[2][z]], W=[r_kh[z]])
                            if need_out:
                                pg.op("dve", lambda e: e.tensor_tensor(kt[z][:, :], kk[:, a0:a0 + CH], ex[1][z][:, :], ALU.mult), R=[rk, r_ex[1][z]], W=[r_kt[z]])
                                pA, rpA = nextpb()
                                pg.op("pe", lambda e: e.matmul(pA[0:CH, 0:CH], kt[z][:, :], qt[z][:, :], start=True, stop=True), R=[r_kt[z], r_qt[z]], W=[rpA])
                                pg.op("dve", lambda e: e.tensor_tensor(Am[z][:, :], pA[0:CH, 0:CH], mk, ALU.mult), R=[rpA, r_c], W=[r_Am[z]])
                            pbv = pbf[0:CH, dirn * 128:(dirn + 1) * 128]
                            pg.op("pe", lambda e: e.transpose(pbv, kh[z][:, :], identb[:, :]), R=[r_kh[z], r_const], W=[r_pbf2[dirn]])
                            pg.op("act", lambda e: e.activation(khT[z][:, :], pbv, AF.Copy), R=[r_pbf2[dirn]], W=[r_khT[z]])
                            if need_out:
                                pO, rpO = nextpb()
                                pg.op("pe", lambda e: e.matmul(pO[:, 0:CH], vch[z][:, :], Am[z][:, :], start=True, stop=False), R=[r_vch[z], r_Am[z]], W=[rpO])
                                pg.op("pe", lambda e: e.matmul(pO[:, 0:CH], Sb_[sc][:, :], qt[z][:, :], start=False, stop=True), R=[r_Sb[sc], r_qt[z]], W=[rpO])
                                pg.op("dve", lambda e: e.tensor_copy(oacc2[dirn][:, a0:a0 + CH], pO[:, 0:CH]), R=[rpO], W=[r_oacc2[dirn]])
                            pS, rpS = nextpb()
                            pg.op("pe", lambda e: e.matmul(pS[:, 0:128], khT[z][:, :], vch[z][:, :], start=True, stop=True), R=[r_khT[z], r_vch[z]], W=[rpS])
                            pg.op("dve", lambda e: e.scalar_tensor_tensor(Sf[sn][:, :], Sf[sc][:, :], ebe, pS[:, 0:128], ALU.mult, ALU.add),
                                  R=[r_Sf[sc], rpS, r_ex[0][z]], W=[r_Sf[sn]])
                            pg.op("act", lambda e: e.activation(Sb_[sn][:, :], Sf[sn][:, :], AF.Copy), R=[r_Sf[sn]], W=[r_Sb[sn]])
                            sd["sc"] = 1 - sd["sc"]

                        for idx in range(NCH):
                            step(0, orders[0][idx])
                            step(1, orders[1][idx])
                        r0 = LC if last else 0
                        pg.op("pool", lambda e: e.tensor_tensor(oacc[:, r0:N], oacc2[0][:, r0:N], oacc2[1][:, r0:N], ALU.add), R=[r_oacc2[1]], W=[r_oacc2[0]])
                        r0 = LC if last else 0
                        for ci, (o, n) in enumerate([(o, min(512, N - o)) for o in range(r0, N, 512)]):
                            a = ci % 2
                            pg.op("act", lambda e, a=a, o=o, n=n: e.activation(sqb[a][:, 0:n], oacc[:, o:o + n], AF.Square), R=[r_oacc2[0]], W=[r_sqb[a]])
                            p, rp = nextpb()
                            pg.op("pe", lambda e, p=p, a=a, n=n: e.matmul(p[:, 0:n], onesb[:, :], sqb[a][:, 0:n], start=True, stop=True), R=[r_sqb[a], r_const], W=[rp])
                            pg.op("act", lambda e, p=p, a=a, n=n: e.activation(tmpa[a][:, 0:n], p[:, 0:n], AF.Ln, scale=1.0 / 128, bias=EPS), R=[rp], W=[r_tmpa[a]])
                            pg.op("act", lambda e, a=a, n=n: e.activation(tmpa[a][:, 0:n], tmpa[a][:, 0:n], AF.Exp, scale=-0.5), R=[r_tmpa[a]], W=[r_tmpa[a]])
                            pg.op("dve", lambda e, a=a, o=o, n=n: e.scalar_tensor_tensor(tmpb[a][:, 0:n], oacc[:, o:o + n], gn[:, 0:1], tmpa[a][:, 0:n], ALU.mult, ALU.mult),
                                  R=[r_oacc2[0], r_tmpa[a], r_c], W=[r_tmpb[a]])
                            pg.op("dve", lambda e, a=a, o=o, n=n: e.tensor_tensor(yo[a][:, 0:n], tmpb[a][:, 0:n], sgate[:, o:o + n], ALU.mult), R=[r_tmpb[a], r_sgt], W=[r_yo[a]])
                            pg.dma("sp", yT_d[256 + h * 128:256 + (h + 1) * 128, g0 + o:g0 + o + n], yo[a][:, 0:n], R=[r_yo[a]], W=[r_yT])

        Gall_d = nc.dram_tensor("Gall_d", [128, NT * NE], F32); GT_d = nc.dram_tensor("GT_d", [NE, T], BF16)
        r_Gall = RS("Gall"); r_GT = RS("GT"); r_Gd = RS("G_d")

        def phaseH(l, last):
            with ExitStack() as st:
                norm_setup(st)
                Gall = sb("Gall", [128, NT * NE], F32, st); GT = sb("GT", [NE, T], BF16, st)
                pg.op("dve", lambda e: e.memset(Gall[:, :], 0.0), W=[r_Gall])
                pg.op("dve", lambda e: e.memset(GT[:, :], 0.0), W=[r_GT])
                wo = sb("wo", [128, 8, 1024], BF16, st); r_wo = RS("wo")
                Wr = sb("Wr", [128, 8, NE], F32, st); Wrp = sb("Wrp", [128, NB1 * 8 * NE], F32, st); rbt = sb("rbt", [1, NE], F32, st)
                rbias = sb("rbias", [1, NB1 * NE], F32, st); r_r = RS("router")
                G1t = sb("G1t", [128, NB1, 1024], F32, st); r_G1 = RS("G1t")
                yts = [sb("h_yt%d" % i, [128, 8, 128], BF16, st) for i in range(2)]; r_yts = [RS("h_yt%d" % i) for i in range(2)]
                xts = [sb("h_xt%d" % i, [128, 1024], F32, st) for i in range(2)]; r_xts = [RS("h_xt%d" % i) for i in range(2)]
                tmp = [sb("h_tmp%d" % i, [128, 512], F32, st) for i in range(2)]; r_tmp = [RS("h_tmp%d" % i) for i in range(2)]
                xnT = sb("h_xnT", [128, 1024], F32, st); r_xnT = RS("h_xnT")
                lg = [sb("h_lg%d" % i, [128, NE], F32, st) for i in range(2)]; r_lg = [RS("h_lg%d" % i) for i in range(2)]
                m8 = [sb("h_m8%d" % i, [128, 16], F32, st) for i in range(2)]
                ee = [sb("h_ee%d" % i, [128, 2 * NE], F32, st) for i in range(2)]
                for kc in range(8):
                    pg.dma("pool", wo[:, kc, :], w_out[l, kc * 128:(kc + 1) * 128, :], W=[r_wo])
                pg.dma("sp", Wr[:, :, :], rw_d[l, :, :].rearrange("(kc p) n -> p kc n", p=128), W=[r_r])
                pg.dma("sp", rbt[:, :], rb_d[l, :, :], W=[r_r])
                for b in range(NB1):
                    pg.dma("sp", G1t[:, b, :], Gt_d[b * 2 + 0, :, :], R=[r_Gt], W=[r_G1])
                    for kc in range(8):
                        ca = (2 * 8 + kc) * NB1 + b
                        pg.op("dve", lambda e: e.tensor_scalar(Wrp[:, (b * 8 + kc) * NE:(b * 8 + kc + 1) * NE], Wr[:, kc, :], MV[:, ca:ca + 1], None, ALU.mult),
                              R=[r_r, r_MV], W=[r_r])
                    p, rp = nextpb()
                    for kc in range(8):
                        cb_ = (3 * 8 + kc) * NB1 + b
                        pg.op("pe", lambda e: e.matmul(p[0:1, 0:NE], MV[:, cb_:cb_ + 1], Wr[:, kc, :], start=(kc == 0), stop=False), R=[r_r, r_MV], W=[rp])
                    pg.op("pe", lambda e: e.matmul(p[0:1, 0:NE], onesf[0:1, 0:1], rbt[0:1, :], start=False, stop=True), R=[r_r, r_const], W=[rp])
                    pg.op("act", lambda e: e.activation(rbias[0:1, b * NE:(b + 1) * NE], p[0:1, 0:NE], AF.Copy), R=[rp], W=[r_r])
                tiles = [i for i in range(NT) if not (last and tile_info(i)[2])]
                for n_, i in enumerate(tiles):
                    b, pos0, isc = tile_info(i)
                    bm = nb if isc else b
                    k = n_ % 2
                    yt, r_yt, xt, r_xt = yts[k], r_yts[k], xts[k], r_xts[k]
                    pg.dma("sp", yt[:, :, :], yT_d[:, i * 128:(i + 1) * 128].rearrange("(kc p) t -> p kc t", p=128), R=[r_yT], W=[r_yt])
                    pg.dma("sp", xt[:, :], xres[i * 128:(i + 1) * 128, :], R=[r_xres[i]], W=[r_xt])
                    for half in range(2):
                        p, rp = nextpb()
                        for kc in range(8):
                            pg.op("pe", lambda e: e.matmul(p[:, :], yt[:, kc, :], wo[:, kc, half * 512:(half + 1) * 512], start=(kc == 0), stop=(kc == 7)),
                                  R=[r_yt, r_wo], W=[rp])
                        pg.op("dve", lambda e: e.tensor_tensor(tmp[half][:, :], p[:, :], G1t[:, bm, half * 512:(half + 1) * 512], ALU.mult), R=[rp, r_G1], W=[r_tmp[half]])
                        pg.op("pool", lambda e: e.tensor_tensor(xt[:, half * 512:(half + 1) * 512], xt[:, half * 512:(half + 1) * 512], tmp[half][:, :], ALU.add),
                              R=[r_tmp[half], r_xt], W=[r_xt])
                    pg.dma("sp", xres[i * 128:(i + 1) * 128, :], xt[:, :], R=[r_xt], W=[r_xres[i]])
                    norm_mod_T(i, xt, r_xt, 2, keep_xnT=(xnT, r_xnT))
                    p, rp = nextpb()
                    for kc in range(8):
                        pg.op("pe", lambda e: e.matmul(p[:, 0:NE], xnT[:, kc * 128:(kc + 1) * 128], Wrp[:, (bm * 8 + kc) * NE:(bm * 8 + kc + 1) * NE], start=(kc == 0), stop=False),
                              R=[r_xnT, r_r], W=[rp])
                    pg.op("pe", lambda e: e.matmul(p[:, 0:NE], onesf[0:1, :], rbias[0:1, bm * NE:(bm + 1) * NE], start=False, stop=True), R=[r_r, r_const], W=[rp])
                    L_, rL = lg[k], r_lg[k]
                    M_, E_ = m8[k], ee[k]
                    pg.op("act", lambda e: e.activation(L_[:, :], p[:, 0:NE], AF.Copy), R=[rp], W=[rL])
                    pg.op("dve", lambda e: e.max(M_[:, 0:8], L_[:, :]), R=[rL], W=[rL])
                    pg.op("dve", lambda e: e.tensor_scalar(M_[:, 8:9], M_[:, 0:1], -1.0, None, ALU.mult), R=[rL], W=[rL])
                    pg.op("act", lambda e: e.activation(E_[:, 0:NE], L_[:, :], AF.Exp, bias=M_[:, 8:9], scale=1.0), R=[rL], W=[rL])
                    pg.op("dve", lambda e: e.tensor_scalar(E_[:, NE:2 * NE], L_[:, :], M_[:, TOPK - 1:TOPK], None, ALU.is_ge), R=[rL], W=[rL])
                    pg.op("dve", lambda e: e.tensor_tensor(E_[:, 0:NE], E_[:, 0:NE], E_[:, NE:2 * NE], ALU.mult), R=[rL], W=[rL])
                    pg.op("dve", lambda e: e.reduce_sum(M_[:, 9:10], E_[:, 0:NE], AX.X), R=[rL], W=[rL])
                    pg.op("dve", lambda e: e.reciprocal(M_[:, 9:10], M_[:, 9:10]), R=[rL], W=[rL])
                    pg.op("dve", lambda e: e.tensor_scalar(Gall[:, i * NE:(i + 1) * NE], E_[:, 0:NE], M_[:, 9:10], None, ALU.mult), R=[rL], W=[r_Gall])
                    p2, rp2 = nextpb()
                    pg.op("pe", lambda e: e.transpose(p2[0:NE, 0:128], Gall[:, i * NE:(i + 1) * NE], identf[:, :]), R=[r_Gall, r_const], W=[rp2])
                    pg.op("act", lambda e: e.activation(GT[:, i * 128:(i + 1) * 128], p2[0:NE, 0:128], AF.Copy), R=[rp2], W=[r_GT])
                for kc in range(8):
                    pg.dma("sp", h2T_d[:, kc, :], hT[:, kc * T:(kc + 1) * T], R=[r_hT], W=[r_h2])
                pg.dma("sp", Gall_d[:, :], Gall[:, :], R=[r_Gall], W=[r_Gd])
                pg.dma("sp", GT_d[:, :], GT[:, :], R=[r_GT], W=[r_Gd])

        def phaseI(l, last):
            TGT = cfg.TG // 128
            banks = [(pb[i], r_pb[i]) for i in range(5)] + [(psT[:, 0:512], RS("psTa")), (psT[:, 512:1024], RS("psTb"))]
            rotI = [0]

            def nextI():
                i = rotI[0]; rotI[0] = (i + 1) % len(banks)
                return banks[i]
            with ExitStack() as st:
                Gall = sb("Gall", [128, NT * NE], F32, st); GT = sb("GT", [NE, T], BF16, st)
                pg.dma("sp", Gall[:, :], Gall_d[:, :], R=[r_Gd], W=[r_Gall])
                pg.dma("sp", GT[:, :], GT_d[:, :], R=[r_Gd], W=[r_GT])
                acc = sb("i_acc", [128, TGT, 1024], F32, st); r_acc = [RS("i_acc%d" % j) for j in range(TGT)]
                h2g = sb("i_h2g", [128, 8, cfg.TG], BF16, st); r_h2g = RS("i_h2g")
                actT = sb("i_actT", [128, FC, cfg.TG], BF16, st); r_actT = RS("i_actT")
                wd = [sb("i_wd%d" % i, [128, FC, 1024], BF16, st) for i in range(2)]; r_wd = [RS("i_wd%d" % i) for i in range(2)]
                wgp = [sb("i_wg%d" % i, [128, 8, 256], BF16, st) for i in range(4)]; r_wgp = [RS("i_wg%d" % i) for i in range(4)]
                bgu = sb("i_bgu", [128, NE * 2 * FC], F32, st); bdf = sb("i_bdf", [NE, 1024], F32, st); bdb = sb("i_bdb", [NE, 1024], BF16, st); r_b = RS("i_bias")
                G2t = sb("i_G2t", [128, NB1, 1024], F32, st); r_G2 = RS("i_G2t")
                tg = [[sb("i_t%d%d" % (j, i), [128, 512], F32, st) for i in range(2)] for j in range(5)]
                r_tg = [[RS("i_t%d%d" % (j, i)) for i in range(2)] for j in range(5)]
                xts = [sb("i_xt%d" % i, [128, 1024], F32, st) for i in range(2)]; r_xts = [RS("i_xt%d" % i) for i in range(2)]
                pg.dma("sp", bgu[:, :], bguT_d[l, :, :], W=[r_b])
                pg.dma("sp", bdf[:, :], bdn_d[l, :, :], W=[r_b])
                pg.op("act", lambda e: e.activation(bdb[:, :], bdf[:, :], AF.Copy), R=[r_b], W=[r_b])
                for b in range(NB1):
                    pg.dma("sp", G2t[:, b, :], Gt_d[b * 2 + 1, :, :], R=[r_Gt], W=[r_G2])
                tiles = [i for i in range(NT) if not (last and tile_info(i)[2])]
                groups = [tiles[i:i + TGT] for i in range(0, len(tiles), TGT)]
                cnt = {"w": 0, "d": 0, "t": 0, "x": 0}
                for grp in groups:
                    ng = len(grp); ntok = ng * 128
                    for j, i in enumerate(grp):
                        pg.dma("sp", h2g[:, :, j * 128:(j + 1) * 128], h2T_d[:, :, i * 128:(i + 1) * 128], R=[r_h2], W=[r_h2g])
                        for half in range(2):
                            p, rp = nextI()
                            pg.op("pe", lambda e: e.matmul(p[:, :], GT[:, i * 128:(i + 1) * 128], bdb[:, half * 512:(half + 1) * 512], start=True, stop=True),
                                  R=[r_GT, r_b], W=[rp])
                            pg.op("act", lambda e: e.activation(acc[:, j, half * 512:(half + 1) * 512], p[:, :], AF.Copy), R=[rp], W=[r_acc[j]])
                    tts = [(o, min(512, ntok - o)) for o in range(0, ntok, 512)]
                    for ex_ in range(NE):
                        wdt, rwd = wd[cnt["d"] % 2], r_wd[cnt["d"] % 2]; cnt["d"] += 1
                        for fc in range(FC):
                            pg.dma("pool", wdt[:, fc, :], wdn_d[l, ex_, fc * 128:(fc + 1) * 128, :], W=[rwd])
                        for fc in range(FC):
                            wg, rwg = wgp[cnt["w"] % 4], r_wgp[cnt["w"] % 4]; cnt["w"] += 1
                            for (c0, o_) in ((fc * 128, 0), (DFF + fc * 128, 128)):
                                pg.dma("pool", wg[:, :, o_:o_ + 128], wgu_d[l, ex_, :, c0:c0 + 128].rearrange("(kc p) n -> p kc n", p=128), W=[rwg])
                            cg = ex_ * 2 * FC + fc; cl = ex_ * 2 * FC + FC + fc
                            for (o, n) in tts:
                                z = cnt["t"] % 2; cnt["t"] += 1
                                pgl, rpgl = nextI()
                                for kc in range(8):
                                    pg.op("pe", lambda e: e.matmul(pgl[:, 0:n], wg[:, kc, 0:128], h2g[:, kc, o:o + n], start=(kc == 0), stop=(kc == 7)), R=[rwg, r_h2g], W=[rpgl])
                                pli, rpli = nextI()
                                for kc in range(8):
                                    pg.op("pe", lambda e: e.matmul(pli[:, 0:n], wg[:, kc, 128:256], h2g[:, kc, o:o + n], start=(kc == 0), stop=(kc == 7)), R=[rwg, r_h2g], W=[rpli])
                                glu, sg_, linb, linc, tt_ = (tg[j][z] for j in range(5))
                                rglu, rsg, rlinb, rlinc, rtt = (r_tg[j][z] for j in range(5))
                                pg.op("dve", lambda e: e.tensor_scalar(glu[:, 0:n], pgl[:, 0:n], bgu[:, cg:cg + 1], 7.0, ALU.add, ALU.min), R=[rpgl, r_b], W=[rglu])
                                pg.op("act", lambda e: e.activation(sg_[:, 0:n], glu[:, 0:n], AF.Sigmoid, scale=1.702), R=[rglu], W=[rsg])
                                pg.op("act", lambda e: e.activation(linb[:, 0:n], pli[:, 0:n], AF.Identity, bias=bgu[:, cl:cl + 1], scale=1.0), R=[rpli, r_b], W=[rlinb])
                                pg.op("pool", lambda e: e.tensor_scalar(linc[:, 0:n], linb[:, 0:n], 7.0, -7.0, ALU.min, ALU.max), R=[rlinb], W=[rlinc])
                                pg.op("dve", lambda e: e.tensor_tensor(tt_[:, 0:n], glu[:, 0:n], sg_[:, 0:n], ALU.mult), R=[rglu, rsg], W=[rtt])
                                pg.op("dve", lambda e: e.scalar_tensor_tensor(actT[:, fc, o:o + n], linc[:, 0:n], 1.0, tt_[:, 0:n], ALU.add, ALU.mult), R=[rlinc, rtt], W=[r_actT])
                        for j, i in enumerate(grp):
                            for half in range(2):
                                p, rp = nextI()
                                for fc in range(FC):
                                    pg.op("pe", lambda e: e.matmul(p[:, :], actT[:, fc, j * 128:(j + 1) * 128], wdt[:, fc, half * 512:(half + 1) * 512], start=(fc == 0), stop=(fc == FC - 1)),
                                          R=[r_actT, rwd], W=[rp])
                                pg.op("dve", lambda e: e.scalar_tensor_tensor(acc[:, j, half * 512:(half + 1) * 512], p[:, :], Gall[:, i * NE + ex_: i * NE + ex_ + 1],
                                                                            acc[:, j, half * 512:(half + 1) * 512], ALU.mult, ALU.add), R=[rp, r_Gall, r_acc[j]], W=[r_acc[j]])
                    for j, i in enumerate(grp):
                        b, pos0, isc = tile_info(i)
                        bm = nb if isc else b
                        k = cnt["x"] % 2; cnt["x"] += 1
                        xt, r_xt = xts[k], r_xts[k]
                        pg.dma("sp", xt[:, :], xres[i * 128:(i + 1) * 128, :], R=[r_xres[i]], W=[r_xt])
                        pg.op("dve", lambda e: e.tensor_tensor(acc[:, j, :], acc[:, j, :], G2t[:, bm, :], ALU.mult), R=[r_acc[j], r_G2], W=[r_acc[j]])
                        pg.op("pool", lambda e: e.tensor_tensor(xt[:, :], xt[:, :], acc[:, j, :], ALU.add), R=[r_acc[j], r_xt], W=[r_xt])
                        if last:
                            row = b * L + (pos0 - LC)
                            pg.dma("sp", y_d[row:row + 128, :], xt[:, :], R=[r_xt], W=[r_y])
                        else:
                            pg.dma("sp", xres[i * 128:(i + 1) * 128, :], xt[:, :], R=[r_xt], W=[r_xres[i]])

        stop_after = getattr(cfg, "stop_after", None)
        def bar():
            pg.barrier((tok_d[0:1, :], ident_d[0:1, 0:64]), r_bar)

        for l in range(dp):
            last = (l == dp - 1)
            with ExitStack() as stL:
                hT = sb("hT%d" % l, [128, 8 * T], BF16, stL)
                phaseA(l); bar()
                phaseB(l); bar()
                phaseC(l); bar()
                phaseEF(l, last); bar()
                phaseG(l, last); bar()
                phaseH(l, last); bar()
            phaseI(l, last); bar()
        pg.finish([r_y] + list(r_dbg.values()))
        nc._n_sems = pg.nsem
        nc._minrem = minrem[0]
        nc._pg = pg
    return nc


_CACHE = {}


def kernel(**inputs):
    n_cores = 8
    B = inputs["x"].shape[0]
    cfg = Cfg(nb=B // n_cores, L=inputs["x"].shape[1], LC=inputs["ctx"].shape[1], NE=inputs["w_gu"].shape[1],
              DFF=inputs["w_down"].shape[2], depth=inputs["w_in"].shape[0], TG=1024)
    nc = build_nc(cfg)
    in_maps = []
    shared = None
    for core in range(n_cores):
        m, shared = prep_inputs(cfg, core, shared=shared, **inputs)
        in_maps.append(m)
    res = run_bass_kernel_spmd(nc, in_maps, core_ids=list(range(n_cores)))
    outs = [np.asarray(r["y"], np.float32).reshape(cfg.nb, cfg.L, D) for r in res.results]
    return np.concatenate(outs, axis=0)
```

```python
import numpy as np
from contextlib import ExitStack
import concourse.bass as bass
import concourse.mybir as mybir
from concourse.bass_utils import run_bass_kernel_spmd

F32 = mybir.dt.float32
BF16 = mybir.dt.bfloat16
AF = mybir.ActivationFunctionType
ALU = mybir.AluOpType
AX = mybir.AxisListType

D = 1024
GRID_W = 64
HD = 64
NA_H = 4
HG_H = 4
HG_DK = 128
SW_QH = 4
SW_KVH = 2
IN_COLS = 3840
EPS = 1e-6
NEG = -1e30
TOPK = 4
C_NAQ, C_NAK, C_NAV = 0, 256, 512
C_HGQ, C_HGFF, C_HGFB, C_HGI, C_HGG = 768, 1280, 1792, 2304, 2816
C_SWQ, C_SWK, C_SWV = 3328, 3584, 3712


class Cfg:
    def __init__(self, nb=2, L=2048, LC=256, NE=32, DFF=1024, depth=2, TG=1536):
        self.nb, self.L, self.LC, self.NE, self.DFF, self.depth, self.TG = nb, L, LC, NE, DFF, depth, TG
        self.N = L + LC
        self.T = nb * self.N
        self.R = L // GRID_W


class Res:
    __slots__ = ("name", "w", "rs", "sem", "cnt", "swq", "used")

    def __init__(self, name):
        self.name, self.w, self.rs, self.sem, self.cnt, self.used = name, {}, {}, None, 0, False


class _Rec:
    def __getattr__(self, name):
        def f(*a, **k):
            self.call = (name, a, k)
            return self
        return f


class _Eng:
    def __init__(self, key, sem):
        self.key, self.sem, self.count, self.ops, self.last, self.pending, self.waited = key, sem, 0, [], None, False, {}


class Prog:
    def __init__(self, nc, es):
        self.nc, self.es = nc, es
        self.eng = {}
        for k in ("pe", "act", "dve", "pool", "sp"):
            self.eng[k] = _Eng(k, es.enter_context(nc.semaphore("e_" + k)))
        self.semown = {id(e.sem): e for e in self.eng.values()}
        self.nsem = 5
        self.dma_res = []
        self.sem_pool = {}
        self.all_res = []
        self.nres = 0

    def res(self, name):
        self.nres += 1
        r = Res(name)
        self.all_res.append(r)
        return r

    def _wait(self, E, ev):
        if ev is None:
            return
        sem, val = ev
        own = self.semown.get(id(sem))
        if own is not None and own is E and E.key == "pe":
            return
        if own is not None and val > own.count:
            assert val == own.count + 1 and own.pending, (own.key, val, own.count)
            own.last["inc"] = True
            own.count += 1
            own.pending = False
        if E.waited.get(id(sem), 0) >= val:
            return
        E.waited[id(sem)] = val
        E.ops.append({"wait": (sem, val)})

    def _deps(self, E, R, W, dma_write=False):
        for r in R:
            for ev in list(r.w.values()):
                self._wait(E, ev)
        for w in W:
            for ev in list(w.w.values()):
                if dma_write and id(ev[0]) not in self.semown:
                    continue
                self._wait(E, ev)
            for ev in list(w.rs.values()):
                self._wait(E, ev)

    def _commit(self, ev, R, W, dma_write=False):
        for r in R:
            old = r.rs.get(id(ev[0]))
            if old is None or old[1] < ev[1]:
                r.rs[id(ev[0])] = ev
        for w in W:
            if dma_write:
                w.w = {k: v for k, v in w.w.items() if k not in self.semown}
                w.w[id(ev[0])] = ev
            else:
                w.w = {id(ev[0]): ev}
            w.rs = {}

    def op(self, ek, fn, R=(), W=()):
        E = self.eng[ek]
        self._deps(E, R, W)
        rec = _Rec()
        fn(rec)
        ent = {"fn": rec.call, "inc": False}
        E.ops.append(ent)
        E.last = ent
        E.pending = True
        self._commit((E.sem, E.count + 1), R, W)

    def dma(self, qk, out, in_, R=(), W=(), **kw):
        E = self.eng[qk]
        is_store = ("DRam" in type(out.tensor).__name__) and ("DRam" not in type(in_.tensor).__name__)
        own = R[0] if is_store else W[0]
        if own.sem is None:
            pool = self.sem_pool.setdefault(qk == "pool", [])
            own.swq = (qk == "pool")
            if pool:
                own.sem, own.cnt = pool.pop()
            else:
                own.sem, own.cnt = self.es.enter_context(self.nc.semaphore("d_%d" % self.nsem)), 0
                self.nsem += 1
            self.dma_res.append(own)
        self._deps(E, R, W, dma_write=True)
        if own.sem is not None and getattr(own, "used", False):
            if is_store:
                cont = id(own.sem) in own.rs
            else:
                cont = (not own.rs) and all(k not in self.semown for k in own.w)
            if not cont:
                self._wait(E, (own.sem, own.cnt))
        own.used = True
        own.cnt += 16
        E.ops.append({"dma": (out, in_, kw, own.sem)})
        self._commit((own.sem, own.cnt), R, W, dma_write=True)

    def barrier(self, tok_d, r_bar):
        sp = self.eng["sp"]
        for k in ("pe", "act", "dve", "pool"):
            X = self.eng[k]
            if X.pending:
                self._wait(sp, (X.sem, X.count + 1))
            elif X.count > 0:
                self._wait(sp, (X.sem, X.count))
        for r in self.dma_res:
            if r is not r_bar:
                self._wait(sp, (r.sem, r.cnt))
        if r_bar.sem is not None:
            self._wait(sp, (r_bar.sem, r_bar.cnt))
        self.dma("sp", tok_d[0], tok_d[1], W=[r_bar])
        for r in self.dma_res:
            if r.swq:
                self._wait(self.eng["pool"], (r.sem, r.cnt))
        for k in ("pe", "act", "dve", "pool"):
            for ev in list(r_bar.w.values()):
                self._wait(self.eng[k], ev)
        for r in self.all_res:
            r.w = {}
            r.rs = {}
            if r.sem is not None and r is not r_bar:
                self.sem_pool[r.swq].append((r.sem, r.cnt))
                r.sem = None
                r.used = False
        self.dma_res = []

    def finish(self, outs):
        E = self.eng["sp"]
        for r in outs:
            for ev in list(r.w.values()):
                self._wait(E, ev)
        nc = self.nc
        with nc.Block() as block:
            def run(E, e):
                for o in E.ops:
                    if "wait" in o:
                        e.wait_ge(o["wait"][0], o["wait"][1])
                    elif "dma" in o:
                        out, in_, kw, sem = o["dma"]
                        e.dma_start(out=out, in_=in_, **kw).then_inc(sem, 16)
                    else:
                        nm_, a_, k_ = o["fn"]
                        ins = getattr(e, nm_)(*a_, **k_)
                        if o["inc"]:
                            ins.then_inc(E.sem, 1)

            @block.sync
            def _(e):
                run(self.eng["sp"], e)

            @block.tensor
            def _(e):
                run(self.eng["pe"], e)

            @block.scalar
            def _(e):
                run(self.eng["act"], e)

            @block.vector
            def _(e):
                run(self.eng["dve"], e)

            @block.gpsimd
            def _(e):
                run(self.eng["pool"], e)


def _fm(v, p=128):
    sh = v.shape
    return np.ascontiguousarray(np.swapaxes(v.reshape(sh[:-1] + (sh[-1] // p, p)), -1, -2))


def _na_bias_table(rpb):
    H = rpb.shape[0]
    kc = np.arange(64)[:, None]
    qc = np.arange(64)[None, :]
    qs = np.clip(qc - 8, 0, 48)
    ok = (kc >= qs) & (kc < qs + 16)
    dc = np.clip(kc - qc + 15, 0, 30)
    g = rpb[:, :, dc]
    g = np.where(ok[None, None], g, np.float32(NEG)).astype(np.float32)
    tab = np.empty((2, 64, H, 14, 64), np.float32)
    for dl in range(2):
        tab[dl] = np.transpose(g[:, dl:dl + 14], (2, 0, 1, 3))
    return np.ascontiguousarray(tab.reshape(128, H * 14 * 64))


def _consts(cfg):
    L = cfg.L
    c = {}
    c["ident"] = np.eye(128, dtype=np.float32)
    s = np.arange(64)[:, None]
    t = np.arange(64)[None, :]
    c["maskUL"] = np.concatenate([(s <= t), (s >= t)], axis=1).astype(np.float32)
    kp = np.arange(128)[:, None]
    qp = np.arange(128)[None, :]
    ml = np.where(kp >= qp, 0.0, NEG)
    mr = np.where(kp <= qp, 0.0, NEG)
    c["swam"] = np.concatenate([ml, mr], axis=1).astype(np.float32)
    tok = np.arange(L)
    row = (tok // GRID_W).astype(np.float64)
    col = (tok % GRID_W).astype(np.float64)
    inv = 10000.0 ** (-np.arange(16, dtype=np.float64) / 16)
    C = np.zeros((64, L)); S = np.zeros((64, L))
    for d in range(64):
        pos = row if d < 32 else col
        ang = pos * inv[d % 16]
        C[d] = np.cos(ang)
        S[d] = -np.sin(ang) if (d % 32) < 16 else np.sin(ang)
    c["ropeC"] = np.concatenate([C, C], 0).astype(np.float32)
    c["ropeS"] = np.concatenate([S, S], 0).astype(np.float32)
    P = np.zeros((128, 128), np.float32)
    for do in range(128):
        dd = do % 32
        di = do + 16 if dd < 16 else do - 16
        P[di, do] = 1.0
    c["ropeP"] = P
    o2 = np.zeros((128, 128), np.float32)
    o2[:64, :64] = 1.0
    o2[64:, 64:] = 1.0
    c["ones2"] = o2
    return c


def prep_inputs(cfg, core, x, c, ctx, c_ctx, hg_lower_bounds, ada_w, ada_b, norm1_g, norm2_g, w_in, na_q_norm,
                na_k_norm, na_rpb, hg_norm_g, swa_q_norm, swa_k_norm, swa_sink, w_out, router_w, router_b,
                w_gu, b_gu, w_down, b_down, shared=None):
    nb, dp = cfg.nb, cfg.depth
    f = np.float32
    b0 = core * nb
    m = {}
    m["x"] = np.ascontiguousarray(x[b0:b0 + nb].reshape(nb * cfg.L, D), f)
    m["ctx"] = np.ascontiguousarray(ctx[b0:b0 + nb].reshape(nb * cfg.LC, D), f)
    cc = np.concatenate([c[b0:b0 + nb], c_ctx[None]], 0).astype(f)
    m["ccT"] = np.ascontiguousarray(np.transpose(cc.reshape(nb + 1, 8, 128), (2, 1, 0)).reshape(128, 8 * (nb + 1)))
    if shared is None:
        shared = {}
        shared["ada_w"] = np.ascontiguousarray(ada_w, f)
        shared["ada_bT"] = _fm(ada_b.astype(f))
        shared["ada_b"] = np.ascontiguousarray(ada_b.astype(f).reshape(dp, 1, 6 * D))
        shared["n1T"] = _fm(norm1_g.astype(f))
        shared["n2T"] = _fm(norm2_g.astype(f))
        shared["w_in"] = np.ascontiguousarray(w_in, f)
        shared["w_out"] = np.ascontiguousarray(w_out, f)
        g4 = np.stack([np.tile(na_q_norm, (1, 2)), np.tile(na_k_norm, (1, 2)), np.tile(swa_q_norm, (1, 2)),
                       np.tile(swa_k_norm, (1, 2))], axis=-1)
        shared["gains4"] = np.ascontiguousarray(g4, f)
        shared["nab"] = np.stack([_na_bias_table(na_rpb[l].astype(f)) for l in range(dp)])
        shared["hlbT"] = np.ascontiguousarray(np.transpose(hg_lower_bounds.astype(f).reshape(dp, HG_H, 128), (2, 0, 1)).reshape(128, dp * HG_H))
        shared["hgn"] = np.ascontiguousarray(hg_norm_g.astype(f).reshape(dp, 128, 1))
        shared["sink"] = np.ascontiguousarray(np.broadcast_to(swa_sink.astype(f)[:, None, :], (dp, 128, SW_QH)))
        shared["router_w"] = np.ascontiguousarray(router_w, f)
        shared["router_b"] = np.ascontiguousarray(router_b.astype(f).reshape(dp, 1, cfg.NE))
        shared["w_gu"] = np.ascontiguousarray(w_gu, f)
        shared["b_guT"] = np.ascontiguousarray(_fm(b_gu.astype(f)).transpose(0, 2, 1, 3).reshape(dp, 128, -1))
        shared["w_down"] = np.ascontiguousarray(w_down, f)
        shared["b_down"] = np.ascontiguousarray(b_down, f)
        shared.update(_consts(cfg))
    m.update(shared)
    return m, shared


def build_nc(cfg, dbg=()):
    nc = bass.Bass("TRN2", target_bir_lowering=False)
    nb, L, LC, N, T, NE, DFF, dp, R = cfg.nb, cfg.L, cfg.LC, cfg.N, cfg.T, cfg.NE, cfg.DFF, cfg.depth, cfg.R
    NB1 = nb + 1
    NT = T // 128
    FC = DFF // 128
    ins = {}

    def din(name, shape):
        ins[name] = nc.dram_tensor(name, list(shape), F32, kind="ExternalInput")
        return ins[name]

    x_d = din("x", [nb * L, D]); ctx_d = din("ctx", [nb * LC, D]); ccT_d = din("ccT", [128, 8 * NB1])
    ada_w = din("ada_w", [dp, D, 6 * D]); ada_bT = din("ada_bT", [dp, 128, 48]); ada_b = din("ada_b", [dp, 1, 6 * D])
    n1T_d = din("n1T", [dp, 128, 8]); n2T_d = din("n2T", [dp, 128, 8])
    w_in = din("w_in", [dp, D, IN_COLS]); w_out = din("w_out", [dp, D, D])
    gains4 = din("gains4", [dp, 128, 4]); nab_d = din("nab", [dp, 128, NA_H * 14 * 64])
    hlbT_d = din("hlbT", [128, dp * HG_H]); hgn_d = din("hgn", [dp, 128, 1]); sink_d = din("sink", [dp, 128, SW_QH])
    rw_d = din("router_w", [dp, D, NE]); rb_d = din("router_b", [dp, 1, NE])
    wgu_d = din("w_gu", [dp, NE, D, 2 * DFF]); bguT_d = din("b_guT", [dp, 128, NE * 2 * FC])
    wdn_d = din("w_down", [dp, NE, DFF, D]); bdn_d = din("b_down", [dp, NE, D])
    ident_d = din("ident", [128, 128]); maskUL_d = din("maskUL", [64, 128]); swam_d = din("swam", [128, 256])
    ropeC_d = din("ropeC", [128, L]); ropeS_d = din("ropeS", [128, L]); ropeP_d = din("ropeP", [128, 128])
    ones2_d = din("ones2", [128, 128])
    y_d = nc.dram_tensor("y", [nb * L, D], F32, kind="ExternalOutput")
    dbg_d = {k: nc.dram_tensor("dbg_" + k, list(s), F32, kind="ExternalOutput") for k, s in dbg}

    xres = nc.dram_tensor("xres", [T, D], F32)
    qk_d = nc.dram_tensor("qk_d", [7, 128, T], BF16)
    v_d = nc.dram_tensor("v_d", [T, 384], BF16)
    yT_d = nc.dram_tensor("yT_d", [D, T], BF16)
    h2T_d = nc.dram_tensor("h2T_d", [128, 8, T], BF16)
    Gt_d = nc.dram_tensor("Gt_d", [NB1 * 2, 128, D], F32)

    with ExitStack() as es:
        pg = Prog(nc, es)

        uid = [0]
        minrem = [1 << 30]

        def sb(name, shape, dt=F32, stack=es):
            uid[0] += 1
            t_ = stack.enter_context(nc.sbuf_tensor("s%d_%s" % (uid[0], name), list(shape), dt))
            minrem[0] = min(minrem[0], nc.sbuf_bytes_remaining)
            return t_

        def ps(name, shape, dt=F32, stack=es):
            uid[0] += 1
            return stack.enter_context(nc.psum_tensor("p%d_%s" % (uid[0], name), list(shape), dt))

        RS = pg.res
        r_hT = RS("hT")
        tok_d = nc.dram_tensor("bar_tok", [2, 64], F32); r_bar = RS("bar")
        identf = sb("identf", [128, 128]); identb = sb("identb", [128, 128], BF16)
        ones2 = sb("ones2b", [128, 128], BF16); onesb = sb("onesb", [128, 128], BF16); onesf = sb("onesf", [128, 128])
        csf = sb("csf", [128, 8 * NB1]); csb = sb("csb", [128, 8 * NB1], BF16)
        MV = sb("MV", [128, 4 * 8 * NB1])
        stage = sb("stage", [128, 128])
        r_const = RS("const"); r_cs = RS("cs"); r_MV = RS("MV"); r_stage = RS("stage")
        psT = ps("psT", [128, 1024]); r_psT = RS("psT")
        pb = [ps("pb%d" % i, [128, 512]) for i in range(5)]; r_pb = [RS("pb%d" % i) for i in range(5)]
        pbf = ps("pbf", [128, 1024], BF16); r_pbf = RS("pbf")
        r_xres = [RS("xres%d" % i) for i in range(NT)]
        r_y = RS("y"); r_qk = RS("qk_d"); r_v = RS("v_d"); r_yT = RS("yT_d"); r_h2 = RS("h2T_d"); r_Gt = RS("Gt_d")
        r_dbg = {k: RS("dbg" + k) for k in dbg_d}
        rot = {"pb": 0}

        def nextpb():
            i = rot["pb"]; rot["pb"] = (i + 1) % 5
            return pb[i], r_pb[i]

        def tile_info(i):
            g0 = i * 128; b = g0 // N; pos0 = g0 - b * N
            return b, pos0, pos0 < LC

        pg.dma("sp", identf[:, :], ident_d[:, :], W=[r_const])
        pg.dma("sp", stage[:, :], ones2_d[:, :], W=[r_stage])
        pg.op("dve", lambda e: e.tensor_copy(identb[:, :], identf[:, :]), R=[r_const], W=[r_const])
        pg.op("dve", lambda e: e.tensor_copy(ones2[:, :], stage[:, :]), R=[r_stage], W=[r_const])
        pg.op("dve", lambda e: e.memset(onesb[:, :], 1.0), W=[r_const])
        pg.op("dve", lambda e: e.memset(onesf[:, :], 1.0), W=[r_const])
        pg.dma("sp", csf[:, :], ccT_d[:, :], W=[r_cs])
        pg.op("act", lambda e: e.activation(csf[:, :], csf[:, :], AF.Silu), R=[r_cs], W=[r_cs])
        pg.op("dve", lambda e: e.tensor_copy(csb[:, :], csf[:, :]), R=[r_cs], W=[r_cs])
        r_init = RS("init")
        for b in range(nb):
            for (src, s0, n, p0) in ((ctx_d, b * LC, LC, 0), (x_d, b * L, L, LC)):
                for j in range(n // 128):
                    gi = (b * N + p0) // 128 + j
                    pg.dma("sp", xres[gi * 128:(gi + 1) * 128, :], src[s0 + j * 128:s0 + (j + 1) * 128, :], W=[r_init])

        pg.barrier((tok_d[0:1, :], ident_d[0:1, 0:64]), r_bar)

        def dbg_out(key, ap, res_list, rows=128):
            if key in dbg_d:
                pg.dma("sp", dbg_d[key][0:rows, :], ap, R=res_list, W=[r_dbg[key]])

        def phaseA(l):
            with ExitStack() as st:
                wts = [sb("adaw%d" % i, [128, 8 * 1024], BF16, st) for i in range(2)]
                r_w = [RS("adaw%d" % i) for i in range(2)]
                abT = sb("abT", [128, 48], F32, st); abrow = sb("abrow", [1, 6 * D], F32, st); abrowb = sb("abrowb", [1, 6 * D], BF16, st)
                nT = sb("nT", [128, 16], F32, st); modT = sb("modT", [128, 4 * 8 * NB1], F32, st)
                gt = [sb("gt%d" % i, [128, 512], F32, st) for i in range(2)]; r_gt = [RS("gt%d" % i) for i in range(2)]
                r_ab = RS("ab"); r_mod = RS("modT")
                csrep = sb("csrep", [128, 8 * NB1 * 128], BF16, st); zerosf = sb("zerosf", [128, 128], F32, st)
                pg.op("dve", lambda e: e.memset(zerosf[:, :], 0.0), W=[r_cs])
                for kc in range(8):
                    for b in range(NB1):
                        c0 = (kc * NB1 + b)
                        pg.op("dve", lambda e: e.tensor_scalar(csrep[:, c0 * 128:(c0 + 1) * 128], zerosf[:, 0:128], csf[:, c0:c0 + 1], None, ALU.add), R=[r_cs], W=[r_cs])
                pg.dma("sp", abT[:, :], ada_bT[l, :, :], W=[r_ab])
                pg.dma("sp", abrow[:, :], ada_b[l, :, :], W=[r_ab])
                pg.dma("sp", nT[:, 0:8], n1T_d[l, :, :], W=[r_ab])
                pg.dma("sp", nT[:, 8:16], n2T_d[l, :, :], W=[r_ab])
                pg.op("act", lambda e: e.activation(abrowb[:, :], abrow[:, :], AF.Copy), R=[r_ab], W=[r_ab])
                gi = 0
                for blk in range(6):
                    wt, rw = wts[blk % 2], r_w[blk % 2]
                    for kc in range(8):
                        pg.dma("pool", wt[:, kc * 1024:(kc + 1) * 1024], ada_w[l, kc * 128:(kc + 1) * 128, blk * 1024:(blk + 1) * 1024], W=[rw])
                    if blk in (0, 1, 3, 4):
                        fm = {0: 0, 1: 1, 3: 2, 4: 3}[blk]
                        p, rp = nextpb()
                        for j in range(8):
                            for kc in range(8):
                                pg.op("pe", lambda e, p=p, j=j, kc=kc, wt=wt: e.matmul(
                                    p[:, j * NB1:(j + 1) * NB1], wt[:, kc * 1024 + j * 128: kc * 1024 + (j + 1) * 128],
                                    csb[:, kc * NB1:(kc + 1) * NB1], start=(kc == 0), stop=(kc == 7)), R=[rw, r_cs], W=[rp])
                        for j in range(8):
                            pg.op("dve", lambda e, p=p, j=j, fm=fm, blk=blk: e.tensor_scalar(
                                modT[:, (fm * 8 + j) * NB1:(fm * 8 + j + 1) * NB1], p[:, j * NB1:(j + 1) * NB1],
                                abT[:, blk * 8 + j: blk * 8 + j + 1], None, ALU.add), R=[rp, r_ab], W=[r_mod])
                    else:
                        which = 0 if blk == 2 else 1
                        for b in range(NB1):
                            for half in range(2):
                                p, rp = nextpb()
                                for kc in range(8):
                                    c0 = kc * NB1 + b
                                    pg.op("pe", lambda e, p=p, kc=kc, c0=c0, half=half, wt=wt: e.matmul(
                                        p[:, :], csrep[:, c0 * 128:(c0 + 1) * 128], wt[:, kc * 1024 + half * 512: kc * 1024 + (half + 1) * 512],
                                        start=(kc == 0), stop=False), R=[rw, r_cs], W=[rp])
                                cb = blk * 1024 + half * 512
                                pg.op("pe", lambda e, p=p, cb=cb: e.matmul(p[:, :], onesb[0:1, :], abrowb[0:1, cb:cb + 512], start=False, stop=True),
                                      R=[r_ab, r_const], W=[rp])
                                g, rg = gt[gi % 2], r_gt[gi % 2]; gi += 1
                                pg.op("act", lambda e, g=g, p=p: e.activation(g[:, :], p[:, :], AF.Copy), R=[rp], W=[rg])
                                pg.dma("sp", Gt_d[b * 2 + which, :, half * 512:(half + 1) * 512], g[:, :], R=[rg], W=[r_Gt])
                for j in range(8):
                    for (w, scf, shf, nofs) in ((0, 1, 0, 0), (2, 3, 2, 8)):
                        pg.op("dve", lambda e, j=j, w=w, scf=scf, nofs=nofs: e.tensor_scalar(
                            MV[:, (w * 8 + j) * NB1:(w * 8 + j + 1) * NB1], modT[:, (scf * 8 + j) * NB1:(scf * 8 + j + 1) * NB1],
                            1.0, nT[:, nofs + j:nofs + j + 1], ALU.add, ALU.mult), R=[r_mod, r_ab], W=[r_MV])
                        pg.op("dve", lambda e, j=j, w=w, shf=shf: e.tensor_copy(
                            MV[:, ((w + 1) * 8 + j) * NB1:((w + 1) * 8 + j + 1) * NB1], modT[:, (shf * 8 + j) * NB1:(shf * 8 + j + 1) * NB1]),
                            R=[r_mod], W=[r_MV])

        nm = {}

        def norm_setup(st):
            nm["junk"] = sb("nm_junk", [128, 1024], BF16, st); nm["ss"] = [sb("nm_ss%d" % i, [128, 2], F32, st) for i in range(2)]
            nm["xn"] = [sb("nm_xn%d" % i, [128, 1024], F32, st) for i in range(2)]
            nm["r_junk"] = RS("nm_junk"); nm["r_ss"] = [RS("nm_ss%d" % i) for i in range(2)]; nm["r_xn"] = [RS("nm_xn%d" % i) for i in range(2)]
            nm["k"] = 0

        def norm_mod_T(i, xt, r_xt, w, dstT=None, r_dst=None, keep_xnT=None):
            b, pos0, isc = tile_info(i)
            bm = nb if isc else b
            k = nm["k"]; nm["k"] = 1 - k
            ss, r_ss, xn, r_xn = nm["ss"][k], nm["r_ss"][k], nm["xn"][k], nm["r_xn"][k]
            pg.op("act", lambda e: e.activation(nm["junk"][:, :], xt[:, :], AF.Square, accum_out=ss[:, 0:1]), R=[r_xt], W=[nm["r_junk"], r_ss])
            pg.op("act", lambda e: e.activation(ss[:, 1:2], ss[:, 0:1], AF.Ln, scale=1.0 / D, bias=EPS), R=[r_ss], W=[r_ss])
            pg.op("act", lambda e: e.activation(ss[:, 1:2], ss[:, 1:2], AF.Exp, scale=-0.5), R=[r_ss], W=[r_ss])
            pg.op("dve", lambda e: e.tensor_scalar(xn[:, :], xt[:, :], ss[:, 1:2], None, ALU.mult), R=[r_xt, r_ss], W=[r_xn])
            for j in range(8):
                pg.op("pe", lambda e, j=j: e.transpose(psT[:, j * 128:(j + 1) * 128], xn[:, j * 128:(j + 1) * 128], identf[:, :]),
                      R=[r_xn, r_const], W=[r_psT])
            for j in range(8):
                ca = (w * 8 + j) * NB1 + bm; cbb = ((w + 1) * 8 + j) * NB1 + bm
                eng = "act" if j % 2 == 0 else "dve"
                dst = hT[:, j * T + i * 128: j * T + (i + 1) * 128]
                if eng == "act":
                    pg.op("act", lambda e, j=j, ca=ca, cbb=cbb, dst=dst: e.activation(dst, psT[:, j * 128:(j + 1) * 128], AF.Identity,
                                                                                    bias=MV[:, cbb:cbb + 1], scale=MV[:, ca:ca + 1]),
                          R=[r_psT, r_MV], W=[r_hT])
                else:
                    pg.op("dve", lambda e, j=j, ca=ca, cbb=cbb, dst=dst: e.tensor_scalar(dst, psT[:, j * 128:(j + 1) * 128], MV[:, ca:ca + 1],
                                                                                       MV[:, cbb:cbb + 1], ALU.mult, ALU.add),
                          R=[r_psT, r_MV], W=[r_hT])
            if keep_xnT is not None:
                kx, r_kx = keep_xnT
                pg.op("act", lambda e: e.activation(kx[:, :], psT[:, :], AF.Copy), R=[r_psT], W=[r_kx])

        def phaseB(l):
            with ExitStack() as st:
                norm_setup(st)
                xts = [sb("xt%d" % i, [128, 1024], F32, st) for i in range(2)]; r_xts = [RS("xt%d" % i) for i in range(2)]
                for i in range(NT):
                    xt, r_xt = xts[i % 2], r_xts[i % 2]
                    pg.dma("sp", xt[:, :], xres[i * 128:(i + 1) * 128, :], R=[r_xres[i]], W=[r_xt])
                    norm_mod_T(i, xt, r_xt, 0)

        def tok_chunks(maxn=512):
            out = []
            for b in range(nb):
                for (p0, n, isc) in ((0, LC, True), (LC, L, False)):
                    o = 0
                    while o < n:
                        m = min(maxn, n - o)
                        out.append((b, b * N + p0 + o, m, isc, o))
                        o += m
            return out

        SLOTS = [((C_NAQ, C_NAQ + 64), 0, False), ((C_NAQ + 128, C_NAQ + 192), 0, False),
                 ((C_NAK, C_NAK + 64), 1, False), ((C_NAK + 128, C_NAK + 192), 1, False),
                 ((C_SWQ, C_SWQ + 128), 2, True), ((C_SWQ + 64, C_SWQ + 192), 2, True),
                 ((C_SWK, C_SWK + 64), 3, True)]

        def phaseC(l):
            with ExitStack() as st:
                wqk = sb("wqk", [128, 8, 7 * 128], BF16, st); wv = sb("wv", [128, 8, 384], BF16, st)
                gt4 = sb("gt4", [128, 4], F32, st); r_w = RS("wqk"); r_g = RS("gt4")
                rC = sb("ropeC", [128, L], F32, st); rS = sb("ropeS", [128, L], F32, st); rP = sb("ropeP", [128, 128], BF16, st); r_rope = RS("rope")
                sq = [sb("c_sq%d" % i, [128, 512], BF16, st) for i in range(2)]; r_sq = [RS("c_sq%d" % i) for i in range(2)]
                rstd = [sb("c_rstd%d" % i, [128, 512], F32, st) for i in range(2)]; r_rstd = [RS("c_rstd%d" % i) for i in range(2)]
                qn = [sb("c_qn%d" % i, [128, 512], BF16, st) for i in range(2)]; r_qn = [RS("c_qn%d" % i) for i in range(2)]
                t1 = [sb("c_t1%d" % i, [128, 512], F32, st) for i in range(2)]; r_t1 = [RS("c_t1%d" % i) for i in range(2)]
                t2 = [sb("c_t2%d" % i, [128, 512], F32, st) for i in range(2)]; r_t2 = [RS("c_t2%d" % i) for i in range(2)]
                qo = [sb("c_qo%d" % i, [128, 512], BF16, st) for i in range(2)]; r_qo = [RS("c_qo%d" % i) for i in range(2)]
                vs = [sb("c_vs%d" % i, [128, 384], BF16, st) for i in range(2)]; r_vs = [RS("c_vs%d" % i) for i in range(2)]
                for s, (cols, gcol, rope) in enumerate(SLOTS):
                    for g, c0 in enumerate(cols):
                        pg.dma("pool", wqk[:, :, s * 128 + g * 64: s * 128 + (g + 1) * 64],
                               w_in[l, :, c0:c0 + 64].rearrange("(kc p) n -> p kc n", p=128), W=[r_w])
                for (c0, n, o) in ((C_NAV, 256, 0), (C_SWV, 128, 256)):
                    pg.dma("pool", wv[:, :, o:o + n], w_in[l, :, c0:c0 + n].rearrange("(kc p) n -> p kc n", p=128), W=[r_w])
                pg.dma("sp", gt4[:, :], gains4[l, :, :], W=[r_g])
                for c in (0, 2):
                    pg.op("dve", lambda e, c=c: e.tensor_scalar(gt4[:, c:c + 1], gt4[:, c:c + 1], HD ** -0.5, None, ALU.mult), R=[r_g], W=[r_g])
                pg.dma("sp", rC[:, :], ropeC_d[:, :], W=[r_rope]); pg.dma("sp", rS[:, :], ropeS_d[:, :], W=[r_rope])
                pg.dma("sp", stage[:, :], ropeP_d[:, :], W=[r_stage])
                pg.op("dve", lambda e: e.tensor_copy(rP[:, :], stage[:, :]), R=[r_stage], W=[r_rope])
                it = 0
                for (b, g0, n, isc, o) in tok_chunks():
                    for s, (cols, gcol, rope) in enumerate(SLOTS):
                        k = it % 2; it += 1
                        p, rp = nextpb()
                        for kc in range(8):
                            pg.op("pe", lambda e, p=p, kc=kc, s=s, g0=g0, n=n: e.matmul(
                                p[:, 0:n], wqk[:, kc, s * 128:(s + 1) * 128], hT[:, kc * T + g0: kc * T + g0 + n], start=(kc == 0), stop=(kc == 7)),
                                R=[r_w, r_hT], W=[rp])
                        pg.op("act", lambda e, p=p, k=k, n=n: e.activation(sq[k][:, 0:n], p[:, 0:n], AF.Square), R=[rp], W=[r_sq[k]])
                        p2, rp2 = nextpb()
                        pg.op("pe", lambda e, p2=p2, k=k, n=n: e.matmul(p2[:, 0:n], ones2[:, :], sq[k][:, 0:n], start=True, stop=True),
                              R=[r_sq[k], r_const], W=[rp2])
                        pg.op("act", lambda e, p2=p2, k=k, n=n: e.activation(rstd[k][:, 0:n], p2[:, 0:n], AF.Ln, scale=1.0 / HD, bias=EPS), R=[rp2], W=[r_rstd[k]])
                        pg.op("act", lambda e, k=k, n=n: e.activation(rstd[k][:, 0:n], rstd[k][:, 0:n], AF.Exp, scale=-0.5), R=[r_rstd[k]], W=[r_rstd[k]])
                        dorope = rope and not isc
                        dst, r_dst = (qn[k], r_qn[k]) if dorope else (qo[k], r_qo[k])
                        pg.op("dve", lambda e, p=p, k=k, n=n, dst=dst, gcol=gcol: e.scalar_tensor_tensor(
                            dst[:, 0:n], p[:, 0:n], gt4[:, gcol:gcol + 1], rstd[k][:, 0:n], ALU.mult, ALU.mult), R=[rp, r_g, r_rstd[k]], W=[r_dst])
                        if dorope:
                            p3, rp3 = nextpb()
                            pg.op("pe", lambda e, p3=p3, k=k, n=n: e.matmul(p3[:, 0:n], rP[:, :], qn[k][:, 0:n], start=True, stop=True),
                                  R=[r_qn[k], r_rope], W=[rp3])
                            pg.op("dve", lambda e, k=k, n=n, o=o: e.tensor_tensor(t1[k][:, 0:n], qn[k][:, 0:n], rC[:, o:o + n], ALU.mult),
                                  R=[r_qn[k], r_rope], W=[r_t1[k]])
                            pg.op("dve", lambda e, p3=p3, k=k, n=n, o=o: e.tensor_tensor(t2[k][:, 0:n], p3[:, 0:n], rS[:, o:o + n], ALU.mult),
                                  R=[rp3, r_rope], W=[r_t2[k]])
                            pg.op("pool", lambda e, k=k, n=n: e.tensor_tensor(qo[k][:, 0:n], t1[k][:, 0:n], t2[k][:, 0:n], ALU.add),
                                  R=[r_t1[k], r_t2[k]], W=[r_qo[k]])
                        pg.dma("sp", qk_d[s, :, g0:g0 + n], qo[k][:, 0:n], R=[r_qo[k]], W=[r_qk])
                for i in range(NT):
                    k = i % 2
                    p, rp = nextpb()
                    for kc in range(8):
                        pg.op("pe", lambda e, p=p, kc=kc, i=i: e.matmul(p[:, 0:384], hT[:, kc * T + i * 128: kc * T + (i + 1) * 128], wv[:, kc, :],
                                                                    start=(kc == 0), stop=(kc == 7)), R=[r_w, r_hT], W=[rp])
                    pg.op("act", lambda e, p=p, k=k: e.activation(vs[k][:, :], p[:, 0:384], AF.Copy), R=[rp], W=[r_vs[k]])
                    pg.dma("sp", v_d[i * 128:(i + 1) * 128, :], vs[k][:, :], R=[r_vs[k]], W=[r_v])

        at = {}

        def attn_setup(st):
            at["E"] = [sb("at_E%d" % i, [128, 512], BF16, st) for i in range(3)]; at["r_E"] = [RS("at_E%d" % i) for i in range(3)]
            at["rd"] = [sb("at_rd%d" % i, [64, 128], F32, st) for i in range(2)]; at["r_rd"] = [RS("at_rd%d" % i) for i in range(2)]
            at["ke"] = 0; at["kr"] = 0

        def attn_unit(hp, q_ap, keys, nq, dst, r_dst, Rq, sink_ap=None):
            per = 512 // nq
            groups = [keys[i:i + per] for i in range(0, len(keys), per)]
            Es = []
            for grp in groups:
                p, rp = nextpb()
                for t, (k_ap, b_ap, v_ap) in enumerate(grp):
                    pg.op("pe", lambda e, p=p, t=t, k_ap=k_ap, b_ap=b_ap: e.matmul(p[:, t * nq:(t + 1) * nq], k_ap, q_ap, start=True, stop=(b_ap is None)),
                          R=Rq, W=[rp])
                    if b_ap is not None:
                        pg.op("pe", lambda e, p=p, t=t, b_ap=b_ap: e.matmul(p[:, t * nq:(t + 1) * nq], identb[:, :], b_ap, start=False, stop=True),
                              R=Rq + [r_const], W=[rp])
                ke = at["ke"]; at["ke"] = (ke + 1) % 3
                E, rE = at["E"][ke], at["r_E"][ke]
                w = len(grp) * nq
                pg.op("act", lambda e, p=p, E=E, w=w: e.activation(E[:, 0:w], p[:, 0:w], AF.Exp), R=[rp], W=[rE])
                Es.append((E, rE, grp))
            po, rpo = nextpb()
            flat = [(E, rE, t, v_ap) for (E, rE, grp) in Es for t, (_, _, v_ap) in enumerate(grp)]
            for i, (E, rE, t, v_ap) in enumerate(flat):
                pg.op("pe", lambda e, E=E, t=t, i=i: e.matmul(po[0:64, nq:2 * nq], onesb[:, 0:64], E[:, t * nq:(t + 1) * nq], start=(i == 0), stop=(i == len(flat) - 1)),
                      R=[rE, r_const], W=[rpo])
            for i, (E, rE, t, v_ap) in enumerate(flat):
                pg.op("pe", lambda e, E=E, t=t, i=i, v_ap=v_ap: e.matmul(po[0:64, 0:nq], v_ap, E[:, t * nq:(t + 1) * nq], start=(i == 0), stop=(i == len(flat) - 1)),
                      R=[rE] + Rq, W=[rpo])
            kr = at["kr"]; at["kr"] = 1 - kr
            rd, r_rd = at["rd"][kr], at["r_rd"][kr]
            if sink_ap is not None:
                pg.op("dve", lambda e: e.tensor_scalar(rd[:, 0:nq], po[0:64, nq:2 * nq], sink_ap, None, ALU.add), R=[rpo] + Rq, W=[r_rd])
                pg.op("dve", lambda e: e.reciprocal(rd[:, 0:nq], rd[:, 0:nq]), R=[r_rd], W=[r_rd])
            else:
                pg.op("dve", lambda e: e.reciprocal(rd[:, 0:nq], po[0:64, nq:2 * nq]), R=[rpo], W=[r_rd])
            pg.op("dve", lambda e: e.tensor_tensor(dst, po[0:64, 0:nq], rd[:, 0:nq], ALU.mult), R=[rpo, r_rd], W=[r_dst])

        def phaseEF(l, last):
            with ExitStack() as st:
                attn_setup(st)
                QK = sb("QK", [128, 7, N], BF16, st); VE = sb("VE", [128, N // 128, 384], BF16, st); VO = sb("VO", [128, L // 128, 384], BF16, st)
                nabf = sb("nabf", [128, 14 * 64], F32, st); nabt = sb("nabt", [128, NA_H * 14 * 64], BF16, st)
                swmf = sb("swmf", [128, 256], F32, st); swm = sb("swm", [128, 256], BF16, st); esink = sb("esink", [128, SW_QH], F32, st)
                stg = [sb("stg%d" % i, [64, N], BF16, st) for i in range(2)]; r_stg = [RS("stg%d" % i) for i in range(2)]
                r_in = RS("attn_in"); r_tab = RS("attn_tab"); r_nabf = RS("nabf")
                for h in range(NA_H):
                    pg.dma("sp", nabf[:, :], nab_d[l, :, h * 896:(h + 1) * 896], W=[r_nabf])
                    pg.op("act", lambda e, h=h: e.activation(nabt[:, h * 896:(h + 1) * 896], nabf[:, :], AF.Copy), R=[r_nabf], W=[r_tab])
                pg.dma("sp", swmf[:, :], swam_d[:, :], W=[r_nabf])
                pg.op("act", lambda e: e.activation(swm[:, :], swmf[:, :], AF.Copy), R=[r_nabf], W=[r_tab])
                pg.dma("sp", esink[:, :], sink_d[l, :, :], W=[r_tab])
                pg.op("act", lambda e: e.activation(esink[:, :], esink[:, :], AF.Exp), R=[r_tab], W=[r_tab])
                Rq = [r_in, r_tab]
                ks = 0
                LT = LC // 128
                for b in range(nb):
                    g0 = b * N
                    for s in range(7):
                        pg.dma("sp", QK[:, s, :], qk_d[s, :, g0:g0 + N], R=[r_qk], W=[r_in])
                    pg.dma("sp", VE[:, :, :], v_d[g0:g0 + N, :].rearrange("(t p) c -> p t c", p=128), R=[r_v], W=[r_in])
                    pg.dma("sp", VO[:, 0:L // 128 - 1, :], v_d[g0 + LC + 64:g0 + LC + 64 + L - 128, :].rearrange("(t p) c -> p t c", p=128), R=[r_v], W=[r_in])
                    for h in range(NA_H):
                        sq_, sk_, hp = h // 2, 2 + h // 2, h % 2
                        ps0 = hp * 64
                        sg, r_sg = stg[ks % 2], r_stg[ks % 2]; ks += 1
                        ckeys = [(QK[ps0:ps0 + 64, sk_, t * 128:(t + 1) * 128], None, VE[:, t, h * 64:(h + 1) * 64]) for t in range(LT)]
                        if not last:
                            for qt in range(LT):
                                attn_unit(hp, QK[ps0:ps0 + 64, sq_, qt * 128:(qt + 1) * 128], ckeys, 128, sg[:, qt * 128:(qt + 1) * 128], r_sg, Rq)
                        for r in range(R):
                            s0 = min(max(r - 4, 0), R - 8)
                            keys = []
                            for t in range(4):
                                kr_ = s0 + 2 * t
                                pos = LC + 64 * kr_
                                d0 = (s0 - r + 7) + 2 * t
                                v_ap = VE[:, LT + kr_ // 2, h * 64:(h + 1) * 64] if kr_ % 2 == 0 else VO[:, (kr_ - 1) // 2, h * 64:(h + 1) * 64]
                                keys.append((QK[ps0:ps0 + 64, sk_, pos:pos + 128], nabt[:, (h * 14 + d0) * 64:(h * 14 + d0 + 1) * 64], v_ap))
                            attn_unit(hp, QK[ps0:ps0 + 64, sq_, LC + 64 * r: LC + 64 * (r + 1)], keys + ckeys, 64,
                                      sg[:, LC + 64 * r: LC + 64 * (r + 1)], r_sg, Rq)
                        c0 = 0 if not last else LC
                        pg.dma("sp", yT_d[h * 64:(h + 1) * 64, g0 + c0:g0 + N], sg[:, c0:N], R=[r_sg], W=[r_yT])
                    for hq in range(SW_QH):
                        sq_, half, kv = 4 + hq % 2, hq // 2, hq // 2
                        ps0 = half * 64
                        vc0 = 256 + kv * 64
                        sg, r_sg = stg[ks % 2], r_stg[ks % 2]; ks += 1
                        ckeys = [(QK[ps0:ps0 + 64, 6, t * 128:(t + 1) * 128], None, VE[:, t, vc0:vc0 + 64]) for t in range(LT)]
                        sk_ap = esink[0:64, hq:hq + 1]
                        if not last:
                            for qt in range(LT):
                                attn_unit(half, QK[ps0:ps0 + 64, sq_, qt * 128:(qt + 1) * 128], ckeys, 128, sg[:, qt * 128:(qt + 1) * 128], r_sg, Rq, sk_ap)
                        nblk = L // 128
                        for n in range(nblk):
                            keys = []
                            for (bk, m0) in ((n - 1, 0), (n, None), (n + 1, 128)):
                                if 0 <= bk < nblk:
                                    pos = LC + 128 * bk
                                    keys.append((QK[ps0:ps0 + 64, 6, pos:pos + 128], None if m0 is None else swm[:, m0:m0 + 128], VE[:, LT + bk, vc0:vc0 + 64]))
                            attn_unit(half, QK[ps0:ps0 + 64, sq_, LC + 128 * n: LC + 128 * (n + 1)], keys + ckeys, 128,
                                      sg[:, LC + 128 * n: LC + 128 * (n + 1)], r_sg, Rq, sk_ap)
                        c0 = 0 if not last else LC
                        pg.dma("sp", yT_d[768 + hq * 64:768 + (hq + 1) * 64, g0 + c0:g0 + N], sg[:, c0:N], R=[r_sg], W=[r_yT])

        def phaseG(l, last):
            CH = 16
            NCH = N // CH
            LCH = LC // CH
            with ExitStack() as st:
                whg = [sb("whg%d" % i, [128, 8, 640], BF16, st) for i in range(2)]; r_whg = [RS("whg%d" % i) for i in range(2)]
                qs = sb("g_qs", [128, N], BF16, st); kd = [sb("g_k%d" % i, [128, N], BF16, st) for i in range(2)]
                Pd = [sb("g_P%d" % i, [128, N + 1], F32, st) for i in range(2)]; nPd = [sb("g_nP%d" % i, [128, NCH + 1], F32, st) for i in range(2)]
                sgate = sb("g_sg", [128, N], BF16, st)
                vch = [sb("g_vch%d" % i, [CH, 128], BF16, st) for i in range(4)]; r_vch = [RS("g_vch%d" % i) for i in range(4)]
                mUL = sb("g_mUL", [64, 128], F32, st); hlb = sb("g_hlb", [128, dp * HG_H], F32, st); lbt = sb("g_lbt", [128, 2 * HG_H], F32, st)
                gn = sb("g_gn", [128, 1], F32, st)
                tmpa = [sb("g_ta%d" % i, [128, 512], F32, st) for i in range(2)]; r_tmpa = [RS("g_ta%d" % i) for i in range(2)]
                tmpb = [sb("g_tb%d" % i, [128, 512], F32, st) for i in range(2)]; r_tmpb = [RS("g_tb%d" % i) for i in range(2)]
                sqb = [sb("g_sqb%d" % i, [128, 512], BF16, st) for i in range(2)]; r_sqb = [RS("g_sqb%d" % i) for i in range(2)]
                yo = [sb("g_yo%d" % i, [128, 512], BF16, st) for i in range(2)]; r_yo = [RS("g_yo%d" % i) for i in range(2)]
                ex = [[sb("g_e%d%d" % (j, i), [128, CH], F32, st) for i in range(4)] for j in range(3)]
                r_ex = [[RS("g_e%d%d" % (j, i)) for i in range(4)] for j in range(3)]
                oacc2 = [sb("g_oacc2%d" % i, [128, N], F32, st) for i in range(2)]; r_oacc2 = [RS("g_oacc2%d" % i) for i in range(2)]
                r_pbf2 = [RS("pbf2%d" % i) for i in range(2)]
                oacc = oacc2[0]
                vT = sb("g_vT", [128, N], BF16, st); r_vT = RS("g_vT"); r_pbf3 = [RS("pbf3%d" % i) for i in range(2)]
                lf = oacc2[1]
                qt = [sb("g_qt%d" % i, [128, CH], BF16, st) for i in range(4)]; r_qt = [RS("g_qt%d" % i) for i in range(4)]
                kt = [sb("g_kt%d" % i, [128, CH], BF16, st) for i in range(4)]; r_kt = [RS("g_kt%d" % i) for i in range(4)]
                kh = [sb("g_kh%d" % i, [128, CH], BF16, st) for i in range(4)]; r_kh = [RS("g_kh%d" % i) for i in range(4)]
                Am = [sb("g_Am%d" % i, [CH, CH], BF16, st) for i in range(4)]; r_Am = [RS("g_Am%d" % i) for i in range(4)]
                khT = [sb("g_khT%d" % i, [CH, 128], BF16, st) for i in range(4)]; r_khT = [RS("g_khT%d" % i) for i in range(4)]
                Sf = [sb("g_S%d" % i, [128, 128], F32, st) for i in range(4)]; r_Sf = [RS("g_S%d" % i) for i in range(4)]
                Sb_ = [sb("g_Sb%d" % i, [128, 128], BF16, st) for i in range(4)]; r_Sb = [RS("g_Sb%d" % i) for i in range(4)]
                r_qs = RS("g_qs"); r_k = [RS("g_k0"), RS("g_k1")]; r_P = [RS("g_P0"), RS("g_P1")]; r_sgt = RS("g_sg")
                r_c = RS("g_const")
                pg.dma("sp", mUL[:, :], maskUL_d[:, :], W=[r_c])
                pg.dma("sp", hlb[:, :], hlbT_d[:, :], W=[r_c])
                pg.dma("sp", gn[:, :], hgn_d[l, :, :], W=[r_c])
                if l == 0:
                    pg.op("dve", lambda e: e.memset(lbt[:, 0:HG_H], 0.0), R=[r_c], W=[r_c])
                else:
                    pg.op("dve", lambda e: e.tensor_tensor(lbt[:, 0:HG_H], hlb[:, l * HG_H:(l + 1) * HG_H], hlb[:, 0:HG_H], ALU.subtract), R=[r_c], W=[r_c])
                    pg.op("act", lambda e: e.activation(lbt[:, 0:HG_H], lbt[:, 0:HG_H], AF.Sigmoid), R=[r_c], W=[r_c])
                pg.op("dve", lambda e: e.tensor_scalar(lbt[:, HG_H:2 * HG_H], lbt[:, 0:HG_H], -1.0, 1.0, ALU.mult, ALU.add), R=[r_c], W=[r_c])
                it = {"a": 0, "c": 0}
                for h in range(HG_H):
                    wt, rw = whg[h % 2], r_whg[h % 2]
                    for j, c0 in enumerate((C_HGQ, C_HGFF, C_HGFB, C_HGI, C_HGG)):
                        pg.dma("pool", wt[:, :, j * 128:(j + 1) * 128], w_in[l, :, c0 + h * 128:c0 + (h + 1) * 128].rearrange("(kc p) n -> p kc n", p=128), W=[rw])
                    for b in range(nb):
                        g0 = b * N
                        chunks = [(o, min(512, N - o)) for o in range(0, N, 512)]

                        def proj(j, o, n):
                            p, rp = nextpb()
                            for kc in range(8):
                                pg.op("pe", lambda e, p=p, kc=kc: e.matmul(p[:, 0:n], wt[:, kc, j * 128:(j + 1) * 128], hT[:, kc * T + g0 + o: kc * T + g0 + o + n],
                                                                         start=(kc == 0), stop=(kc == 7)), R=[rw, r_hT], W=[rp])
                            return p, rp
                        for (o, n) in chunks:
                            p, rp = proj(0, o, n)
                            pg.op("act", lambda e, p=p, o=o, n=n: e.activation(qs[:, o:o + n], p[:, 0:n], AF.Silu), R=[rp], W=[r_qs])
                            p, rp = proj(4, o, n)
                            pg.op("act", lambda e, p=p, o=o, n=n: e.activation(sgate[:, o:o + n], p[:, 0:n], AF.Silu), R=[rp], W=[r_sgt])
                            p, rp = proj(3, o, n)
                            pg.op("dve", lambda e, p=p, o=o, n=n: e.tensor_copy(vT[:, o:o + n], p[:, 0:n]), R=[rp], W=[r_vT])
                        for dirn in range(2):
                            kk, rk, P, rP, nP = kd[dirn], r_k[dirn], Pd[dirn], r_P[dirn], nPd[dirn]
                            for (o, n) in chunks:
                                p, rp = proj(1 + dirn, o, n)
                                a = it["a"] % 2; it["a"] += 1
                                pg.op("act", lambda e, p=p, a=a, n=n: e.activation(tmpa[a][:, 0:n], p[:, 0:n], AF.Sigmoid), R=[rp], W=[r_tmpa[a]])
                                pg.op("dve", lambda e, a=a, n=n: e.tensor_scalar(tmpb[a][:, 0:n], tmpa[a][:, 0:n], lbt[:, HG_H + h:HG_H + h + 1], lbt[:, h:h + 1], ALU.mult, ALU.add),
                                      R=[r_tmpa[a], r_c], W=[r_tmpb[a]])
                                pg.op("act", lambda e, a=a, o=o, n=n: e.activation(lf[:, o:o + n], tmpb[a][:, 0:n], AF.Ln), R=[r_tmpb[a]], W=[r_oacc2[1]])
                                pg.op("dve", lambda e, a=a, o=o, n=n, kk=kk: e.tensor_scalar(kk[:, o:o + n], tmpb[a][:, 0:n], -1.0, 1.0, ALU.mult, ALU.add),
                                      R=[r_tmpb[a]], W=[rk])
                            pg.op("dve", lambda e, P=P: e.memset(P[:, 0:1], 0.0), W=[rP])
                            pg.op("dve", lambda e, P=P: e.tensor_tensor_scan(P[:, 1:N + 1], lf[:, :], lf[:, :], 0.0, ALU.add, ALU.bypass), R=[r_oacc2[1], r_c], W=[rP])
                            pg.op("dve", lambda e, P=P, nP=nP: e.tensor_scalar(nP[:, :], P[:, 0:N + 1:CH], -1.0, None, ALU.mult), R=[rP], W=[rP])
                        orders = [list(range(NCH)), list(range(LCH - 1, -1, -1)) + list(range(NCH - 1, LCH - 1, -1))]
                        stt_ = [{"sc": 0, "n": 0}, {"sc": 0, "n": 0}]
                        for dirn in range(2):
                            pg.op("dve", lambda e: e.memset(Sf[2 * dirn][:, :], 0.0), W=[r_Sf[2 * dirn]])
                            pg.op("dve", lambda e: e.memset(Sb_[2 * dirn][:, :], 0.0), W=[r_Sb[2 * dirn]])

                        def step(dirn, c):
                            kk, rk, P, rP, nP = kd[dirn], r_k[dirn], Pd[dirn], r_P[dirn], nPd[dirn]
                            sd = stt_[dirn]
                            sc = 2 * dirn + sd["sc"]; sn = 2 * dirn + 1 - sd["sc"]
                            z = 2 * dirn + sd["n"] % 2; sd["n"] += 1
                            a0 = c * CH
                            need_out = not (last and c < LCH)
                            pvv = pbf[0:CH, 256 + dirn * 128:256 + (dirn + 1) * 128]
                            pg.op("pe", lambda e: e.transpose(pvv, vT[:, a0:a0 + CH], identb[:, :]), R=[r_vT, r_const], W=[r_pbf3[dirn]])
                            pg.op("act", lambda e: e.activation(vch[z][:, :], pvv, AF.Copy), R=[r_pbf3[dirn]], W=[r_vch[z]])
                            if dirn == 0:
                                src = P[:, a0 + 1:a0 + CH + 1]
                                specs = ((1.0, nP[:, c:c + 1]), (-1.0, P[:, a0:a0 + 1]), (-1.0, P[:, a0 + CH:a0 + CH + 1]))
                                ebe = ex[0][z][:, CH - 1:CH]; mk = mUL[0:CH, 0:CH]
                            else:
                                src = P[:, a0:a0 + CH]
                                specs = ((-1.0, P[:, a0 + CH:a0 + CH + 1]), (1.0, nP[:, c + 1:c + 2]), (1.0, nP[:, c:c + 1]))
                                ebe = ex[0][z][:, 0:1]; mk = mUL[0:CH, 64:64 + CH]
                            for j, (scl, bias) in enumerate(specs):
                                if j == 1 and not need_out:
                                    continue
                                pg.op("act", lambda e: e.activation(ex[j][z][:, :], src, AF.Exp, bias=bias, scale=scl), R=[rP], W=[r_ex[j][z]])
                            pg.op("dve", lambda e: e.scalar_tensor_tensor(qt[z][:, :], qs[:, a0:a0 + CH], HG_DK ** -0.5, ex[0][z][:, :], ALU.mult, ALU.mult),
                                  R=[r_qs, r_ex[0][z]], W=[r_qt[z]])
                            pg.op("dve", lambda e: e.tensor_tensor(kh[z][:, :], kk[:, a0:a0 + CH], ex[2][z][:, :], ALU.mult), R=[rk, r_ex[2][z]], W=[r_kh[z]])
                            if need_out:
                                pg.op("dve", lambda e: e.tensor_tensor(kt[z][:, :], kk[:, a0:a0 + CH], ex[1][z][:, :], ALU.mult), R=[rk, r_ex[1][z]], W=[r_kt[z]])
                                pA, rpA = nextpb()
                                pg.op("pe", lambda e: e.matmul(pA[0:CH, 0:CH], kt[z][:, :], qt[z][:, :], start=True, stop=True), R=[r_kt[z], r_qt[z]], W=[rpA])
                                pg.op("dve", lambda e: e.tensor_tensor(Am[z][:, :], pA[0:CH, 0:CH], mk, ALU.mult), R=[rpA, r_c], W=[r_Am[z]])
                            pbv = pbf[0:CH, dirn * 128:(dirn + 1) * 128]
                            pg.op("pe", lambda e: e.transpose(pbv, kh[z][:, :], identb[:, :]), R=[r_kh[z], r_const], W=[r_pbf2[dirn]])
                            pg.op("act", lambda e: e.activation(khT[z][:, :], pbv, AF.Copy), R=[r_pbf2[dirn]], W=[r_khT[z]])
                            if need_out:
                                pO, rpO = nextpb()
                                pg.op("pe", lambda e: e.matmul(pO[:, 0:CH], vch[z][:, :], Am[z][:, :], start=True, stop=False), R=[r_vch[z], r_Am[z]], W=[rpO])
                                pg.op("pe", lambda e: e.matmul(pO[:, 0:CH], Sb_[sc][:, :], qt[z][:, :], start=False, stop=True), R=[r_Sb[sc], r_qt[z]], W=[rpO])
                                pg.op("dve", lambda e: e.tensor_copy(oacc2[dirn][:, a0:a0 + CH], pO[:, 0:CH]), R=[rpO], W=[r_oacc2[dirn]])
                            pS, rpS = nextpb()
                            pg.op("pe", lambda e: e.matmul(pS[:, 0:128], khT[z][:, :], vch[z][:, :], start=True, stop=True), R=[r_khT[z], r_vch[z]], W=[rpS])
                            pg.op("dve", lambda e: e.scalar_tensor_tensor(Sf[sn][:, :], Sf[sc][:, :], ebe, pS[:, 0:128], ALU.mult, ALU.add),
                                  R=[r_Sf[sc], rpS, r_ex[0][z]], W=[r_Sf[sn]])
                            pg.op("act", lambda e: e.activation(Sb_[sn][:, :], Sf[sn][:, :], AF.Copy), R=[r_Sf[sn]], W=[r_Sb[sn]])
                            sd["sc"] = 1 - sd["sc"]

                        for idx in range(NCH):
                            step(0, orders[0][idx])
                            step(1, orders[1][idx])
                        r0 = LC if last else 0
                        pg.op("pool", lambda e: e.tensor_tensor(oacc[:, r0:N], oacc2[0][:, r0:N], oacc2[1][:, r0:N], ALU.add), R=[r_oacc2[1]], W=[r_oacc2[0]])
                        r0 = LC if last else 0
                        for ci, (o, n) in enumerate([(o, min(512, N - o)) for o in range(r0, N, 512)]):
                            a = ci % 2
                            pg.op("act", lambda e, a=a, o=o, n=n: e.activation(sqb[a][:, 0:n], oacc[:, o:o + n], AF.Square), R=[r_oacc2[0]], W=[r_sqb[a]])
                            p, rp = nextpb()
                            pg.op("pe", lambda e, p=p, a=a, n=n: e.matmul(p[:, 0:n], onesb[:, :], sqb[a][:, 0:n], start=True, stop=True), R=[r_sqb[a], r_const], W=[rp])
                            pg.op("act", lambda e, p=p, a=a, n=n: e.activation(tmpa[a][:, 0:n], p[:, 0:n], AF.Ln, scale=1.0 / 128, bias=EPS), R=[rp], W=[r_tmpa[a]])
                            pg.op("act", lambda e, a=a, n=n: e.activation(tmpa[a][:, 0:n], tmpa[a][:, 0:n], AF.Exp, scale=-0.5), R=[r_tmpa[a]], W=[r_tmpa[a]])
                            pg.op("dve", lambda e, a=a, o=o, n=n: e.scalar_tensor_tensor(tmpb[a][:, 0:n], oacc[:, o:o + n], gn[:, 0:1], tmpa[a][:, 0:n], ALU.mult, ALU.mult),
                                  R=[r_oacc2[0], r_tmpa[a], r_c], W=[r_tmpb[a]])
                            pg.op("dve", lambda e, a=a, o=o, n=n: e.tensor_tensor(yo[a][:, 0:n], tmpb[a][:, 0:n], sgate[:, o:o + n], ALU.mult), R=[r_tmpb[a], r_sgt], W=[r_yo[a]])
                            pg.dma("sp", yT_d[256 + h * 128:256 + (h + 1) * 128, g0 + o:g0 + o + n], yo[a][:, 0:n], R=[r_yo[a]], W=[r_yT])

        Gall_d = nc.dram_tensor("Gall_d", [128, NT * NE], F32); GT_d = nc.dram_tensor("GT_d", [NE, T], BF16)
        r_Gall = RS("Gall"); r_GT = RS("GT"); r_Gd = RS("G_d")

        def phaseH(l, last):
            with ExitStack() as st:
                norm_setup(st)
                Gall = sb("Gall", [128, NT * NE], F32, st); GT = sb("GT", [NE, T], BF16, st)
                pg.op("dve", lambda e: e.memset(Gall[:, :], 0.0), W=[r_Gall])
                pg.op("dve", lambda e: e.memset(GT[:, :], 0.0), W=[r_GT])
                wo = sb("wo", [128, 8, 1024], BF16, st); r_wo = RS("wo")
                Wr = sb("Wr", [128, 8, NE], F32, st); Wrp = sb("Wrp", [128, NB1 * 8 * NE], F32, st); rbt = sb("rbt", [1, NE], F32, st)
                rbias = sb("rbias", [1, NB1 * NE], F32, st); r_r = RS("router")
                G1t = sb("G1t", [128, NB1, 1024], F32, st); r_G1 = RS("G1t")
                yts = [sb("h_yt%d" % i, [128, 8, 128], BF16, st) for i in range(2)]; r_yts = [RS("h_yt%d" % i) for i in range(2)]
                xts = [sb("h_xt%d" % i, [128, 1024], F32, st) for i in range(2)]; r_xts = [RS("h_xt%d" % i) for i in range(2)]
                tmp = [sb("h_tmp%d" % i, [128, 512], F32, st) for i in range(2)]; r_tmp = [RS("h_tmp%d" % i) for i in range(2)]
                xnT = sb("h_xnT", [128, 1024], F32, st); r_xnT = RS("h_xnT")
                lg = [sb("h_lg%d" % i, [128, NE], F32, st) for i in range(2)]; r_lg = [RS("h_lg%d" % i) for i in range(2)]
                m8 = [sb("h_m8%d" % i, [128, 16], F32, st) for i in range(2)]
                ee = [sb("h_ee%d" % i, [128, 2 * NE], F32, st) for i in range(2)]
                for kc in range(8):
                    pg.dma("pool", wo[:, kc, :], w_out[l, kc * 128:(kc + 1) * 128, :], W=[r_wo])
                pg.dma("sp", Wr[:, :, :], rw_d[l, :, :].rearrange("(kc p) n -> p kc n", p=128), W=[r_r])
                pg.dma("sp", rbt[:, :], rb_d[l, :, :], W=[r_r])
                for b in range(NB1):
                    pg.dma("sp", G1t[:, b, :], Gt_d[b * 2 + 0, :, :], R=[r_Gt], W=[r_G1])
                    for kc in range(8):
                        ca = (2 * 8 + kc) * NB1 + b
                        pg.op("dve", lambda e: e.tensor_scalar(Wrp[:, (b * 8 + kc) * NE:(b * 8 + kc + 1) * NE], Wr[:, kc, :], MV[:, ca:ca + 1], None, ALU.mult),
                              R=[r_r, r_MV], W=[r_r])
                    p, rp = nextpb()
                    for kc in range(8):
                        cb_ = (3 * 8 + kc) * NB1 + b
                        pg.op("pe", lambda e: e.matmul(p[0:1, 0:NE], MV[:, cb_:cb_ + 1], Wr[:, kc, :], start=(kc == 0), stop=False), R=[r_r, r_MV], W=[rp])
                    pg.op("pe", lambda e: e.matmul(p[0:1, 0:NE], onesf[0:1, 0:1], rbt[0:1, :], start=False, stop=True), R=[r_r, r_const], W=[rp])
                    pg.op("act", lambda e: e.activation(rbias[0:1, b * NE:(b + 1) * NE], p[0:1, 0:NE], AF.Copy), R=[rp], W=[r_r])
                tiles = [i for i in range(NT) if not (last and tile_info(i)[2])]
                for n_, i in enumerate(tiles):
                    b, pos0, isc = tile_info(i)
                    bm = nb if isc else b
                    k = n_ % 2
                    yt, r_yt, xt, r_xt = yts[k], r_yts[k], xts[k], r_xts[k]
                    pg.dma("sp", yt[:, :, :], yT_d[:, i * 128:(i + 1) * 128].rearrange("(kc p) t -> p kc t", p=128), R=[r_yT], W=[r_yt])
                    pg.dma("sp", xt[:, :], xres[i * 128:(i + 1) * 128, :], R=[r_xres[i]], W=[r_xt])
                    for half in range(2):
                        p, rp = nextpb()
                        for kc in range(8):
                            pg.op("pe", lambda e: e.matmul(p[:, :], yt[:, kc, :], wo[:, kc, half * 512:(half + 1) * 512], start=(kc == 0), stop=(kc == 7)),
                                  R=[r_yt, r_wo], W=[rp])
                        pg.op("dve", lambda e: e.tensor_tensor(tmp[half][:, :], p[:, :], G1t[:, bm, half * 512:(half + 1) * 512], ALU.mult), R=[rp, r_G1], W=[r_tmp[half]])
                        pg.op("pool", lambda e: e.tensor_tensor(xt[:, half * 512:(half + 1) * 512], xt[:, half * 512:(half + 1) * 512], tmp[half][:, :], ALU.add),
                              R=[r_tmp[half], r_xt], W=[r_xt])
                    pg.dma("sp", xres[i * 128:(i + 1) * 128, :], xt[:, :], R=[r_xt], W=[r_xres[i]])
                    norm_mod_T(i, xt, r_xt, 2, keep_xnT=(xnT, r_xnT))
                    p, rp = nextpb()
                    for kc in range(8):
                        pg.op("pe", lambda e: e.matmul(p[:, 0:NE], xnT[:, kc * 128:(kc + 1) * 128], Wrp[:, (bm * 8 + kc) * NE:(bm * 8 + kc + 1) * NE], start=(kc == 0), stop=False),
                              R=[r_xnT, r_r], W=[rp])
                    pg.op("pe", lambda e: e.matmul(p[:, 0:NE], onesf[0:1, :], rbias[0:1, bm * NE:(bm + 1) * NE], start=False, stop=True), R=[r_r, r_const], W=[rp])
                    L_, rL = lg[k], r_lg[k]
                    M_, E_ = m8[k], ee[k]
                    pg.op("act", lambda e: e.activation(L_[:, :], p[:, 0:NE], AF.Copy), R=[rp], W=[rL])
                    pg.op("dve", lambda e: e.max(M_[:, 0:8], L_[:, :]), R=[rL], W=[rL])
                    pg.op("dve", lambda e: e.tensor_scalar(M_[:, 8:9], M_[:, 0:1], -1.0, None, ALU.mult), R=[rL], W=[rL])
                    pg.op("act", lambda e: e.activation(E_[:, 0:NE], L_[:, :], AF.Exp, bias=M_[:, 8:9], scale=1.0), R=[rL], W=[rL])
                    pg.op("dve", lambda e: e.tensor_scalar(E_[:, NE:2 * NE], L_[:, :], M_[:, TOPK - 1:TOPK], None, ALU.is_ge), R=[rL], W=[rL])
                    pg.op("dve", lambda e: e.tensor_tensor(E_[:, 0:NE], E_[:, 0:NE], E_[:, NE:2 * NE], ALU.mult), R=[rL], W=[rL])
                    pg.op("dve", lambda e: e.reduce_sum(M_[:, 9:10], E_[:, 0:NE], AX.X), R=[rL], W=[rL])
                    pg.op("dve", lambda e: e.reciprocal(M_[:, 9:10], M_[:, 9:10]), R=[rL], W=[rL])
                    pg.op("dve", lambda e: e.tensor_scalar(Gall[:, i * NE:(i + 1) * NE], E_[:, 0:NE], M_[:, 9:10], None, ALU.mult), R=[rL], W=[r_Gall])
                    p2, rp2 = nextpb()
                    pg.op("pe", lambda e: e.transpose(p2[0:NE, 0:128], Gall[:, i * NE:(i + 1) * NE], identf[:, :]), R=[r_Gall, r_const], W=[rp2])
                    pg.op("act", lambda e: e.activation(GT[:, i * 128:(i + 1) * 128], p2[0:NE, 0:128], AF.Copy), R=[rp2], W=[r_GT])
                for kc in range(8):
                    pg.dma("sp", h2T_d[:, kc, :], hT[:, kc * T:(kc + 1) * T], R=[r_hT], W=[r_h2])
                pg.dma("sp", Gall_d[:, :], Gall[:, :], R=[r_Gall], W=[r_Gd])
                pg.dma("sp", GT_d[:, :], GT[:, :], R=[r_GT], W=[r_Gd])

        def phaseI(l, last):
            TGT = cfg.TG // 128
            banks = [(pb[i], r_pb[i]) for i in range(5)] + [(psT[:, 0:512], RS("psTa")), (psT[:, 512:1024], RS("psTb"))]
            rotI = [0]

            def nextI():
                i = rotI[0]; rotI[0] = (i + 1) % len(banks)
                return banks[i]
            with ExitStack() as st:
                Gall = sb("Gall", [128, NT * NE], F32, st); GT = sb("GT", [NE, T], BF16, st)
                pg.dma("sp", Gall[:, :], Gall_d[:, :], R=[r_Gd], W=[r_Gall])
                pg.dma("sp", GT[:, :], GT_d[:, :], R=[r_Gd], W=[r_GT])
                acc = sb("i_acc", [128, TGT, 1024], F32, st); r_acc = [RS("i_acc%d" % j) for j in range(TGT)]
                h2g = sb("i_h2g", [128, 8, cfg.TG], BF16, st); r_h2g = RS("i_h2g")
                actT = sb("i_actT", [128, FC, cfg.TG], BF16, st); r_actT = RS("i_actT")
                wd = [sb("i_wd%d" % i, [128, FC, 1024], BF16, st) for i in range(2)]; r_wd = [RS("i_wd%d" % i) for i in range(2)]
                wgp = [sb("i_wg%d" % i, [128, 8, 256], BF16, st) for i in range(4)]; r_wgp = [RS("i_wg%d" % i) for i in range(4)]
                bgu = sb("i_bgu", [128, NE * 2 * FC], F32, st); bdf = sb("i_bdf", [NE, 1024], F32, st); bdb = sb("i_bdb", [NE, 1024], BF16, st); r_b = RS("i_bias")
                G2t = sb("i_G2t", [128, NB1, 1024], F32, st); r_G2 = RS("i_G2t")
                tg = [[sb("i_t%d%d" % (j, i), [128, 512], F32, st) for i in range(2)] for j in range(5)]
                r_tg = [[RS("i_t%d%d" % (j, i)) for i in range(2)] for j in range(5)]
                xts = [sb("i_xt%d" % i, [128, 1024], F32, st) for i in range(2)]; r_xts = [RS("i_xt%d" % i) for i in range(2)]
                pg.dma("sp", bgu[:, :], bguT_d[l, :, :], W=[r_b])
                pg.dma("sp", bdf[:, :], bdn_d[l, :, :], W=[r_b])
                pg.op("act", lambda e: e.activation(bdb[:, :], bdf[:, :], AF.Copy), R=[r_b], W=[r_b])
                for b in range(NB1):
                    pg.dma("sp", G2t[:, b, :], Gt_d[b * 2 + 1, :, :], R=[r_Gt], W=[r_G2])
                tiles = [i for i in range(NT) if not (last and tile_info(i)[2])]
                groups = [tiles[i:i + TGT] for i in range(0, len(tiles), TGT)]
                cnt = {"w": 0, "d": 0, "t": 0, "x": 0}
                for grp in groups:
                    ng = len(grp); ntok = ng * 128
                    for j, i in enumerate(grp):
                        pg.dma("sp", h2g[:, :, j * 128:(j + 1) * 128], h2T_d[:, :, i * 128:(i + 1) * 128], R=[r_h2], W=[r_h2g])
                        for half in range(2):
                            p, rp = nextI()
                            pg.op("pe", lambda e: e.matmul(p[:, :], GT[:, i * 128:(i + 1) * 128], bdb[:, half * 512:(half + 1) * 512], start=True, stop=True),
                                  R=[r_GT, r_b], W=[rp])
                            pg.op("act", lambda e: e.activation(acc[:, j, half * 512:(half + 1) * 512], p[:, :], AF.Copy), R=[rp], W=[r_acc[j]])
                    tts = [(o, min(512, ntok - o)) for o in range(0, ntok, 512)]
                    for ex_ in range(NE):
                        wdt, rwd = wd[cnt["d"] % 2], r_wd[cnt["d"] % 2]; cnt["d"] += 1
                        for fc in range(FC):
                            pg.dma("pool", wdt[:, fc, :], wdn_d[l, ex_, fc * 128:(fc + 1) * 128, :], W=[rwd])
                        for fc in range(FC):
                            wg, rwg = wgp[cnt["w"] % 4], r_wgp[cnt["w"] % 4]; cnt["w"] += 1
                            for (c0, o_) in ((fc * 128, 0), (DFF + fc * 128, 128)):
                                pg.dma("pool", wg[:, :, o_:o_ + 128], wgu_d[l, ex_, :, c0:c0 + 128].rearrange("(kc p) n -> p kc n", p=128), W=[rwg])
                            cg = ex_ * 2 * FC + fc; cl = ex_ * 2 * FC + FC + fc
                            for (o, n) in tts:
                                z = cnt["t"] % 2; cnt["t"] += 1
                                pgl, rpgl = nextI()
                                for kc in range(8):
                                    pg.op("pe", lambda e: e.matmul(pgl[:, 0:n], wg[:, kc, 0:128], h2g[:, kc, o:o + n], start=(kc == 0), stop=(kc == 7)), R=[rwg, r_h2g], W=[rpgl])
                                pli, rpli = nextI()
                                for kc in range(8):
                                    pg.op("pe", lambda e: e.matmul(pli[:, 0:n], wg[:, kc, 128:256], h2g[:, kc, o:o + n], start=(kc == 0), stop=(kc == 7)), R=[rwg, r_h2g], W=[rpli])
                                glu, sg_, linb, linc, tt_ = (tg[j][z] for j in range(5))
                                rglu, rsg, rlinb, rlinc, rtt = (r_tg[j][z] for j in range(5))
                                pg.op("dve", lambda e: e.tensor_scalar(glu[:, 0:n], pgl[:, 0:n], bgu[:, cg:cg + 1], 7.0, ALU.add, ALU.min), R=[rpgl, r_b], W=[rglu])
                                pg.op("act", lambda e: e.activation(sg_[:, 0:n], glu[:, 0:n], AF.Sigmoid, scale=1.702), R=[rglu], W=[rsg])
                                pg.op("act", lambda e: e.activation(linb[:, 0:n], pli[:, 0:n], AF.Identity, bias=bgu[:, cl:cl + 1], scale=1.0), R=[rpli, r_b], W=[rlinb])
                                pg.op("pool", lambda e: e.tensor_scalar(linc[:, 0:n], linb[:, 0:n], 7.0, -7.0, ALU.min, ALU.max), R=[rlinb], W=[rlinc])
                                pg.op("dve", lambda e: e.tensor_tensor(tt_[:, 0:n], glu[:, 0:n], sg_[:, 0:n], ALU.mult), R=[rglu, rsg], W=[rtt])
                                pg.op("dve", lambda e: e.scalar_tensor_tensor(actT[:, fc, o:o + n], linc[:, 0:n], 1.0, tt_[:, 0:n], ALU.add, ALU.mult), R=[rlinc, rtt], W=[r_actT])
                        for j, i in enumerate(grp):
                            for half in range(2):
                                p, rp = nextI()
                                for fc in range(FC):
                                    pg.op("pe", lambda e: e.matmul(p[:, :], actT[:, fc, j * 128:(j + 1) * 128], wdt[:, fc, half * 512:(half + 1) * 512], start=(fc == 0), stop=(fc == FC - 1)),
                                          R=[r_actT, rwd], W=[rp])
                                pg.op("dve", lambda e: e.scalar_tensor_tensor(acc[:, j, half * 512:(half + 1) * 512], p[:, :], Gall[:, i * NE + ex_: i * NE + ex_ + 1],
                                                                            acc[:, j, half * 512:(half + 1) * 512], ALU.mult, ALU.add), R=[rp, r_Gall, r_acc[j]], W=[r_acc[j]])
                    for j, i in enumerate(grp):
                        b, pos0, isc = tile_info(i)
                        bm = nb if isc else b
                        k = cnt["x"] % 2; cnt["x"] += 1
                        xt, r_xt = xts[k], r_xts[k]
                        pg.dma("sp", xt[:, :], xres[i * 128:(i + 1) * 128, :], R=[r_xres[i]], W=[r_xt])
                        pg.op("dve", lambda e: e.tensor_tensor(acc[:, j, :], acc[:, j, :], G2t[:, bm, :], ALU.mult), R=[r_acc[j], r_G2], W=[r_acc[j]])
                        pg.op("pool", lambda e: e.tensor_tensor(xt[:, :], xt[:, :], acc[:, j, :], ALU.add), R=[r_acc[j], r_xt], W=[r_xt])
                        if last:
                            row = b * L + (pos0 - LC)
                            pg.dma("sp", y_d[row:row + 128, :], xt[:, :], R=[r_xt], W=[r_y])
                        else:
                            pg.dma("sp", xres[i * 128:(i + 1) * 128, :], xt[:, :], R=[r_xt], W=[r_xres[i]])

        stop_after = getattr(cfg, "stop_after", None)
        def bar():
            pg.barrier((tok_d[0:1, :], ident_d[0:1, 0:64]), r_bar)

        for l in range(dp):
            last = (l == dp - 1)
            with ExitStack() as stL:
                hT = sb("hT%d" % l, [128, 8 * T], BF16, stL)
                phaseA(l); bar()
                phaseB(l); bar()
                phaseC(l); bar()
                phaseEF(l, last); bar()
                phaseG(l, last); bar()
                phaseH(l, last); bar()
            phaseI(l, last); bar()
        pg.finish([r_y] + list(r_dbg.values()))
        nc._n_sems = pg.nsem
        nc._minrem = minrem[0]
        nc._pg = pg
    return nc


_CACHE = {}


def kernel(**inputs):
    n_cores = 8
    B = inputs["x"].shape[0]
    cfg = Cfg(nb=B // n_cores, L=inputs["x"].shape[1], LC=inputs["ctx"].shape[1], NE=inputs["w_gu"].shape[1],
              DFF=inputs["w_down"].shape[2], depth=inputs["w_in"].shape[0], TG=1024)
    nc = build_nc(cfg)
    in_maps = []
    shared = None
    for core in range(n_cores):
        m, shared = prep_inputs(cfg, core, shared=shared, **inputs)
        in_maps.append(m)
    res = run_bass_kernel_spmd(nc, in_maps, core_ids=list(range(n_cores)))
    outs = [np.asarray(r["y"], np.float32).reshape(cfg.nb, cfg.L, D) for r in res.results]
    return np.concatenate(outs, axis=0)
```

```python
import numpy as np
from contextlib import ExitStack
import concourse.bass as bass
import concourse.mybir as mybir
from concourse.bass_utils import run_bass_kernel_spmd

F32 = mybir.dt.float32
BF16 = mybir.dt.bfloat16
AF = mybir.ActivationFunctionType
ALU = mybir.AluOpType
AX = mybir.AxisListType

D = 1024
GRID_W = 64
HD = 64
NA_H = 4
HG_H = 4
HG_DK = 128
SW_QH = 4
SW_KVH = 2
IN_COLS = 3840
EPS = 1e-6
NEG = -1e30
TOPK = 4
C_NAQ, C_NAK, C_NAV = 0, 256, 512
C_HGQ, C_HGFF, C_HGFB, C_HGI, C_HGG = 768, 1280, 1792, 2304, 2816
C_SWQ, C_SWK, C_SWV = 3328, 3584, 3712


class Cfg:
    def __init__(self, nb=2, L=2048, LC=256, NE=32, DFF=1024, depth=2, TG=1536):
        self.nb, self.L, self.LC, self.NE, self.DFF, self.depth, self.TG = nb, L, LC, NE, DFF, depth, TG
        self.N = L + LC
        self.T = nb * self.N
        self.R = L // GRID_W


class Res:
    __slots__ = ("name", "w", "rs", "sem", "cnt", "swq", "used")

    def __init__(self, name):
        self.name, self.w, self.rs, self.sem, self.cnt, self.used = name, {}, {}, None, 0, False


class _Rec:
    def __getattr__(self, name):
        def f(*a, **k):
            self.call = (name, a, k)
            return self
        return f


class _Eng:
    def __init__(self, key, sem):
        self.key, self.sem, self.count, self.ops, self.last, self.pending, self.waited = key, sem, 0, [], None, False, {}


class Prog:
    def __init__(self, nc, es):
        self.nc, self.es = nc, es
        self.eng = {}
        for k in ("pe", "act", "dve", "pool", "sp"):
            self.eng[k] = _Eng(k, es.enter_context(nc.semaphore("e_" + k)))
        self.semown = {id(e.sem): e for e in self.eng.values()}
        self.nsem = 5
        self.dma_res = []
        self.sem_pool = {}
        self.all_res = []
        self.nres = 0

    def res(self, name):
        self.nres += 1
        r = Res(name)
        self.all_res.append(r)
        return r

    def _wait(self, E, ev):
        if ev is None:
            return
        sem, val = ev
        own = self.semown.get(id(sem))
        if own is not None and own is E and E.key == "pe":
            return
        if own is not None and val > own.count:
            assert val == own.count + 1 and own.pending, (own.key, val, own.count)
            own.last["inc"] = True
            own.count += 1
            own.pending = False
        if E.waited.get(id(sem), 0) >= val:
            return
        E.waited[id(sem)] = val
        E.ops.append({"wait": (sem, val)})

    def _deps(self, E, R, W, dma_write=False):
        for r in R:
            for ev in list(r.w.values()):
                self._wait(E, ev)
        for w in W:
            for ev in list(w.w.values()):
                if dma_write and id(ev[0]) not in self.semown:
                    continue
                self._wait(E, ev)
            for ev in list(w.rs.values()):
                self._wait(E, ev)

    def _commit(self, ev, R, W, dma_write=False):
        for r in R:
            old = r.rs.get(id(ev[0]))
            if old is None or old[1] < ev[1]:
                r.rs[id(ev[0])] = ev
        for w in W:
            if dma_write:
                w.w = {k: v for k, v in w.w.items() if k not in self.semown}
                w.w[id(ev[0])] = ev
            else:
                w.w = {id(ev[0]): ev}
            w.rs = {}

    def op(self, ek, fn, R=(), W=()):
        E = self.eng[ek]
        self._deps(E, R, W)
        rec = _Rec()
        fn(rec)
        ent = {"fn": rec.call, "inc": False}
        E.ops.append(ent)
        E.last = ent
        E.pending = True
        self._commit((E.sem, E.count + 1), R, W)

    def dma(self, qk, out, in_, R=(), W=(), **kw):
        E = self.eng[qk]
        is_store = ("DRam" in type(out.tensor).__name__) and ("DRam" not in type(in_.tensor).__name__)
        own = R[0] if is_store else W[0]
        if own.sem is None:
            pool = self.sem_pool.setdefault(qk == "pool", [])
            own.swq = (qk == "pool")
            if pool:
                own.sem, own.cnt = pool.pop()
            else:
                own.sem, own.cnt = self.es.enter_context(self.nc.semaphore("d_%d" % self.nsem)), 0
                self.nsem += 1
            self.dma_res.append(own)
        self._deps(E, R, W, dma_write=True)
        if own.sem is not None and getattr(own, "used", False):
            if is_store:
                cont = id(own.sem) in own.rs
            else:
                cont = (not own.rs) and all(k not in self.semown for k in own.w)
            if not cont:
                self._wait(E, (own.sem, own.cnt))
        own.used = True
        own.cnt += 16
        E.ops.append({"dma": (out, in_, kw, own.sem)})
        self._commit((own.sem, own.cnt), R, W, dma_write=True)

    def barrier(self, tok_d, r_bar):
        sp = self.eng["sp"]
        for k in ("pe", "act", "dve", "pool"):
            X = self.eng[k]
            if X.pending:
                self._wait(sp, (X.sem, X.count + 1))
            elif X.count > 0:
                self._wait(sp, (X.sem, X.count))
        for r in self.dma_res:
            if r is not r_bar:
                self._wait(sp, (r.sem, r.cnt))
        if r_bar.sem is not None:
            self._wait(sp, (r_bar.sem, r_bar.cnt))
        self.dma("sp", tok_d[0], tok_d[1], W=[r_bar])
        for r in self.dma_res:
            if r.swq:
                self._wait(self.eng["pool"], (r.sem, r.cnt))
        for k in ("pe", "act", "dve", "pool"):
            for ev in list(r_bar.w.values()):
                self._wait(self.eng[k], ev)
        for r in self.all_res:
            r.w = {}
            r.rs = {}
            if r.sem is not None and r is not r_bar:
                self.sem_pool[r.swq].append((r.sem, r.cnt))
                r.sem = None
                r.used = False
        self.dma_res = []

    def finish(self, outs):
        E = self.eng["sp"]
        for r in outs:
            for ev in list(r.w.values()):
                self._wait(E, ev)
        nc = self.nc
        with nc.Block() as block:
            def run(E, e):
                for o in E.ops:
                    if "wait" in o:
                        e.wait_ge(o["wait"][0], o["wait"][1])
                    elif "dma" in o:
                        out, in_, kw, sem = o["dma"]
                        e.dma_start(out=out, in_=in_, **kw).then_inc(sem, 16)
                    else:
                        nm_, a_, k_ = o["fn"]
                        ins = getattr(e, nm_)(*a_, **k_)
                        if o["inc"]:
                            ins.then_inc(E.sem, 1)

            @block.sync
            def _(e):
                run(self.eng["sp"], e)

            @block.tensor
            def _(e):
                run(self.eng["pe"], e)

            @block.scalar
            def _(e):
                run(self.eng["act"], e)

            @block.vector
            def _(e):
                run(self.eng["dve"], e)

            @block.gpsimd
            def _(e):
                run(self.eng["pool"], e)


def _fm(v, p=128):
    sh = v.shape
    return np.ascontiguousarray(np.swapaxes(v.reshape(sh[:-1] + (sh[-1] // p, p)), -1, -2))


def _na_bias_table(rpb):
    H = rpb.shape[0]
    kc = np.arange(64)[:, None]
    qc = np.arange(64)[None, :]
    qs = np.clip(qc - 8, 0, 48)
    ok = (kc >= qs) & (kc < qs + 16)
    dc = np.clip(kc - qc + 15, 0, 30)
    g = rpb[:, :, dc]
    g = np.where(ok[None, None], g, np.float32(NEG)).astype(np.float32)
    tab = np.empty((2, 64, H, 14, 64), np.float32)
    for dl in range(2):
        tab[dl] = np.transpose(g[:, dl:dl + 14], (2, 0, 1, 3))
    return np.ascontiguousarray(tab.reshape(128, H * 14 * 64))


def _consts(cfg):
    L = cfg.L
    c = {}
    c["ident"] = np.eye(128, dtype=np.float32)
    s = np.arange(64)[:, None]
    t = np.arange(64)[None, :]
    c["maskUL"] = np.concatenate([(s <= t), (s >= t)], axis=1).astype(np.float32)
    kp = np.arange(128)[:, None]
    qp = np.arange(128)[None, :]
    ml = np.where(kp >= qp, 0.0, NEG)
    mr = np.where(kp <= qp, 0.0, NEG)
    c["swam"] = np.concatenate([ml, mr], axis=1).astype(np.float32)
    tok = np.arange(L)
    row = (tok // GRID_W).astype(np.float64)
    col = (tok % GRID_W).astype(np.float64)
    inv = 10000.0 ** (-np.arange(16, dtype=np.float64) / 16)
    C = np.zeros((64, L)); S = np.zeros((64, L))
    for d in range(64):
        pos = row if d < 32 else col
        ang = pos * inv[d % 16]
        C[d] = np.cos(ang)
        S[d] = -np.sin(ang) if (d % 32) < 16 else np.sin(ang)
    c["ropeC"] = np.concatenate([C, C], 0).astype(np.float32)
    c["ropeS"] = np.concatenate([S, S], 0).astype(np.float32)
    P = np.zeros((128, 128), np.float32)
    for do in range(128):
        dd = do % 32
        di = do + 16 if dd < 16 else do - 16
        P[di, do] = 1.0
    c["ropeP"] = P
    o2 = np.zeros((128, 128), np.float32)
    o2[:64, :64] = 1.0
    o2[64:, 64:] = 1.0
    c["ones2"] = o2
    return c


def prep_inputs(cfg, core, x, c, ctx, c_ctx, hg_lower_bounds, ada_w, ada_b, norm1_g, norm2_g, w_in, na_q_norm,
                na_k_norm, na_rpb, hg_norm_g, swa_q_norm, swa_k_norm, swa_sink, w_out, router_w, router_b,
                w_gu, b_gu, w_down, b_down, shared=None):
    nb, dp = cfg.nb, cfg.depth
    f = np.float32
    b0 = core * nb
    m = {}
    m["x"] = np.ascontiguousarray(x[b0:b0 + nb].reshape(nb * cfg.L, D), f)
    m["ctx"] = np.ascontiguousarray(ctx[b0:b0 + nb].reshape(nb * cfg.LC, D), f)
    cc = np.concatenate([c[b0:b0 + nb], c_ctx[None]], 0).astype(f)
    m["ccT"] = np.ascontiguousarray(np.transpose(cc.reshape(nb + 1, 8, 128), (2, 1, 0)).reshape(128, 8 * (nb + 1)))
    if shared is None:
        shared = {}
        shared["ada_w"] = np.ascontiguousarray(ada_w, f)
        shared["ada_bT"] = _fm(ada_b.astype(f))
        shared["ada_b"] = np.ascontiguousarray(ada_b.astype(f).reshape(dp, 1, 6 * D))
        shared["n1T"] = _fm(norm1_g.astype(f))
        shared["n2T"] = _fm(norm2_g.astype(f))
        shared["w_in"] = np.ascontiguousarray(w_in, f)
        shared["w_out"] = np.ascontiguousarray(w_out, f)
        g4 = np.stack([np.tile(na_q_norm, (1, 2)), np.tile(na_k_norm, (1, 2)), np.tile(swa_q_norm, (1, 2)),
                       np.tile(swa_k_norm, (1, 2))], axis=-1)
        shared["gains4"] = np.ascontiguousarray(g4, f)
        shared["nab"] = np.stack([_na_bias_table(na_rpb[l].astype(f)) for l in range(dp)])
        shared["hlbT"] = np.ascontiguousarray(np.transpose(hg_lower_bounds.astype(f).reshape(dp, HG_H, 128), (2, 0, 1)).reshape(128, dp * HG_H))
        shared["hgn"] = np.ascontiguousarray(hg_norm_g.astype(f).reshape(dp, 128, 1))
        shared["sink"] = np.ascontiguousarray(np.broadcast_to(swa_sink.astype(f)[:, None, :], (dp, 128, SW_QH)))
        shared["router_w"] = np.ascontiguousarray(router_w, f)
        shared["router_b"] = np.ascontiguousarray(router_b.astype(f).reshape(dp, 1, cfg.NE))
        shared["w_gu"] = np.ascontiguousarray(w_gu, f)
        shared["b_guT"] = np.ascontiguousarray(_fm(b_gu.astype(f)).transpose(0, 2, 1, 3).reshape(dp, 128, -1))
        shared["w_down"] = np.ascontiguousarray(w_down, f)
        shared["b_down"] = np.ascontiguousarray(b_down, f)
        shared.update(_consts(cfg))
    m.update(shared)
    return m, shared


def build_nc(cfg, dbg=()):
    nc = bass.Bass("TRN2", target_bir_lowering=False)
    nb, L, LC, N, T, NE, DFF, dp, R = cfg.nb, cfg.L, cfg.LC, cfg.N, cfg.T, cfg.NE, cfg.DFF, cfg.depth, cfg.R
    NB1 = nb + 1
    NT = T // 128
    FC = DFF // 128
    ins = {}

    def din(name, shape):
        ins[name] = nc.dram_tensor(name, list(shape), F32, kind="ExternalInput")
        return ins[name]

    x_d = din("x", [nb * L, D]); ctx_d = din("ctx", [nb * LC, D]); ccT_d = din("ccT", [128, 8 * NB1])
    ada_w = din("ada_w", [dp, D, 6 * D]); ada_bT = din("ada_bT", [dp, 128, 48]); ada_b = din("ada_b", [dp, 1, 6 * D])
    n1T_d = din("n1T", [dp, 128, 8]); n2T_d = din("n2T", [dp, 128, 8])
    w_in = din("w_in", [dp, D, IN_COLS]); w_out = din("w_out", [dp, D, D])
    gains4 = din("gains4", [dp, 128, 4]); nab_d = din("nab", [dp, 128, NA_H * 14 * 64])
    hlbT_d = din("hlbT", [128, dp * HG_H]); hgn_d = din("hgn", [dp, 128, 1]); sink_d = din("sink", [dp, 128, SW_QH])
    rw_d = din("router_w", [dp, D, NE]); rb_d = din("router_b", [dp, 1, NE])
    wgu_d = din("w_gu", [dp, NE, D, 2 * DFF]); bguT_d = din("b_guT", [dp, 128, NE * 2 * FC])
    wdn_d = din("w_down", [dp, NE, DFF, D]); bdn_d = din("b_down", [dp, NE, D])
    ident_d = din("ident", [128, 128]); maskUL_d = din("maskUL", [64, 128]); swam_d = din("swam", [128, 256])
    ropeC_d = din("ropeC", [128, L]); ropeS_d = din("ropeS", [128, L]); ropeP_d = din("ropeP", [128, 128])
    ones2_d = din("ones2", [128, 128])
    y_d = nc.dram_tensor("y", [nb * L, D], F32, kind="ExternalOutput")
    dbg_d = {k: nc.dram_tensor("dbg_" + k, list(s), F32, kind="ExternalOutput") for k, s in dbg}

    xres = nc.dram_tensor("xres", [T, D], F32)
    qk_d = nc.dram_tensor("qk_d", [7, 128, T], BF16)
    v_d = nc.dram_tensor("v_d", [T, 384], BF16)
    yT_d = nc.dram_tensor("yT_d", [D, T], BF16)
    h2T_d = nc.dram_tensor("h2T_d", [128, 8, T], BF16)
    Gt_d = nc.dram_tensor("Gt_d", [NB1 * 2, 128, D], F32)

    with ExitStack() as es:
        pg = Prog(nc, es)

        uid = [0]
        minrem = [1 << 30]

        def sb(name, shape, dt=F32, stack=es):
            uid[0] += 1
            t_ = stack.enter_context(nc.sbuf_tensor("s%d_%s" % (uid[0], name), list(shape), dt))
            minrem[0] = min(minrem[0], nc.sbuf_bytes_remaining)
            return t_

        def ps(name, shape, dt=F32, stack=es):
            uid[0] += 1
            return stack.enter_context(nc.psum_tensor("p%d_%s" % (uid[0], name), list(shape), dt))

        RS = pg.res
        r_hT = RS("hT")
        tok_d = nc.dram_tensor("bar_tok", [2, 64], F32); r_bar = RS("bar")
        identf = sb("identf", [128, 128]); identb = sb("identb", [128, 128], BF16)
        ones2 = sb("ones2b", [128, 128], BF16); onesb = sb("onesb", [128, 128], BF16); onesf = sb("onesf", [128, 128])
        csf = sb("csf", [128, 8 * NB1]); csb = sb("csb", [128, 8 * NB1], BF16)
        MV = sb("MV", [128, 4 * 8 * NB1])
        stage = sb("stage", [128, 128])
        r_const = RS("const"); r_cs = RS("cs"); r_MV = RS("MV"); r_stage = RS("stage")
        psT = ps("psT", [128, 1024]); r_psT = RS("psT")
        pb = [ps("pb%d" % i, [128, 512]) for i in range(5)]; r_pb = [RS("pb%d" % i) for i in range(5)]
        pbf = ps("pbf", [128, 1024], BF16); r_pbf = RS("pbf")
        r_xres = [RS("xres%d" % i) for i in range(NT)]
        r_y = RS("y"); r_qk = RS("qk_d"); r_v = RS("v_d"); r_yT = RS("yT_d"); r_h2 = RS("h2T_d"); r_Gt = RS("Gt_d")
        r_dbg = {k: RS("dbg" + k) for k in dbg_d}
        rot = {"pb": 0}

        def nextpb():
            i = rot["pb"]; rot["pb"] = (i + 1) % 5
            return pb[i], r_pb[i]

        def tile_info(i):
            g0 = i * 128; b = g0 // N; pos0 = g0 - b * N
            return b, pos0, pos0 < LC

        pg.dma("sp", identf[:, :], ident_d[:, :], W=[r_const])
        pg.dma("sp", stage[:, :], ones2_d[:, :], W=[r_stage])
        pg.op("dve", lambda e: e.tensor_copy(identb[:, :], identf[:, :]), R=[r_const], W=[r_const])
        pg.op("dve", lambda e: e.tensor_copy(ones2[:, :], stage[:, :]), R=[r_stage], W=[r_const])
        pg.op("dve", lambda e: e.memset(onesb[:, :], 1.0), W=[r_const])
        pg.op("dve", lambda e: e.memset(onesf[:, :], 1.0), W=[r_const])
        pg.dma("sp", csf[:, :], ccT_d[:, :], W=[r_cs])
        pg.op("act", lambda e: e.activation(csf[:, :], csf[:, :], AF.Silu), R=[r_cs], W=[r_cs])
        pg.op("dve", lambda e: e.tensor_copy(csb[:, :], csf[:, :]), R=[r_cs], W=[r_cs])
        r_init = RS("init")
        for b in range(nb):
            for (src, s0, n, p0) in ((ctx_d, b * LC, LC, 0), (x_d, b * L, L, LC)):
                for j in range(n // 128):
                    gi = (b * N + p0) // 128 + j
                    pg.dma("sp", xres[gi * 128:(gi + 1) * 128, :], src[s0 + j * 128:s0 + (j + 1) * 128, :], W=[r_init])

        pg.barrier((tok_d[0:1, :], ident_d[0:1, 0:64]), r_bar)

        def dbg_out(key, ap, res_list, rows=128):
            if key in dbg_d:
                pg.dma("sp", dbg_d[key][0:rows, :], ap, R=res_list, W=[r_dbg[key]])

        def phaseA(l):
            with ExitStack() as st:
                wts = [sb("adaw%d" % i, [128, 8 * 1024], BF16, st) for i in range(2)]
                r_w = [RS("adaw%d" % i) for i in range(2)]
                abT = sb("abT", [128, 48], F32, st); abrow = sb("abrow", [1, 6 * D], F32, st); abrowb = sb("abrowb", [1, 6 * D], BF16, st)
                nT = sb("nT", [128, 16], F32, st); modT = sb("modT", [128, 4 * 8 * NB1], F32, st)
                gt = [sb("gt%d" % i, [128, 512], F32, st) for i in range(2)]; r_gt = [RS("gt%d" % i) for i in range(2)]
                r_ab = RS("ab"); r_mod = RS("modT")
                csrep = sb("csrep", [128, 8 * NB1 * 128], BF16, st); zerosf = sb("zerosf", [128, 128], F32, st)
                pg.op("dve", lambda e: e.memset(zerosf[:, :], 0.0), W=[r_cs])
                for kc in range(8):
                    for b in range(NB1):
                        c0 = (kc * NB1 + b)
                        pg.op("dve", lambda e: e.tensor_scalar(csrep[:, c0 * 128:(c0 + 1) * 128], zerosf[:, 0:128], csf[:, c0:c0 + 1], None, ALU.add), R=[r_cs], W=[r_cs])
                pg.dma("sp", abT[:, :], ada_bT[l, :, :], W=[r_ab])
                pg.dma("sp", abrow[:, :], ada_b[l, :, :], W=[r_ab])
                pg.dma("sp", nT[:, 0:8], n1T_d[l, :, :], W=[r_ab])
                pg.dma("sp", nT[:, 8:16], n2T_d[l, :, :], W=[r_ab])
                pg.op("act", lambda e: e.activation(abrowb[:, :], abrow[:, :], AF.Copy), R=[r_ab], W=[r_ab])
                gi = 0
                for blk in range(6):
                    wt, rw = wts[blk % 2], r_w[blk % 2]
                    for kc in range(8):
                        pg.dma("pool", wt[:, kc * 1024:(kc + 1) * 1024], ada_w[l, kc * 128:(kc + 1) * 128, blk * 1024:(blk + 1) * 1024], W=[rw])
                    if blk in (0, 1, 3, 4):
                        fm = {0: 0, 1: 1, 3: 2, 4: 3}[blk]
                        p, rp = nextpb()
                        for j in range(8):
                            for kc in range(8):
                                pg.op("pe", lambda e, p=p, j=j, kc=kc, wt=wt: e.matmul(
                                    p[:, j * NB1:(j + 1) * NB1], wt[:, kc * 1024 + j * 128: kc * 1024 + (j + 1) * 128],
                                    csb[:, kc * NB1:(kc + 1) * NB1], start=(kc == 0), stop=(kc == 7)), R=[rw, r_cs], W=[rp])
                        for j in range(8):
                            pg.op("dve", lambda e, p=p, j=j, fm=fm, blk=blk: e.tensor_scalar(
                                modT[:, (fm * 8 + j) * NB1:(fm * 8 + j + 1) * NB1], p[:, j * NB1:(j + 1) * NB1],
                                abT[:, blk * 8 + j: blk * 8 + j + 1], None, ALU.add), R=[rp, r_ab], W=[r_mod])
                    else:
                        which = 0 if blk == 2 else 1
                        for b in range(NB1):
                            for half in range(2):
                                p, rp = nextpb()
                                for kc in range(8):
                                    c0 = kc * NB1 + b
                                    pg.op("pe", lambda e, p=p, kc=kc, c0=c0, half=half, wt=wt: e.matmul(
                                        p[:, :], csrep[:, c0 * 128:(c0 + 1) * 128], wt[:, kc * 1024 + half * 512: kc * 1024 + (half + 1) * 512],
                                        start=(kc == 0), stop=False), R=[rw, r_cs], W=[rp])
                                cb = blk * 1024 + half * 512
                                pg.op("pe", lambda e, p=p, cb=cb: e.matmul(p[:, :], onesb[0:1, :], abrowb[0:1, cb:cb + 512], start=False, stop=True),
                                      R=[r_ab, r_const], W=[rp])
                                g, rg = gt[gi % 2], r_gt[gi % 2]; gi += 1
                                pg.op("act", lambda e, g=g, p=p: e.activation(g[:, :], p[:, :], AF.Copy), R=[rp], W=[rg])
                                pg.dma("sp", Gt_d[b * 2 + which, :, half * 512:(half + 1) * 512], g[:, :], R=[rg], W=[r_Gt])
                for j in range(8):
                    for (w, scf, shf, nofs) in ((0, 1, 0, 0), (2, 3, 2, 8)):
                        pg.op("dve", lambda e, j=j, w=w, scf=scf, nofs=nofs: e.tensor_scalar(
                            MV[:, (w * 8 + j) * NB1:(w * 8 + j + 1) * NB1], modT[:, (scf * 8 + j) * NB1:(scf * 8 + j + 1) * NB1],
                            1.0, nT[:, nofs + j:nofs + j + 1], ALU.add, ALU.mult), R=[r_mod, r_ab], W=[r_MV])
                        pg.op("dve", lambda e, j=j, w=w, shf=shf: e.tensor_copy(
                            MV[:, ((w + 1) * 8 + j) * NB1:((w + 1) * 8 + j + 1) * NB1], modT[:, (shf * 8 + j) * NB1:(shf * 8 + j + 1) * NB1]),
                            R=[r_mod], W=[r_MV])

        nm = {}

        def norm_setup(st):
            nm["junk"] = sb("nm_junk", [128, 1024], BF16, st); nm["ss"] = [sb("nm_ss%d" % i, [128, 2], F32, st) for i in range(2)]
            nm["xn"] = [sb("nm_xn%d" % i, [128, 1024], F32, st) for i in range(2)]
            nm["r_junk"] = RS("nm_junk"); nm["r_ss"] = [RS("nm_ss%d" % i) for i in range(2)]; nm["r_xn"] = [RS("nm_xn%d" % i) for i in range(2)]
            nm["k"] = 0

        def norm_mod_T(i, xt, r_xt, w, dstT=None, r_dst=None, keep_xnT=None):
            b, pos0, isc = tile_info(i)
            bm = nb if isc else b
            k = nm["k"]; nm["k"] = 1 - k
            ss, r_ss, xn, r_xn = nm["ss"][k], nm["r_ss"][k], nm["xn"][k], nm["r_xn"][k]
            pg.op("act", lambda e: e.activation(nm["junk"][:, :], xt[:, :], AF.Square, accum_out=ss[:, 0:1]), R=[r_xt], W=[nm["r_junk"], r_ss])
            pg.op("act", lambda e: e.activation(ss[:, 1:2], ss[:, 0:1], AF.Ln, scale=1.0 / D, bias=EPS), R=[r_ss], W=[r_ss])
            pg.op("act", lambda e: e.activation(ss[:, 1:2], ss[:, 1:2], AF.Exp, scale=-0.5), R=[r_ss], W=[r_ss])
            pg.op("dve", lambda e: e.tensor_scalar(xn[:, :], xt[:, :], ss[:, 1:2], None, ALU.mult), R=[r_xt, r_ss], W=[r_xn])
            for j in range(8):
                pg.op("pe", lambda e, j=j: e.transpose(psT[:, j * 128:(j + 1) * 128], xn[:, j * 128:(j + 1) * 128], identf[:, :]),
                      R=[r_xn, r_const], W=[r_psT])
            for j in range(8):
                ca = (w * 8 + j) * NB1 + bm; cbb = ((w + 1) * 8 + j) * NB1 + bm
                eng = "act" if j % 2 == 0 else "dve"
                dst = hT[:, j * T + i * 128: j * T + (i + 1) * 128]
                if eng == "act":
                    pg.op("act", lambda e, j=j, ca=ca, cbb=cbb, dst=dst: e.activation(dst, psT[:, j * 128:(j + 1) * 128], AF.Identity,
                                                                                    bias=MV[:, cbb:cbb + 1], scale=MV[:, ca:ca + 1]),
                          R=[r_psT, r_MV], W=[r_hT])
                else:
                    pg.op("dve", lambda e, j=j, ca=ca, cbb=cbb, dst=dst: e.tensor_scalar(dst, psT[:, j * 128:(j + 1) * 128], MV[:, ca:ca + 1],
                                                                                       MV[:, cbb:cbb + 1], ALU.mult, ALU.add),
                          R=[r_psT, r_MV], W=[r_hT])
            if keep_xnT is not None:
                kx, r_kx = keep_xnT
                pg.op("act", lambda e: e.activation(kx[:, :], psT[:, :], AF.Copy), R=[r_psT], W=[r_kx])

        def phaseB(l):
            with ExitStack() as st:
                norm_setup(st)
                xts = [sb("xt%d" % i, [128, 1024], F32, st) for i in range(2)]; r_xts = [RS("xt%d" % i) for i in range(2)]
                for i in range(NT):
                    xt, r_xt = xts[i % 2], r_xts[i % 2]
                    pg.dma("sp", xt[:, :], xres[i * 128:(i + 1) * 128, :], R=[r_xres[i]], W=[r_xt])
                    norm_mod_T(i, xt, r_xt, 0)

        def tok_chunks(maxn=512):
            out = []
            for b in range(nb):
                for (p0, n, isc) in ((0, LC, True), (LC, L, False)):
                    o = 0
                    while o < n:
                        m = min(maxn, n - o)
                        out.append((b, b * N + p0 + o, m, isc, o))
                        o += m
            return out

        SLOTS = [((C_NAQ, C_NAQ + 64), 0, False), ((C_NAQ + 128, C_NAQ + 192), 0, False),
                 ((C_NAK, C_NAK + 64), 1, False), ((C_NAK + 128, C_NAK + 192), 1, False),
                 ((C_SWQ, C_SWQ + 128), 2, True), ((C_SWQ + 64, C_SWQ + 192), 2, True),
                 ((C_SWK, C_SWK + 64), 3, True)]

        def phaseC(l):
            with ExitStack() as st:
                wqk = sb("wqk", [128, 8, 7 * 128], BF16, st); wv = sb("wv", [128, 8, 384], BF16, st)
                gt4 = sb("gt4", [128, 4], F32, st); r_w = RS("wqk"); r_g = RS("gt4")
                rC = sb("ropeC", [128, L], F32, st); rS = sb("ropeS", [128, L], F32, st); rP = sb("ropeP", [128, 128], BF16, st); r_rope = RS("rope")
                sq = [sb("c_sq%d" % i, [128, 512], BF16, st) for i in range(2)]; r_sq = [RS("c_sq%d" % i) for i in range(2)]
                rstd = [sb("c_rstd%d" % i, [128, 512], F32, st) for i in range(2)]; r_rstd = [RS("c_rstd%d" % i) for i in range(2)]
                qn = [sb("c_qn%d" % i, [128, 512], BF16, st) for i in range(2)]; r_qn = [RS("c_qn%d" % i) for i in range(2)]
                t1 = [sb("c_t1%d" % i, [128, 512], F32, st) for i in range(2)]; r_t1 = [RS("c_t1%d" % i) for i in range(2)]
                t2 = [sb("c_t2%d" % i, [128, 512], F32, st) for i in range(2)]; r_t2 = [RS("c_t2%d" % i) for i in range(2)]
                qo = [sb("c_qo%d" % i, [128, 512], BF16, st) for i in range(2)]; r_qo = [RS("c_qo%d" % i) for i in range(2)]
                vs = [sb("c_vs%d" % i, [128, 384], BF16, st) for i in range(2)]; r_vs = [RS("c_vs%d" % i) for i in range(2)]
                for s, (cols, gcol, rope) in enumerate(SLOTS):
                    for g, c0 in enumerate(cols):
                        pg.dma("pool", wqk[:, :, s * 128 + g * 64: s * 128 + (g + 1) * 64],
                               w_in[l, :, c0:c0 + 64].rearrange("(kc p) n -> p kc n", p=128), W=[r_w])
                for (c0, n, o) in ((C_NAV, 256, 0), (C_SWV, 128, 256)):
                    pg.dma("pool", wv[:, :, o:o + n], w_in[l, :, c0:c0 + n].rearrange("(kc p) n -> p kc n", p=128), W=[r_w])
                pg.dma("sp", gt4[:, :], gains4[l, :, :], W=[r_g])
                for c in (0, 2):
                    pg.op("dve", lambda e, c=c: e.tensor_scalar(gt4[:, c:c + 1], gt4[:, c:c + 1], HD ** -0.5, None, ALU.mult), R=[r_g], W=[r_g])
                pg.dma("sp", rC[:, :], ropeC_d[:, :], W=[r_rope]); pg.dma("sp", rS[:, :], ropeS_d[:, :], W=[r_rope])
                pg.dma("sp", stage[:, :], ropeP_d[:, :], W=[r_stage])
                pg.op("dve", lambda e: e.tensor_copy(rP[:, :], stage[:, :]), R=[r_stage], W=[r_rope])
                it = 0
                for (b, g0, n, isc, o) in tok_chunks():
                    for s, (cols, gcol, rope) in enumerate(SLOTS):
                        k = it % 2; it += 1
                        p, rp = nextpb()
                        for kc in range(8):
                            pg.op("pe", lambda e, p=p, kc=kc, s=s, g0=g0, n=n: e.matmul(
                                p[:, 0:n], wqk[:, kc, s * 128:(s + 1) * 128], hT[:, kc * T + g0: kc * T + g0 + n], start=(kc == 0), stop=(kc == 7)),
                                R=[r_w, r_hT], W=[rp])
                        pg.op("act", lambda e, p=p, k=k, n=n: e.activation(sq[k][:, 0:n], p[:, 0:n], AF.Square), R=[rp], W=[r_sq[k]])
                        p2, rp2 = nextpb()
                        pg.op("pe", lambda e, p2=p2, k=k, n=n: e.matmul(p2[:, 0:n], ones2[:, :], sq[k][:, 0:n], start=True, stop=True),
                              R=[r_sq[k], r_const], W=[rp2])
                        pg.op("act", lambda e, p2=p2, k=k, n=n: e.activation(rstd[k][:, 0:n], p2[:, 0:n], AF.Ln, scale=1.0 / HD, bias=EPS), R=[rp2], W=[r_rstd[k]])
                        pg.op("act", lambda e, k=k, n=n: e.activation(rstd[k][:, 0:n], rstd[k][:, 0:n], AF.Exp, scale=-0.5), R=[r_rstd[k]], W=[r_rstd[k]])
                        dorope = rope and not isc
                        dst, r_dst = (qn[k], r_qn[k]) if dorope else (qo[k], r_qo[k])
                        pg.op("dve", lambda e, p=p, k=k, n=n, dst=dst, gcol=gcol: e.scalar_tensor_tensor(
                            dst[:, 0:n], p[:, 0:n], gt4[:, gcol:gcol + 1], rstd[k][:, 0:n], ALU.mult, ALU.mult), R=[rp, r_g, r_rstd[k]], W=[r_dst])
                        if dorope:
                            p3, rp3 = nextpb()
                            pg.op("pe", lambda e, p3=p3, k=k, n=n: e.matmul(p3[:, 0:n], rP[:, :], qn[k][:, 0:n], start=True, stop=True),
                                  R=[r_qn[k], r_rope], W=[rp3])
                            pg.op("dve", lambda e, k=k, n=n, o=o: e.tensor_tensor(t1[k][:, 0:n], qn[k][:, 0:n], rC[:, o:o + n], ALU.mult),
                                  R=[r_qn[k], r_rope], W=[r_t1[k]])
                            pg.op("dve", lambda e, p3=p3, k=k, n=n, o=o: e.tensor_tensor(t2[k][:, 0:n], p3[:, 0:n], rS[:, o:o + n], ALU.mult),
                                  R=[rp3, r_rope], W=[r_t2[k]])
                            pg.op("pool", lambda e, k=k, n=n: e.tensor_tensor(qo[k][:, 0:n], t1[k][:, 0:n], t2[k][:, 0:n], ALU.add),
                                  R=[r_t1[k], r_t2[k]], W=[r_qo[k]])
                        pg.dma("sp", qk_d[s, :, g0:g0 + n], qo[k][:, 0:n], R=[r_qo[k]], W=[r_qk])
                for i in range(NT):
                    k = i % 2
                    p, rp = nextpb()
                    for kc in range(8):
                        pg.op("pe", lambda e, p=p, kc=kc, i=i: e.matmul(p[:, 0:384], hT[:, kc * T + i * 128: kc * T + (i + 1) * 128], wv[:, kc, :],
                                                                    start=(kc == 0), stop=(kc == 7)), R=[r_w, r_hT], W=[rp])
                    pg.op("act", lambda e, p=p, k=k: e.activation(vs[k][:, :], p[:, 0:384], AF.Copy), R=[rp], W=[r_vs[k]])
                    pg.dma("sp", v_d[i * 128:(i + 1) * 128, :], vs[k][:, :], R=[r_vs[k]], W=[r_v])

        at = {}

        def attn_setup(st):
            at["E"] = [sb("at_E%d" % i, [128, 512], BF16, st) for i in range(3)]; at["r_E"] = [RS("at_E%d" % i) for i in range(3)]
            at["rd"] = [sb("at_rd%d" % i, [64, 128], F32, st) for i in range(2)]; at["r_rd"] = [RS("at_rd%d" % i) for i in range(2)]
            at["ke"] = 0; at["kr"] = 0

        def attn_unit(hp, q_ap, keys, nq, dst, r_dst, Rq, sink_ap=None):
            per = 512 // nq
            groups = [keys[i:i + per] for i in range(0, len(keys), per)]
            Es = []
            for grp in groups:
                p, rp = nextpb()
                for t, (k_ap, b_ap, v_ap) in enumerate(grp):
                    pg.op("pe", lambda e, p=p, t=t, k_ap=k_ap, b_ap=b_ap: e.matmul(p[:, t * nq:(t + 1) * nq], k_ap, q_ap, start=True, stop=(b_ap is None)),
                          R=Rq, W=[rp])
                    if b_ap is not None:
                        pg.op("pe", lambda e, p=p, t=t, b_ap=b_ap: e.matmul(p[:, t * nq:(t + 1) * nq], identb[:, :], b_ap, start=False, stop=True),
                              R=Rq + [r_const], W=[rp])
                ke = at["ke"]; at["ke"] = (ke + 1) % 3
                E, rE = at["E"][ke], at["r_E"][ke]
                w = len(grp) * nq
                pg.op("act", lambda e, p=p, E=E, w=w: e.activation(E[:, 0:w], p[:, 0:w], AF.Exp), R=[rp], W=[rE])
                Es.append((E, rE, grp))
            po, rpo = nextpb()
            flat = [(E, rE, t, v_ap) for (E, rE, grp) in Es for t, (_, _, v_ap) in enumerate(grp)]
            for i, (E, rE, t, v_ap) in enumerate(flat):
                pg.op("pe", lambda e, E=E, t=t, i=i: e.matmul(po[0:64, nq:2 * nq], onesb[:, 0:64], E[:, t * nq:(t + 1) * nq], start=(i == 0), stop=(i == len(flat) - 1)),
                      R=[rE, r_const], W=[rpo])
            for i, (E, rE, t, v_ap) in enumerate(flat):
                pg.op("pe", lambda e, E=E, t=t, i=i, v_ap=v_ap: e.matmul(po[0:64, 0:nq], v_ap, E[:, t * nq:(t + 1) * nq], start=(i == 0), stop=(i == len(flat) - 1)),
                      R=[rE] + Rq, W=[rpo])
            kr = at["kr"]; at["kr"] = 1 - kr
            rd, r_rd = at["rd"][kr], at["r_rd"][kr]
            if sink_ap is not None:
                pg.op("dve", lambda e: e.tensor_scalar(rd[:, 0:nq], po[0:64, nq:2 * nq], sink_ap, None, ALU.add), R=[rpo] + Rq, W=[r_rd])
                pg.op("dve", lambda e: e.reciprocal(rd[:, 0:nq], rd[:, 0:nq]), R=[r_rd], W=[r_rd])
            else:
                pg.op("dve", lambda e: e.reciprocal(rd[:, 0:nq], po[0:64, nq:2 * nq]), R=[rpo], W=[r_rd])
            pg.op("dve", lambda e: e.tensor_tensor(dst, po[0:64, 0:nq], rd[:, 0:nq], ALU.mult), R=[rpo, r_rd], W=[r_dst])

        def phaseEF(l, last):
            with ExitStack() as st:
                attn_setup(st)
                QK = sb("QK", [128, 7, N], BF16, st); VE = sb("VE", [128, N // 128, 384], BF16, st); VO = sb("VO", [128, L // 128, 384], BF16, st)
                nabf = sb("nabf", [128, 14 * 64], F32, st); nabt = sb("nabt", [128, NA_H * 14 * 64], BF16, st)
                swmf = sb("swmf", [128, 256], F32, st); swm = sb("swm", [128, 256], BF16, st); esink = sb("esink", [128, SW_QH], F32, st)
                stg = [sb("stg%d" % i, [64, N], BF16, st) for i in range(2)]; r_stg = [RS("stg%d" % i) for i in range(2)]
                r_in = RS("attn_in"); r_tab = RS("attn_tab"); r_nabf = RS("nabf")
                for h in range(NA_H):
                    pg.dma("sp", nabf[:, :], nab_d[l, :, h * 896:(h + 1) * 896], W=[r_nabf])
                    pg.op("act", lambda e, h=h: e.activation(nabt[:, h * 896:(h + 1) * 896], nabf[:, :], AF.Copy), R=[r_nabf], W=[r_tab])
                pg.dma("sp", swmf[:, :], swam_d[:, :], W=[r_nabf])
                pg.op("act", lambda e: e.activation(swm[:, :], swmf[:, :], AF.Copy), R=[r_nabf], W=[r_tab])
                pg.dma("sp", esink[:, :], sink_d[l, :, :], W=[r_tab])
                pg.op("act", lambda e: e.activation(esink[:, :], esink[:, :], AF.Exp), R=[r_tab], W=[r_tab])
                Rq = [r_in, r_tab]
                ks = 0
                LT = LC // 128
                for b in range(nb):
                    g0 = b * N
                    for s in range(7):
                        pg.dma("sp", QK[:, s, :], qk_d[s, :, g0:g0 + N], R=[r_qk], W=[r_in])
                    pg.dma("sp", VE[:, :, :], v_d[g0:g0 + N, :].rearrange("(t p) c -> p t c", p=128), R=[r_v], W=[r_in])
                    pg.dma("sp", VO[:, 0:L // 128 - 1, :], v_d[g0 + LC + 64:g0 + LC + 64 + L - 128, :].rearrange("(t p) c -> p t c", p=128), R=[r_v], W=[r_in])
                    for h in range(NA_H):
                        sq_, sk_, hp = h // 2, 2 + h // 2, h % 2
                        ps0 = hp * 64
                        sg, r_sg = stg[ks % 2], r_stg[ks % 2]; ks += 1
                        ckeys = [(QK[ps0:ps0 + 64, sk_, t * 128:(t + 1) * 128], None, VE[:, t, h * 64:(h + 1) * 64]) for t in range(LT)]
                        if not last:
                            for qt in range(LT):
                                attn_unit(hp, QK[ps0:ps0 + 64, sq_, qt * 128:(qt + 1) * 128], ckeys, 128, sg[:, qt * 128:(qt + 1) * 128], r_sg, Rq)
                        for r in range(R):
                            s0 = min(max(r - 4, 0), R - 8)
                            keys = []
                            for t in range(4):
                                kr_ = s0 + 2 * t
                                pos = LC + 64 * kr_
                                d0 = (s0 - r + 7) + 2 * t
                                v_ap = VE[:, LT + kr_ // 2, h * 64:(h + 1) * 64] if kr_ % 2 == 0 else VO[:, (kr_ - 1) // 2, h * 64:(h + 1) * 64]
                                keys.append((QK[ps0:ps0 + 64, sk_, pos:pos + 128], nabt[:, (h * 14 + d0) * 64:(h * 14 + d0 + 1) * 64], v_ap))
                            attn_unit(hp, QK[ps0:ps0 + 64, sq_, LC + 64 * r: LC + 64 * (r + 1)], keys + ckeys, 64,
                                      sg[:, LC + 64 * r: LC + 64 * (r + 1)], r_sg, Rq)
                        c0 = 0 if not last else LC
                        pg.dma("sp", yT_d[h * 64:(h + 1) * 64, g0 + c0:g0 + N], sg[:, c0:N], R=[r_sg], W=[r_yT])
                    for hq in range(SW_QH):
                        sq_, half, kv = 4 + hq % 2, hq // 2, hq // 2
                        ps0 = half * 64
                        vc0 = 256 + kv * 64
                        sg, r_sg = stg[ks % 2], r_stg[ks % 2]; ks += 1
                        ckeys = [(QK[ps0:ps0 + 64, 6, t * 128:(t + 1) * 128], None, VE[:, t, vc0:vc0 + 64]) for t in range(LT)]
                        sk_ap = esink[0:64, hq:hq + 1]
                        if not last:
                            for qt in range(LT):
                                attn_unit(half, QK[ps0:ps0 + 64, sq_, qt * 128:(qt + 1) * 128], ckeys, 128, sg[:, qt * 128:(qt + 1) * 128], r_sg, Rq, sk_ap)
                        nblk = L // 128
                        for n in range(nblk):
                            keys = []
                            for (bk, m0) in ((n - 1, 0), (n, None), (n + 1, 128)):
                                if 0 <= bk < nblk:
                                    pos = LC + 128 * bk
                                    keys.append((QK[ps0:ps0 + 64, 6, pos:pos + 128], None if m0 is None else swm[:, m0:m0 + 128], VE[:, LT + bk, vc0:vc0 + 64]))
                            attn_unit(half, QK[ps0:ps0 + 64, sq_, LC + 128 * n: LC + 128 * (n + 1)], keys + ckeys, 128,
                                      sg[:, LC + 128 * n: LC + 128 * (n + 1)], r_sg, Rq, sk_ap)
                        c0 = 0 if not last else LC
                        pg.dma("sp", yT_d[768 + hq * 64:768 + (hq + 1) * 64, g0 + c0:g0 + N], sg[:, c0:N], R=[r_sg], W=[r_yT])

        def phaseG(l, last):
            CH = 16
            NCH = N // CH
            LCH = LC // CH
            with ExitStack() as st:
                whg = [sb("whg%d" % i, [128, 8, 640], BF16, st) for i in range(2)]; r_whg = [RS("whg%d" % i) for i in range(2)]
                qs = sb("g_qs", [128, N], BF16, st); kd = [sb("g_k%d" % i, [128, N], BF16, st) for i in range(2)]
                Pd = [sb("g_P%d" % i, [128, N + 1], F32, st) for i in range(2)]; nPd = [sb("g_nP%d" % i, [128, NCH + 1], F32, st) for i in range(2)]
                sgate = sb("g_sg", [128, N], BF16, st)
                vch = [sb("g_vch%d" % i, [CH, 128], BF16, st) for i in range(4)]; r_vch = [RS("g_vch%d" % i) for i in range(4)]
                mUL = sb("g_mUL", [64, 128], F32, st); hlb = sb("g_hlb", [128, dp * HG_H], F32, st); lbt = sb("g_lbt", [128, 2 * HG_H], F32, st)
                gn = sb("g_gn", [128, 1], F32, st)
                tmpa = [sb("g_ta%d" % i, [128, 512], F32, st) for i in range(2)]; r_tmpa = [RS("g_ta%d" % i) for i in range(2)]
                tmpb = [sb("g_tb%d" % i, [128, 512], F32, st) for i in range(2)]; r_tmpb = [RS("g_tb%d" % i) for i in range(2)]
                sqb = [sb("g_sqb%d" % i, [128, 512], BF16, st) for i in range(2)]; r_sqb = [RS("g_sqb%d" % i) for i in range(2)]
                yo = [sb("g_yo%d" % i, [128, 512], BF16, st) for i in range(2)]; r_yo = [RS("g_yo%d" % i) for i in range(2)]
                ex = [[sb("g_e%d%d" % (j, i), [128, CH], F32, st) for i in range(4)] for j in range(3)]
                r_ex = [[RS("g_e%d%d" % (j, i)) for i in range(4)] for j in range(3)]
                oacc2 = [sb("g_oacc2%d" % i, [128, N], F32, st) for i in range(2)]; r_oacc2 = [RS("g_oacc2%d" % i) for i in range(2)]
                r_pbf2 = [RS("pbf2%d" % i) for i in range(2)]
                oacc = oacc2[0]
                vT = sb("g_vT", [128, N], BF16, st); r_vT = RS("g_vT"); r_pbf3 = [RS("pbf3%d" % i) for i in range(2)]
                lf = oacc2[1]
                qt = [sb("g_qt%d" % i, [128, CH], BF16, st) for i in range(4)]; r_qt = [RS("g_qt%d" % i) for i in range(4)]
                kt = [sb("g_kt%d" % i, [128, CH], BF16, st) for i in range(4)]; r_kt = [RS("g_kt%d" % i) for i in range(4)]
                kh = [sb("g_kh%d" % i, [128, CH], BF16, st) for i in range(4)]; r_kh = [RS("g_kh%d" % i) for i in range(4)]
                Am = [sb("g_Am%d" % i, [CH, CH], BF16, st) for i in range(4)]; r_Am = [RS("g_Am%d" % i) for i in range(4)]
                khT = [sb("g_khT%d" % i, [CH, 128], BF16, st) for i in range(4)]; r_khT = [RS("g_khT%d" % i) for i in range(4)]
                Sf = [sb("g_S%d" % i, [128, 128], F32, st) for i in range(4)]; r_Sf = [RS("g_S%d" % i) for i in range(4)]
                Sb_ = [sb("g_Sb%d" % i, [128, 128], BF16, st) for i in range(4)]; r_Sb = [RS("g_Sb%d" % i) for i in range(4)]
                r_qs = RS("g_qs"); r_k = [RS("g_k0"), RS("g_k1")]; r_P = [RS("g_P0"), RS("g_P1")]; r_sgt = RS("g_sg")
                r_c = RS("g_const")
                pg.dma("sp", mUL[:, :], maskUL_d[:, :], W=[r_c])
                pg.dma("sp", hlb[:, :], hlbT_d[:, :], W=[r_c])
                pg.dma("sp", gn[:, :], hgn_d[l, :, :], W=[r_c])
                if l == 0:
                    pg.op("dve", lambda e: e.memset(lbt[:, 0:HG_H], 0.0), R=[r_c], W=[r_c])
                else:
                    pg.op("dve", lambda e: e.tensor_tensor(lbt[:, 0:HG_H], hlb[:, l * HG_H:(l + 1) * HG_H], hlb[:, 0:HG_H], ALU.subtract), R=[r_c], W=[r_c])
                    pg.op("act", lambda e: e.activation(lbt[:, 0:HG_H], lbt[:, 0:HG_H], AF.Sigmoid), R=[r_c], W=[r_c])
                pg.op("dve", lambda e: e.tensor_scalar(lbt[:, HG_H:2 * HG_H], lbt[:, 0:HG_H], -1.0, 1.0, ALU.mult, ALU.add), R=[r_c], W=[r_c])
                it = {"a": 0, "c": 0}
                for h in range(HG_H):
                    wt, rw = whg[h % 2], r_whg[h % 2]
                    for j, c0 in enumerate((C_HGQ, C_HGFF, C_HGFB, C_HGI, C_HGG)):
                        pg.dma("pool", wt[:, :, j * 128:(j + 1) * 128], w_in[l, :, c0 + h * 128:c0 + (h + 1) * 128].rearrange("(kc p) n -> p kc n", p=128), W=[rw])
                    for b in range(nb):
                        g0 = b * N
                        chunks = [(o, min(512, N - o)) for o in range(0, N, 512)]

                        def proj(j, o, n):
                            p, rp = nextpb()
                            for kc in range(8):
                                pg.op("pe", lambda e, p=p, kc=kc: e.matmul(p[:, 0:n], wt[:, kc, j * 128:(j + 1) * 128], hT[:, kc * T + g0 + o: kc * T + g0 + o + n],
                                                                         start=(kc == 0), stop=(kc == 7)), R=[rw, r_hT], W=[rp])
                            return p, rp
                        for (o, n) in chunks:
                            p, rp = proj(0, o, n)
                            pg.op("act", lambda e, p=p, o=o, n=n: e.activation(qs[:, o:o + n], p[:, 0:n], AF.Silu), R=[rp], W=[r_qs])
                            p, rp = proj(4, o, n)
                            pg.op("act", lambda e, p=p, o=o, n=n: e.activation(sgate[:, o:o + n], p[:, 0:n], AF.Silu), R=[rp], W=[r_sgt])
                            p, rp = proj(3, o, n)
                            pg.op("dve", lambda e, p=p, o=o, n=n: e.tensor_copy(vT[:, o:o + n], p[:, 0:n]), R=[rp], W=[r_vT])
                        for dirn in range(2):
                            kk, rk, P, rP, nP = kd[dirn], r_k[dirn], Pd[dirn], r_P[dirn], nPd[dirn]
                            for (o, n) in chunks:
                                p, rp = proj(1 + dirn, o, n)
                                a = it["a"] % 2; it["a"] += 1
                                pg.op("act", lambda e, p=p, a=a, n=n: e.activation(tmpa[a][:, 0:n], p[:, 0:n], AF.Sigmoid), R=[rp], W=[r_tmpa[a]])
                                pg.op("dve", lambda e, a=a, n=n: e.tensor_scalar(tmpb[a][:, 0:n], tmpa[a][:, 0:n], lbt[:, HG_H + h:HG_H + h + 1], lbt[:, h:h + 1], ALU.mult, ALU.add),
                                      R=[r_tmpa[a], r_c], W=[r_tmpb[a]])
                                pg.op("act", lambda e, a=a, o=o, n=n: e.activation(lf[:, o:o + n], tmpb[a][:, 0:n], AF.Ln), R=[r_tmpb[a]], W=[r_oacc2[1]])
                                pg.op("dve", lambda e, a=a, o=o, n=n, kk=kk: e.tensor_scalar(kk[:, o:o + n], tmpb[a][:, 0:n], -1.0, 1.0, ALU.mult, ALU.add),
                                      R=[r_tmpb[a]], W=[rk])
                            pg.op("dve", lambda e, P=P: e.memset(P[:, 0:1], 0.0), W=[rP])
                            pg.op("dve", lambda e, P=P: e.tensor_tensor_scan(P[:, 1:N + 1], lf[:, :], lf[:, :], 0.0, ALU.add, ALU.bypass), R=[r_oacc2[1], r_c], W=[rP])
                            pg.op("dve", lambda e, P=P, nP=nP: e.tensor_scalar(nP[:, :], P[:, 0:N + 1:CH], -1.0, None, ALU.mult), R=[rP], W=[rP])
                        orders = [list(range(NCH)), list(range(LCH - 1, -1, -1)) + list(range(NCH - 1, LCH - 1, -1))]
                        stt_ = [{"sc": 0, "n": 0}, {"sc": 0, "n": 0}]
                        for dirn in range(2):
                            pg.op("dve", lambda e: e.memset(Sf[2 * dirn][:, :], 0.0), W=[r_Sf[2 * dirn]])
                            pg.op("dve", lambda e: e.memset(Sb_[2 * dirn][:, :], 0.0), W=[r_Sb[2 * dirn]])

                        def step(dirn, c):
                            kk, rk, P, rP, nP = kd[dirn], r_k[dirn], Pd[dirn], r_P[dirn], nPd[dirn]
                            sd = stt_[dirn]
                            sc = 2 * dirn + sd["sc"]; sn = 2 * dirn + 1 - sd["sc"]
                            z = 2 * dirn + sd["n"] % 2; sd["n"] += 1
                            a0 = c * CH
                            need_out = not (last and c < LCH)
                            pvv = pbf[0:CH, 256 + dirn * 128:256 + (dirn + 1) * 128]
                            pg.op("pe", lambda e: e.transpose(pvv, vT[:, a0:a0 + CH], identb[:, :]), R=[r_vT, r_const], W=[r_pbf3[dirn]])
                            pg.op("act", lambda e: e.activation(vch[z][:, :], pvv, AF.Copy), R=[r_pbf3[dirn]], W=[r_vch[z]])
                            if dirn == 0:
                                src = P[:, a0 + 1:a0 + CH + 1]
                                specs = ((1.0, nP[:, c:c + 1]), (-1.0, P[:, a0:a0 + 1]), (-1.0, P[:, a0 + CH:a0 + CH + 1]))
                                ebe = ex[0][z][:, CH - 1:CH]; mk = mUL[0:CH, 0:CH]
                            else:
                                src = P[:, a0:a0 + CH]
                                specs = ((-1.0, P[:, a0 + CH:a0 + CH + 1]), (1.0, nP[:, c + 1:c + 2]), (1.0, nP[:, c:c + 1]))
                                ebe = ex[0][z][:, 0:1]; mk = mUL[0:CH, 64:64 + CH]
                            for j, (scl, bias) in enumerate(specs):
                                if j == 1 and not need_out:
                                    continue
                                pg.op("act", lambda e: e.activation(ex[j][z][:, :], src, AF.Exp, bias=bias, scale=scl), R=[rP], W=[r_ex[j][z]])
                            pg.op("dve", lambda e: e.scalar_tensor_tensor(qt[z][:, :], qs[:, a0:a0 + CH], HG_DK ** -0.5, ex[0][z][:, :], ALU.mult, ALU.mult),
                                  R=[r_qs, r_ex[0][z]], W=[r_qt[z]])
                            pg.op("dve", lambda e: e.tensor_tensor(kh[z][:, :], kk[:, a0:a0 + CH], ex[2][z][:, :], ALU.mult), R=[rk, r_ex[2][z]], W=[r_kh[z]])
                            if need_out:
                                pg.op("dve", lambda e: e.tensor_tensor(kt[z][:, :], kk[:, a0:a0 + CH], ex[1][z][:, :], ALU.mult), R=[rk, r_ex[1][z]], W=[r_kt[z]])
                                pA, rpA = nextpb()
                                pg.op("pe", lambda e: e.matmul(pA[0:CH, 0:CH], kt[z][:, :], qt[z][:, :], start=True, stop=True), R=[r_kt[z], r_qt[z]], W=[rpA])
                                pg.op("dve", lambda e: e.tensor_tensor(Am[z][:, :], pA[0:CH, 0:CH], mk, ALU.mult), R=[rpA, r_c], W=[r_Am[z]])
                            pbv = pbf[0:CH, dirn * 128:(dirn + 1) * 128]
                            pg.op("pe", lambda e: e.transpose(pbv, kh[z][:, :], identb[:, :]), R=[r_kh[z], r_const], W=[r_pbf2[dirn]])
                            pg.op("act", lambda e: e.activation(khT[z][:, :], pbv, AF.Copy), R=[r_pbf2[dirn]], W=[r_khT[z]])
                            if need_out:
                                pO, rpO = nextpb()
                                pg.op("pe", lambda e: e.matmul(pO[:, 0:CH], vch[z][:, :], Am[z][:, :], start=True, stop=False), R=[r_vch[z], r_Am[z]], W=[rpO])
                                pg.op("pe", lambda e: e.matmul(pO[:, 0:CH], Sb_[sc][:, :], qt[z][:, :], start=False, stop=True), R=[r_Sb[sc], r_qt[z]], W=[rpO])
                                pg.op("dve", lambda e: e.tensor_copy(oacc2[dirn][:, a0:a0 + CH], pO[:, 0:CH]), R=[rpO], W=[r_oacc2[dirn]])
                            pS, rpS = nextpb()
                            pg.op("pe", lambda e: e.matmul(pS[:, 0:128], khT[z][:, :], vch[z][:, :], start=True, stop=True), R=[r_khT[z], r_vch[z]], W=[rpS])
                            pg.op("dve", lambda e: e.scalar_tensor_tensor(Sf[sn][:, :], Sf[sc][:, :], ebe, pS[:, 0:128], ALU.mult, ALU.add),
                                  R=[r_Sf[sc], rpS, r_ex[0][z]], W=[r_Sf[sn]])
                            pg.op("act", lambda e: e.activation(Sb_[sn][:, :], Sf[sn][:, :], AF.Copy), R=[r_Sf[sn]], W=[r_Sb[sn]])
                            sd["sc"] = 1 - sd["sc"]

                        for idx in range(NCH):
                            step(0, orders[0][idx])
                            step(1, orders[1][idx])
                        r0 = LC if last else 0
                        pg.op("pool", lambda e: e.tensor_tensor(oacc[:, r0:N], oacc2[0][:, r0:N], oacc2[1][:, r0:N], ALU.add), R=[r_oacc2[1]], W=[r_oacc2[0]])
                        r0 = LC if last else 0
                        for ci, (o, n) in enumerate([(o, min(512, N - o)) for o in range(r0, N, 512)]):
                            a = ci % 2
                            pg.op("act", lambda e, a=a, o=o, n=n: e.activation(sqb[a][:, 0:n], oacc[:, o:o + n], AF.Square), R=[r_oacc2[0]], W=[r_sqb[a]])
                            p, rp = nextpb()
                            pg.op("pe", lambda e, p=p, a=a, n=n: e.matmul(p[:, 0:n], onesb[:, :], sqb[a][:, 0:n], start=True, stop=True), R=[r_sqb[a], r_const], W=[rp])
                            pg.op("act", lambda e, p=p, a=a, n=n: e.activation(tmpa[a][:, 0:n], p[:, 0:n], AF.Ln, scale=1.0 / 128, bias=EPS), R=[rp], W=[r_tmpa[a]])
                            pg.op("act", lambda e, a=a, n=n: e.activation(tmpa[a][:, 0:n], tmpa[a][:, 0:n], AF.Exp, scale=-0.5), R=[r_tmpa[a]], W=[r_tmpa[a]])
                            pg.op("dve", lambda e, a=a, o=o, n=n: e.scalar_tensor_tensor(tmpb[a][:, 0:n], oacc[:, o:o + n], gn[:, 0:1], tmpa[a][:, 0:n], ALU.mult, ALU.mult),
                                  R=[r_oacc2[0], r_tmpa[a], r_c], W=[r_tmpb[a]])
                            pg.op("dve", lambda e, a=a, o=o, n=n: e.tensor_tensor(yo[a][:, 0:n], tmpb[a][:, 0:n], sgate[:, o:o + n], ALU.mult), R=[r_tmpb[a], r_sgt], W=[r_yo[a]])
                            pg.dma("sp", yT_d[256 + h * 128:256 + (h + 1) * 128, g0 + o:g0 + o + n], yo[a][:, 0:n], R=[r_yo[a]], W=[r_yT])

        Gall_d = nc.dram_tensor("Gall_d", [128, NT * NE], F32); GT_d = nc.dram_tensor("GT_d", [NE, T], BF16)
        r_Gall = RS("Gall"); r_GT = RS("GT"); r_Gd = RS("G_d")

        def phaseH(l, last):
            with ExitStack() as st:
                norm_setup(st)
                Gall = sb("Gall", [128, NT * NE], F32, st); GT = sb("GT", [NE, T], BF16, st)
                pg.op("dve", lambda e: e.memset(Gall[:, :], 0.0), W=[r_Gall])
                pg.op("dve", lambda e: e.memset(GT[:, :], 0.0), W=[r_GT])
                wo = sb("wo", [128, 8, 1024], BF16, st); r_wo = RS("wo")
                Wr = sb("Wr", [128, 8, NE], F32, st); Wrp = sb("Wrp", [128, NB1 * 8 * NE], F32, st); rbt = sb("rbt", [1, NE], F32, st)
                rbias = sb("rbias", [1, NB1 * NE], F32, st); r_r = RS("router")
                G1t = sb("G1t", [128, NB1, 1024], F32, st); r_G1 = RS("G1t")
                yts = [sb("h_yt%d" % i, [128, 8, 128], BF16, st) for i in range(2)]; r_yts = [RS("h_yt%d" % i) for i in range(2)]
                xts = [sb("h_xt%d" % i, [128, 1024], F32, st) for i in range(2)]; r_xts = [RS("h_xt%d" % i) for i in range(2)]
                tmp = [sb("h_tmp%d" % i, [128, 512], F32, st) for i in range(2)]; r_tmp = [RS("h_tmp%d" % i) for i in range(2)]
                xnT = sb("h_xnT", [128, 1024], F32, st); r_xnT = RS("h_xnT")
                lg = [sb("h_lg%d" % i, [128, NE], F32, st) for i in range(2)]; r_lg = [RS("h_lg%d" % i) for i in range(2)]
                m8 = [sb("h_m8%d" % i, [128, 16], F32, st) for i in range(2)]
                ee = [sb("h_ee%d" % i, [128, 2 * NE], F32, st) for i in range(2)]
                for kc in range(8):
                    pg.dma("pool", wo[:, kc, :], w_out[l, kc * 128:(kc + 1) * 128, :], W=[r_wo])
                pg.dma("sp", Wr[:, :, :], rw_d[l, :, :].rearrange("(kc p) n -> p kc n", p=128), W=[r_r])
                pg.dma("sp", rbt[:, :], rb_d[l, :, :], W=[r_r])
                for b in range(NB1):
                    pg.dma("sp", G1t[:, b, :], Gt_d[b * 2 + 0, :, :], R=[r_Gt], W=[r_G1])
                    for kc in range(8):
                        ca = (2 * 8 + kc) * NB1 + b
                        pg.op("dve", lambda e: e.tensor_scalar(Wrp[:, (b * 8 + kc) * NE:(b * 8 + kc + 1) * NE], Wr[:, kc, :], MV[:, ca:ca + 1], None, ALU.mult),
                              R=[r_r, r_MV], W=[r_r])
                    p, rp = nextpb()
                    for kc in range(8):
                        cb_ = (3 * 8 + kc) * NB1 + b
                        pg.op("pe", lambda e: e.matmul(p[0:1, 0:NE], MV[:, cb_:cb_ + 1], Wr[:, kc, :], start=(kc == 0), stop=False), R=[r_r, r_MV], W=[rp])
                    pg.op("pe", lambda e: e.matmul(p[0:1, 0:NE], onesf[0:1, 0:1], rbt[0:1, :], start=False, stop=True), R=[r_r, r_const], W=[rp])
                    pg.op("act", lambda e: e.activation(rbias[0:1, b * NE:(b + 1) * NE], p[0:1, 0:NE], AF.Copy), R=[rp], W=[r_r])
                tiles = [i for i in range(NT) if not (last and tile_info(i)[2])]
                for n_, i in enumerate(tiles):
                    b, pos0, isc = tile_info(i)
                    bm = nb if isc else b
                    k = n_ % 2
                    yt, r_yt, xt, r_xt = yts[k], r_yts[k], xts[k], r_xts[k]
                    pg.dma("sp", yt[:, :, :], yT_d[:, i * 128:(i + 1) * 128].rearrange("(kc p) t -> p kc t", p=128), R=[r_yT], W=[r_yt])
                    pg.dma("sp", xt[:, :], xres[i * 128:(i + 1) * 128, :], R=[r_xres[i]], W=[r_xt])
                    for half in range(2):
                        p, rp = nextpb()
                        for kc in range(8):
                            pg.op("pe", lambda e: e.matmul(p[:, :], yt[:, kc, :], wo[:, kc, half * 512:(half + 1) * 512], start=(kc == 0), stop=(kc == 7)),
                                  R=[r_yt, r_wo], W=[rp])
                        pg.op("dve", lambda e: e.tensor_tensor(tmp[half][:, :], p[:, :], G1t[:, bm, half * 512:(half + 1) * 512], ALU.mult), R=[rp, r_G1], W=[r_tmp[half]])
                        pg.op("pool", lambda e: e.tensor_tensor(xt[:, half * 512:(half + 1) * 512], xt[:, half * 512:(half + 1) * 512], tmp[half][:, :], ALU.add),
                              R=[r_tmp[half], r_xt], W=[r_xt])
                    pg.dma("sp", xres[i * 128:(i + 1) * 128, :], xt[:, :], R=[r_xt], W=[r_xres[i]])
                    norm_mod_T(i, xt, r_xt, 2, keep_xnT=(xnT, r_xnT))
                    p, rp = nextpb()
                    for kc in range(8):
                        pg.op("pe", lambda e: e.matmul(p[:, 0:NE], xnT[:, kc * 128:(kc + 1) * 128], Wrp[:, (bm * 8 + kc) * NE:(bm * 8 + kc + 1) * NE], start=(kc == 0), stop=False),
                              R=[r_xnT, r_r], W=[rp])
                    pg.op("pe", lambda e: e.matmul(p[:, 0:NE], onesf[0:1, :], rbias[0:1, bm * NE:(bm + 1) * NE], start=False, stop=True), R=[r_r, r_const], W=[rp])
                    L_, rL = lg[k], r_lg[k]
                    M_, E_ = m8[k], ee[k]
                    pg.op("act", lambda e: e.activation(L_[:, :], p[:, 0:NE], AF.Copy), R=[rp], W=[rL])
                    pg.op("dve", lambda e: e.max(M_[:, 0:8], L_[:, :]), R=[rL], W=[rL])
                    pg.op("dve", lambda e: e.tensor_scalar(M_[:, 8:9], M_[:, 0:1], -1.0, None, ALU.mult), R=[rL], W=[rL])
                    pg.op("act", lambda e: e.activation(E_[:, 0:NE], L_[:, :], AF.Exp, bias=M_[:, 8:9], scale=1.0), R=[rL], W=[rL])
                    pg.op("dve", lambda e: e.tensor_scalar(E_[:, NE:2 * NE], L_[:, :], M_[:, TOPK - 1:TOPK], None, ALU.is_ge), R=[rL], W=[rL])
                    pg.op("dve", lambda e: e.tensor_tensor(E_[:, 0:NE], E_[:, 0:NE], E_[:, NE:2 * NE], ALU.mult), R=[rL], W=[rL])
                    pg.op("dve", lambda e: e.reduce_sum(M_[:, 9:10], E_[:, 0:NE], AX.X), R=[rL], W=[rL])
                    pg.op("dve", lambda e: e.reciprocal(M_[:, 9:10], M_[:, 9:10]), R=[rL], W=[rL])
                    pg.op("dve", lambda e: e.tensor_scalar(Gall[:, i * NE:(i + 1) * NE], E_[:, 0:NE], M_[:, 9:10], None, ALU.mult), R=[rL], W=[r_Gall])
                    p2, rp2 = nextpb()
                    pg.op("pe", lambda e: e.transpose(p2[0:NE, 0:128], Gall[:, i * NE:(i + 1) * NE], identf[:, :]), R=[r_Gall, r_const], W=[rp2])
                    pg.op("act", lambda e: e.activation(GT[:, i * 128:(i + 1) * 128], p2[0:NE, 0:128], AF.Copy), R=[rp2], W=[r_GT])
                for kc in range(8):
                    pg.dma("sp", h2T_d[:, kc, :], hT[:, kc * T:(kc + 1) * T], R=[r_hT], W=[r_h2])
                pg.dma("sp", Gall_d[:, :], Gall[:, :], R=[r_Gall], W=[r_Gd])
                pg.dma("sp", GT_d[:, :], GT[:, :], R=[r_GT], W=[r_Gd])

        def phaseI(l, last):
            TGT = cfg.TG // 128
            banks = [(pb[i], r_pb[i]) for i in range(5)] + [(psT[:, 0:512], RS("psTa")), (psT[:, 512:1024], RS("psTb"))]
            rotI = [0]

            def nextI():
                i = rotI[0]; rotI[0] = (i + 1) % len(banks)
                return banks[i]
            with ExitStack() as st:
                Gall = sb("Gall", [128, NT * NE], F32, st); GT = sb("GT", [NE, T], BF16, st)
                pg.dma("sp", Gall[:, :], Gall_d[:, :], R=[r_Gd], W=[r_Gall])
                pg.dma("sp", GT[:, :], GT_d[:, :], R=[r_Gd], W=[r_GT])
                acc = sb("i_acc", [128, TGT, 1024], F32, st); r_acc = [RS("i_acc%d" % j) for j in range(TGT)]
                h2g = sb("i_h2g", [128, 8, cfg.TG], BF16, st); r_h2g = RS("i_h2g")
                actT = sb("i_actT", [128, FC, cfg.TG], BF16, st); r_actT = RS("i_actT")
                wd = [sb("i_wd%d" % i, [128, FC, 1024], BF16, st) for i in range(2)]; r_wd = [RS("i_wd%d" % i) for i in range(2)]
                wgp = [sb("i_wg%d" % i, [128, 8, 256], BF16, st) for i in range(4)]; r_wgp = [RS("i_wg%d" % i) for i in range(4)]
                bgu = sb("i_bgu", [128, NE * 2 * FC], F32, st); bdf = sb("i_bdf", [NE, 1024], F32, st); bdb = sb("i_bdb", [NE, 1024], BF16, st); r_b = RS("i_bias")
                G2t = sb("i_G2t", [128, NB1, 1024], F32, st); r_G2 = RS("i_G2t")
                tg = [[sb("i_t%d%d" % (j, i), [128, 512], F32, st) for i in range(2)] for j in range(5)]
                r_tg = [[RS("i_t%d%d" % (j, i)) for i in range(2)] for j in range(5)]
                xts = [sb("i_xt%d" % i, [128, 1024], F32, st) for i in range(2)]; r_xts = [RS("i_xt%d" % i) for i in range(2)]
                pg.dma("sp", bgu[:, :], bguT_d[l, :, :], W=[r_b])
                pg.dma("sp", bdf[:, :], bdn_d[l, :, :], W=[r_b])
                pg.op("act", lambda e: e.activation(bdb[:, :], bdf[:, :], AF.Copy), R=[r_b], W=[r_b])
                for b in range(NB1):
                    pg.dma("sp", G2t[:, b, :], Gt_d[b * 2 + 1, :, :], R=[r_Gt], W=[r_G2])
                tiles = [i for i in range(NT) if not (last and tile_info(i)[2])]
                groups = [tiles[i:i + TGT] for i in range(0, len(tiles), TGT)]
                cnt = {"w": 0, "d": 0, "t": 0, "x": 0}
                for grp in groups:
                    ng = len(grp); ntok = ng * 128
                    for j, i in enumerate(grp):
                        pg.dma("sp", h2g[:, :, j * 128:(j + 1) * 128], h2T_d[:, :, i * 128:(i + 1) * 128], R=[r_h2], W=[r_h2g])
                        for half in range(2):
                            p, rp = nextI()
                            pg.op("pe", lambda e: e.matmul(p[:, :], GT[:, i * 128:(i + 1) * 128], bdb[:, half * 512:(half + 1) * 512], start=True, stop=True),
                                  R=[r_GT, r_b], W=[rp])
                            pg.op("act", lambda e: e.activation(acc[:, j, half * 512:(half + 1) * 512], p[:, :], AF.Copy), R=[rp], W=[r_acc[j]])
                    tts = [(o, min(512, ntok - o)) for o in range(0, ntok, 512)]
                    for ex_ in range(NE):
                        wdt, rwd = wd[cnt["d"] % 2], r_wd[cnt["d"] % 2]; cnt["d"] += 1
                        for fc in range(FC):
                            pg.dma("pool", wdt[:, fc, :], wdn_d[l, ex_, fc * 128:(fc + 1) * 128, :], W=[rwd])
                        for fc in range(FC):
                            wg, rwg = wgp[cnt["w"] % 4], r_wgp[cnt["w"] % 4]; cnt["w"] += 1
                            for (c0, o_) in ((fc * 128, 0), (DFF + fc * 128, 128)):
                                pg.dma("pool", wg[:, :, o_:o_ + 128], wgu_d[l, ex_, :, c0:c0 + 128].rearrange("(kc p) n -> p kc n", p=128), W=[rwg])
                            cg = ex_ * 2 * FC + fc; cl = ex_ * 2 * FC + FC + fc
                            for (o, n) in tts:
                                z = cnt["t"] % 2; cnt["t"] += 1
                                pgl, rpgl = nextI()
                                for kc in range(8):
                                    pg.op("pe", lambda e: e.matmul(pgl[:, 0:n], wg[:, kc, 0:128], h2g[:, kc, o:o + n], start=(kc == 0), stop=(kc == 7)), R=[rwg, r_h2g], W=[rpgl])
                                pli, rpli = nextI()
                                for kc in range(8):
                                    pg.op("pe", lambda e: e.matmul(pli[:, 0:n], wg[:, kc, 128:256], h2g[:, kc, o:o + n], start=(kc == 0), stop=(kc == 7)), R=[rwg, r_h2g], W=[rpli])
                                glu, sg_, linb, linc, tt_ = (tg[j][z] for j in range(5))
                                rglu, rsg, rlinb, rlinc, rtt = (r_tg[j][z] for j in range(5))
                                pg.op("dve", lambda e: e.tensor_scalar(glu[:, 0:n], pgl[:, 0:n], bgu[:, cg:cg + 1], 7.0, ALU.add, ALU.min), R=[rpgl, r_b], W=[rglu])
                                pg.op("act", lambda e: e.activation(sg_[:, 0:n], glu[:, 0:n], AF.Sigmoid, scale=1.702), R=[rglu], W=[rsg])
                                pg.op("act", lambda e: e.activation(linb[:, 0:n], pli[:, 0:n], AF.Identity, bias=bgu[:, cl:cl + 1], scale=1.0), R=[rpli, r_b], W=[rlinb])
                                pg.op("dve", lambda e: e.tensor_scalar(linc[:, 0:n], linb[:, 0:n], 7.0, -7.0, ALU.min, ALU.max), R=[rlinb], W=[rlinc])
                                pg.op("dve", lambda e: e.tensor_tensor(tt_[:, 0:n], glu[:, 0:n], sg_[:, 0:n], ALU.mult), R=[rglu, rsg], W=[rtt])
                                pg.op("dve", lambda e: e.scalar_tensor_tensor(actT[:, fc, o:o + n], linc[:, 0:n], 1.0, tt_[:, 0:n], ALU.add, ALU.mult), R=[rlinc, rtt], W=[r_actT])
                        for j, i in enumerate(grp):
                            for half in range(2):
                                p, rp = nextI()
                                for fc in range(FC):
                                    pg.op("pe", lambda e: e.matmul(p[:, :], actT[:, fc, j * 128:(j + 1) * 128], wdt[:, fc, half * 512:(half + 1) * 512], start=(fc == 0), stop=(fc == FC - 1)),
                                          R=[r_actT, rwd], W=[rp])
                                pg.op("dve", lambda e: e.scalar_tensor_tensor(acc[:, j, half * 512:(half + 1) * 512], p[:, :], Gall[:, i * NE + ex_: i * NE + ex_ + 1],
                                                                            acc[:, j, half * 512:(half + 1) * 512], ALU.mult, ALU.add), R=[rp, r_Gall, r_acc[j]], W=[r_acc[j]])
                    for j, i in enumerate(grp):
                        b, pos0, isc = tile_info(i)
                        bm = nb if isc else b
                        k = cnt["x"] % 2; cnt["x"] += 1
                        xt, r_xt = xts[k], r_xts[k]
                        pg.dma("sp", xt[:, :], xres[i * 128:(i + 1) * 128, :], R=[r_xres[i]], W=[r_xt])
                        pg.op("dve", lambda e: e.tensor_tensor(acc[:, j, :], acc[:, j, :], G2t[:, bm, :], ALU.mult), R=[r_acc[j], r_G2], W=[r_acc[j]])
                        pg.op("pool", lambda e: e.tensor_tensor(xt[:, :], xt[:, :], acc[:, j, :], ALU.add), R=[r_acc[j], r_xt], W=[r_xt])
                        if last:
                            row = b * L + (pos0 - LC)
                            pg.dma("sp", y_d[row:row + 128, :], xt[:, :], R=[r_xt], W=[r_y])
                        else:
                            pg.dma("sp", xres[i * 128:(i + 1) * 128, :], xt[:, :], R=[r_xt], W=[r_xres[i]])

        stop_after = getattr(cfg, "stop_after", None)
        def bar():
            pg.barrier((tok_d[0:1, :], ident_d[0:1, 0:64]), r_bar)

        for l in range(dp):
            last = (l == dp - 1)
            with ExitStack() as stL:
                hT = sb("hT%d" % l, [128, 8 * T], BF16, stL)
                phaseA(l); bar()
                phaseB(l); bar()
                phaseC(l); bar()
                phaseEF(l, last); bar()
                phaseG(l, last); bar()
                phaseH(l, last); bar()
            phaseI(l, last); bar()
        pg.finish([r_y] + list(r_dbg.values()))
        nc._n_sems = pg.nsem
        nc._minrem = minrem[0]
        nc._pg = pg
    return nc


_CACHE = {}


def kernel(**inputs):
    n_cores = 8
    B = inputs["x"].shape[0]
    cfg = Cfg(nb=B // n_cores, L=inputs["x"].shape[1], LC=inputs["ctx"].shape[1], NE=inputs["w_gu"].shape[1],
              DFF=inputs["w_down"].shape[2], depth=inputs["w_in"].shape[0], TG=1024)
    nc = build_nc(cfg)
    in_maps = []
    shared = None
    for core in range(n_cores):
        m, shared = prep_inputs(cfg, core, shared=shared, **inputs)
        in_maps.append(m)
    res = run_bass_kernel_spmd(nc, in_maps, core_ids=list(range(n_cores)))
    outs = [np.asarray(r["y"], np.float32).reshape(cfg.nb, cfg.L, D) for r in res.results]
    return np.concatenate(outs, axis=0)
```
